# Optimizing a Trainium2 kernel written in Bass

```python
import math
import jax, jax.numpy as jnp
from jax import lax
import numpy as np

D_MODEL = 1024
BATCH = 16
SEQ = 2048
DEPTH = 4

CONV_W = D_MODEL // 4
CONV_WIDTH = 31
DIFF_HEADS = 4
DIFF_HD = D_MODEL // 16
DIFF_VD = 2 * DIFF_HD
DN_HEADS = 4
DN_HD = D_MODEL // 16
DN_CONV = 5
DN_CHUNK = 64
Q_BLOCK = 128
ROPE_THETA = 10000.0
MIX_WIDTH = CONV_W + DIFF_HEADS * DIFF_VD + DN_HEADS * DN_HD

D_FF = 11 * D_MODEL // 4
N_EXPERTS = 8
TOP_K = 2
D_FF_EXPERT = 7 * D_MODEL // 2
N_DENSE = (DEPTH + 1) // 2
N_MOE = DEPTH // 2

DEEPNORM_ALPHA = (2 * DEPTH) ** 0.25
DEEPNORM_BETA = (8 * DEPTH) ** -0.25
LN_EPS = 1e-5

PROJ_SIZES = (2 * CONV_W,
              DIFF_HEADS * 2 * DIFF_HD,
              DIFF_HEADS * 2 * DIFF_HD,
              DIFF_HEADS * DIFF_VD,
              3 * DN_HEADS * DN_HD,
              DN_HEADS * DN_HD,
              2 * DN_HEADS,
              2 * DN_HEADS)
PROJ_DIM = sum(PROJ_SIZES)

kernel_name = 'hybrid_conv_diffattn_deltanet_moe_encoder'


def layer_norm(x, g, b):
    xf = x.astype(jnp.float32)
    mu = jnp.mean(xf, axis=-1, keepdims=True)
    var = jnp.mean(jnp.square(xf - mu), axis=-1, keepdims=True)
    return ((xf - mu) * lax.rsqrt(var + LN_EPS) * g.astype(jnp.float32) + b.astype(jnp.float32)).astype(x.dtype)


def rms_norm(x, g):
    xf = x.astype(jnp.float32)
    y = xf * lax.rsqrt(jnp.mean(jnp.square(xf), axis=-1, keepdims=True) + LN_EPS)
    return (y * g.astype(jnp.float32)).astype(x.dtype)


def l2_norm(x):
    return x * lax.rsqrt(jnp.sum(jnp.square(x), axis=-1, keepdims=True) + 1e-6)


def depthwise_conv(x, w):
    width, ch = w.shape
    pad = (width - 1) // 2
    return lax.conv_general_dilated(x, w[:, None, :].astype(x.dtype), window_strides=(1,),
                                    padding=[(pad, pad)], dimension_numbers=('NWC', 'WIO', 'NWC'),
                                    feature_group_count=ch)


def split_projection(proj):
    idx, acc = [], 0
    for s in PROJ_SIZES[:-1]:
        acc += s
        idx.append(acc)
    return jnp.split(proj, idx, axis=-1)


def rope_tables(seq_len, dim):
    inv = ROPE_THETA ** (-jnp.arange(0, dim, 2, dtype=jnp.float32) / dim)
    ang = jnp.arange(seq_len, dtype=jnp.float32)[:, None] * inv[None, :]
    return jnp.cos(ang), jnp.sin(ang)


def apply_rope(x, cos, sin):
    half = x.shape[-1] // 2
    xf = x.astype(jnp.float32)
    c = cos[None, :, None, None, :]
    s = sin[None, :, None, None, :]
    x1, x2 = xf[..., :half], xf[..., half:]
    return jnp.concatenate([x1 * c - x2 * s, x2 * c + x1 * s], axis=-1).astype(x.dtype)


def differential_attention(q, k, v, lam, subln_g, lambda_init):
    B, S, H, _, dh = q.shape
    nb = S // Q_BLOCK
    scale = dh ** -0.5
    q_blocks = jnp.moveaxis(q.reshape(B, nb, Q_BLOCK, H, 2, dh), 1, 0)

    def one_block(qb):
        s = jnp.einsum('bqhcd,bkhcd->bhcqk', qb, k).astype(jnp.float32) * scale
        p = jax.nn.softmax(s, axis=-1)
        a = p[:, :, 0] - lam * p[:, :, 1]
        return jnp.einsum('bhqk,bkhe->bqhe', a.astype(v.dtype), v)

    o = lax.map(one_block, q_blocks)
    o = jnp.moveaxis(o, 0, 1).reshape(B, S, H, -1)
    return rms_norm(o, subln_g) * (1.0 - lambda_init)


def chunk_gated_delta_rule(q, k, v, g, beta):
    B, H, S, dk = q.shape
    dv = v.shape[-1]
    C = DN_CHUNK
    N = S // C
    q = (q * dk ** -0.5).reshape(B, H, N, C, dk)
    k = k.reshape(B, H, N, C, dk)
    v = v.reshape(B, H, N, C, dv)
    beta = beta.reshape(B, H, N, C, 1)
    g = jnp.cumsum(g.reshape(B, H, N, C), axis=-1)
    lower_incl = jnp.tril(jnp.ones((C, C), dtype=bool))
    strict = jnp.tril(jnp.ones((C, C), dtype=bool), -1)
    decay = jnp.exp(jnp.where(lower_incl, g[..., :, None] - g[..., None, :], -jnp.inf))
    kb = k * beta
    kk = jnp.einsum('bhncd,bhnmd->bhncm', kb, k) * decay
    tri = jnp.eye(C, dtype=q.dtype) + jnp.where(strict, kk, 0.0)
    rhs = jnp.concatenate([v * beta, kb * jnp.exp(g)[..., None]], axis=-1)
    sol = lax.linalg.triangular_solve(tri, rhs, left_side=True, lower=True)
    u, w = sol[..., :dv], sol[..., dv:]
    qk = jnp.einsum('bhncd,bhnmd->bhncm', q, k) * decay
    g_last = g[..., -1:]
    q_dec = q * jnp.exp(g)[..., None]
    k_dec = k * jnp.exp(g_last - g)[..., None]
    chunk_decay = jnp.exp(g_last[..., 0])

    def step(state, inp):
        qk_c, u_c, w_c, q_c, k_c, d_c = inp
        v_new = u_c - jnp.einsum('bhcd,bhde->bhce', w_c, state)
        out = jnp.einsum('bhcd,bhde->bhce', q_c, state) + jnp.einsum('bhcm,bhme->bhce', qk_c, v_new)
        state = state * d_c[..., None, None] + jnp.einsum('bhcd,bhce->bhde', k_c, v_new)
        return state, out

    xs = (jnp.moveaxis(qk, 2, 0), jnp.moveaxis(u, 2, 0), jnp.moveaxis(w, 2, 0),
          jnp.moveaxis(q_dec, 2, 0), jnp.moveaxis(k_dec, 2, 0), jnp.moveaxis(chunk_decay, 2, 0))
    state0 = jnp.zeros((B, H, dk, dv), q.dtype)
    _, out = lax.scan(step, state0, xs)
    return jnp.moveaxis(out, 0, 2).reshape(B, H, S, dv)


def gated_deltanet_bidir(p_qkv, p_z, p_b, p_a, conv_w, a_log, dt_bias, norm_g):
    B, S, _ = p_qkv.shape
    qkv = jax.nn.silu(depthwise_conv(p_qkv, conv_w))
    q, k, v = jnp.split(qkv, 3, axis=-1)

    def to_heads(t):
        return t.reshape(B, S, DN_HEADS, DN_HD).astype(jnp.float32).transpose(0, 2, 1, 3)

    q = l2_norm(to_heads(q))
    k = l2_norm(to_heads(k))
    v = to_heads(v)
    beta = jax.nn.sigmoid(p_b.astype(jnp.float32)).reshape(B, S, 2, DN_HEADS).transpose(2, 0, 3, 1)
    a_in = p_a.astype(jnp.float32).reshape(B, S, 2, DN_HEADS).transpose(2, 0, 3, 1)
    g = -jnp.exp(a_log.astype(jnp.float32))[:, None, :, None] * jax.nn.softplus(
        a_in + dt_bias.astype(jnp.float32)[:, None, :, None])
    o_fwd = chunk_gated_delta_rule(q, k, v, g[0], beta[0])
    o_bwd = jnp.flip(chunk_gated_delta_rule(jnp.flip(q, 2), jnp.flip(k, 2), jnp.flip(v, 2),
                                            jnp.flip(g[1], 2), jnp.flip(beta[1], 2)), 2)
    o = (o_fwd + o_bwd).transpose(0, 2, 1, 3)
    z = p_z.reshape(B, S, DN_HEADS, DN_HD).astype(jnp.float32)
    o = rms_norm(o, norm_g) * jax.nn.silu(z)
    return o.reshape(B, S, DN_HEADS * DN_HD).astype(p_qkv.dtype)


def hybrid_mixer(x, cos, sin, lambda_init, w_in, w_o, conv_dw, conv_dw_b, conv_ln_g, conv_ln_b, conv_pw,
                 diff_lambda, diff_subln_g, dn_conv, dn_a_log, dn_dt_bias, dn_norm_g):
    B, S, _ = x.shape
    proj = x @ w_in
    p_conv, p_q, p_k, p_v, p_dn, p_z, p_b, p_a = split_projection(proj)

    c_lin, c_gate = jnp.split(p_conv, 2, axis=-1)
    c = c_lin * jax.nn.sigmoid(c_gate)
    c = depthwise_conv(c, conv_dw) + conv_dw_b
    c = jax.nn.silu(layer_norm(c, conv_ln_g, conv_ln_b))
    y_conv = c @ conv_pw

    q = apply_rope(p_q.reshape(B, S, DIFF_HEADS, 2, DIFF_HD), cos, sin)
    k = apply_rope(p_k.reshape(B, S, DIFF_HEADS, 2, DIFF_HD), cos, sin)
    v = p_v.reshape(B, S, DIFF_HEADS, DIFF_VD)
    lf = diff_lambda.astype(jnp.float32)
    lam = jnp.exp(jnp.sum(lf[0] * lf[1])) - jnp.exp(jnp.sum(lf[2] * lf[3])) + lambda_init
    y_diff = differential_attention(q, k, v, lam, diff_subln_g, lambda_init).reshape(B, S, DIFF_HEADS * DIFF_VD)

    y_dn = gated_deltanet_bidir(p_dn, p_z, p_b, p_a, dn_conv, dn_a_log, dn_dt_bias, dn_norm_g)

    return jnp.concatenate([y_conv, y_diff, y_dn], axis=-1) @ w_o


def swiglu(x, w1, w3, w2):
    return (jax.nn.silu(x @ w1) * (x @ w3)) @ w2


def moe_swiglu(x, router_w, w1, w3, w2):
    B, S, D = x.shape
    t = x.reshape(B * S, D)
    logits = (t @ router_w).astype(jnp.float32)
    top_v, top_i = lax.top_k(logits, TOP_K)
    gates = jax.nn.softmax(top_v, axis=-1)
    combine = jnp.sum(jax.nn.one_hot(top_i, N_EXPERTS, dtype=jnp.float32) * gates[..., None], axis=1)
    out = jnp.zeros_like(t)
    for e in range(N_EXPERTS):
        out = out + combine[:, e:e + 1].astype(t.dtype) * swiglu(t, w1[e], w3[e], w2[e])
    return out.reshape(B, S, D)


def setup_inputs(seed: int = 0) -> dict:
    key = jax.random.key(seed)
    keys = jax.random.split(key, 32)
    counter = iter(range(32))

    def nk():
        return keys[next(counter)]

    def nrm(shape, scale):
        return jax.random.normal(nk(), shape, jnp.float32) * scale

    L = DEPTH
    dt = jnp.exp(jax.random.uniform(nk(), (L, 2, DN_HEADS), jnp.float32, math.log(1e-3), math.log(1e-1)))
    a_log = jnp.log(jax.random.uniform(nk(), (L, 2, DN_HEADS), jnp.float32, 1.0, 16.0))
    return {
        'x': nrm((BATCH, SEQ, D_MODEL), 1.0),
        'w_in': nrm((L, D_MODEL, PROJ_DIM), D_MODEL ** -0.5),
        'w_o': nrm((L, MIX_WIDTH, D_MODEL), MIX_WIDTH ** -0.5 * DEEPNORM_BETA),
        'ln1_g': 1.0 + nrm((L, D_MODEL), 0.02),
        'ln1_b': nrm((L, D_MODEL), 0.02),
        'ln2_g': 1.0 + nrm((L, D_MODEL), 0.02),
        'ln2_b': nrm((L, D_MODEL), 0.02),
        'conv_dw': nrm((L, CONV_WIDTH, CONV_W), CONV_WIDTH ** -0.5),
        'conv_dw_b': nrm((L, CONV_W), 0.02),
        'conv_ln_g': 1.0 + nrm((L, CONV_W), 0.02),
        'conv_ln_b': nrm((L, CONV_W), 0.02),
        'conv_pw': nrm((L, CONV_W, CONV_W), CONV_W ** -0.5),
        'diff_lambda': nrm((L, 4, DIFF_HD), 0.1),
        'diff_subln_g': 1.0 + nrm((L, DIFF_VD), 0.02),
        'dn_conv': nrm((L, DN_CONV, 3 * DN_HEADS * DN_HD), DN_CONV ** -0.5),
        'dn_a_log': a_log,
        'dn_dt_bias': dt + jnp.log(-jnp.expm1(-dt)),
        'dn_norm_g': 1.0 + nrm((L, DN_HD), 0.02),
        'ffn_w1': nrm((N_DENSE, D_MODEL, D_FF), D_MODEL ** -0.5),
        'ffn_w3': nrm((N_DENSE, D_MODEL, D_FF), D_MODEL ** -0.5),
        'ffn_w2': nrm((N_DENSE, D_FF, D_MODEL), D_FF ** -0.5 * DEEPNORM_BETA),
        'router_w': nrm((N_MOE, D_MODEL, N_EXPERTS), D_MODEL ** -0.5),
        'moe_w1': nrm((N_MOE, N_EXPERTS, D_MODEL, D_FF_EXPERT), D_MODEL ** -0.5),
        'moe_w3': nrm((N_MOE, N_EXPERTS, D_MODEL, D_FF_EXPERT), D_MODEL ** -0.5),
        'moe_w2': nrm((N_MOE, N_EXPERTS, D_FF_EXPERT, D_MODEL), D_FF_EXPERT ** -0.5 * DEEPNORM_BETA),
    }


def reference(x, w_in, w_o, ln1_g, ln1_b, ln2_g, ln2_b, conv_dw, conv_dw_b, conv_ln_g, conv_ln_b, conv_pw,
              diff_lambda, diff_subln_g, dn_conv, dn_a_log, dn_dt_bias, dn_norm_g,
              ffn_w1, ffn_w3, ffn_w2, router_w, moe_w1, moe_w3, moe_w2):
    S = x.shape[1]
    cos, sin = rope_tables(S, DIFF_HD)
    for layer in range(DEPTH):
        lambda_init = 0.8 - 0.6 * math.exp(-0.3 * layer)
        h = hybrid_mixer(x, cos, sin, lambda_init, w_in[layer], w_o[layer],
                         conv_dw[layer], conv_dw_b[layer], conv_ln_g[layer], conv_ln_b[layer], conv_pw[layer],
                         diff_lambda[layer], diff_subln_g[layer],
                         dn_conv[layer], dn_a_log[layer], dn_dt_bias[layer], dn_norm_g[layer])
        x = layer_norm(DEEPNORM_ALPHA * x + h, ln1_g[layer], ln1_b[layer])
        j = layer // 2
        if layer % 2 == 0:
            f = swiglu(x, ffn_w1[j], ffn_w3[j], ffn_w2[j])
        else:
            f = moe_swiglu(x, router_w[j], moe_w1[j], moe_w3[j], moe_w2[j])
        x = layer_norm(DEEPNORM_ALPHA * x + f, ln2_g[layer], ln2_b[layer])
    return x
```

```python
import contextlib
import math
import numpy as np
import concourse.bass as bass
import concourse.mybir as mybir
from concourse.bass_utils import run_bass_kernel_spmd

F32 = mybir.dt.float32
BF16 = mybir.dt.bfloat16
AF = mybir.ActivationFunctionType
ALU = mybir.AluOpType
AX = mybir.AxisListType

D = 1024
SEQ = 2048
DEPTH = 4
PROJ = 3088
DFF = 2816
DFFE = 3584
NEXP = 8
ALPHA = (2 * DEPTH) ** 0.25
EPS = 1e-5
C_CONV, C_Q, C_K, C_V, C_DN, C_Z, C_B, C_A = 0, 512, 1024, 1536, 2048, 2816, 3072, 3080


class Buf:
    __slots__ = ("w", "r")

    def __init__(self):
        self.w = None
        self.r = {}


class TT:
    __slots__ = ("t", "b")

    def __init__(self, t):
        self.t = t
        self.b = Buf()

    def __getitem__(self, k):
        return self.t[k]


class Sched:
    ENGS = ("pe", "act", "dve", "pool", "sp")
    NDS = 48

    def __init__(self, nc, es):
        self.nc = nc
        self.sems = {e: es.enter_context(nc.semaphore("pg_" + e)) for e in self.ENGS}
        self.cnt = {e: 0 for e in self.ENGS}
        self.seen = {e: {} for e in self.ENGS}
        self.ops = {e: [] for e in self.ENGS}
        self.dsems = [es.enter_context(nc.semaphore("dq%d" % i)) for i in range(self.NDS)]
        self.dval = [0] * self.NDS
        self.dnext = 0
        self.same_engine_sync = True

    def _collect(self, eng, reads, writes):
        deps = {}

        def add(tok):
            if tok is None:
                return
            k, v = tok
            if k == eng and (eng == "pe" or not self.same_engine_sync):
                return
            if deps.get(k, 0) < v:
                deps[k] = v

        for b in reads:
            add(b.w)
        for b in writes:
            add(b.w)
            for k, v in b.r.items():
                add((k, v))
        waits = []
        seen = self.seen[eng]
        for k, v in deps.items():
            if seen.get(k, 0) >= v:
                continue
            seen[k] = v
            waits.append((k, v))
        return waits

    def _semof(self, k):
        return self.sems[k] if isinstance(k, str) else self.dsems[k]

    def op(self, eng, fn, reads=(), writes=()):
        reads = [x.b if isinstance(x, TT) else x for x in reads]
        writes = [x.b if isinstance(x, TT) else x for x in writes]
        waits = self._collect(eng, reads, writes)
        self.cnt[eng] += 1
        tok = (eng, self.cnt[eng])
        self.ops[eng].append((waits, fn, (self.sems[eng], 1)))
        for b in writes:
            b.w = tok
            b.r = {}
        for b in reads:
            if b.r.get(eng, 0) < tok[1]:
                b.r[eng] = tok[1]
        return tok

    def dma(self, q, out, in_, reads=(), writes=(), slow=False):
        reads = [x.b if isinstance(x, TT) else x for x in reads]
        writes = [x.b if isinstance(x, TT) else x for x in writes]
        waits = self._collect(q, reads, writes)
        i = self.dnext
        self.dnext = (self.dnext + 1) % self.NDS
        seen = self.seen[q]
        if self.dval[i] > 0 and seen.get(i, 0) < self.dval[i]:
            seen[i] = self.dval[i]
            waits.append((i, self.dval[i]))
        self.dval[i] += 16
        tok = (i, self.dval[i])
        if slow:
            fn = lambda e, o=out, a=in_: e.dma_start(out=o, in_=a, allow_slow_non_contiguous=True)
        else:
            fn = lambda e, o=out, a=in_: e.dma_start(out=o, in_=a)
        self.ops[q].append((waits, fn, (self.dsems[i], 16)))
        for b in writes:
            b.w = tok
            b.r = {}
        for b in reads:
            if b.r.get(i, 0) < tok[1]:
                b.r[i] = tok[1]
        return tok

    def barrier(self):
        for e in self.ENGS:
            waits = []
            seen = self.seen[e]
            for k in self.ENGS:
                if k != e and self.cnt[k] > seen.get(k, 0):
                    seen[k] = self.cnt[k]
                    waits.append((k, self.cnt[k]))
            for i in range(self.NDS):
                if self.dval[i] > seen.get(i, 0):
                    seen[i] = self.dval[i]
                    waits.append((i, self.dval[i]))
            if waits:
                self.ops[e].append((waits, None, None))

    def finish(self, es):
        nc = self.nc
        block = es.enter_context(nc.Block())

        def run(ename):
            def body(e):
                for waits, fn, inc in self.ops[ename]:
                    for k, v in waits:
                        e.wait_ge(self._semof(k), v)
                    if fn is not None:
                        fn(e).then_inc(inc[0], inc[1])
            return body

        block.tensor(run("pe"))
        block.scalar(run("act"))
        block.vector(run("dve"))
        block.gpsimd(run("pool"))
        block.sync(run("sp"))


class Builder:
    def __init__(self, nc, nseq, n_layers):
        self.nc = nc
        self.nseq = nseq
        self.ntok = nseq * SEQ
        self.n_layers = n_layers
        self.uid = 0

    def tile(self, es, shape, dt=F32, name="t"):
        self.uid += 1
        return TT(es.enter_context(self.nc.sbuf_tensor("%s_%d" % (name, self.uid), list(shape), dt)))

    def ps(self):
        p = self.psum[self.psi]
        self.psi = (self.psi + 1) % len(self.psum)
        return p

    def MM(self, out, lhsT, rhs, start, stop, rd, wr):
        self.S.op("pe", lambda e: e.matmul(out, lhsT=lhsT, rhs=rhs, start=start, stop=stop), rd, wr)

    def TR(self, out, in_, ident, rd, wr):
        self.S.op("pe", lambda e: e.transpose(out, in_, ident), rd, wr)

    def ACT(self, out, in_, func, rd, wr, bias=None, scale=None, accum=None):
        kw = {}
        if bias is not None:
            kw["bias"] = bias
        if scale is not None:
            kw["scale"] = scale
        if accum is not None:
            kw["accum_out"] = accum
        self.S.op("act", lambda e: e.activation(out=out, in_=in_, func=func, **kw), rd, wr)

    def TT_(self, eng, out, in0, in1, op, rd, wr):
        self.S.op(eng, lambda e: e.tensor_tensor(out=out, in0=in0, in1=in1, op=op), rd, wr)

    def TS(self, eng, out, in0, s1, s2, op0, op1, rd, wr):
        if op1 is None:
            self.S.op(eng, lambda e: e.tensor_scalar(out=out, in0=in0, scalar1=s1, scalar2=None, op0=op0), rd, wr)
        else:
            self.S.op(eng, lambda e: e.tensor_scalar(out=out, in0=in0, scalar1=s1, scalar2=s2, op0=op0, op1=op1), rd, wr)

    def STT(self, out, in0, scalar, in1, op0, op1, rd, wr):
        self.S.op("dve", lambda e: e.scalar_tensor_tensor(out=out, in0=in0, scalar=scalar, in1=in1, op0=op0, op1=op1), rd, wr)

    def CP(self, eng, out, in_, rd, wr):
        if eng == "act":
            self.S.op("act", lambda e: e.activation(out=out, in_=in_, func=AF.Copy), rd, wr)
        else:
            self.S.op(eng, lambda e: e.tensor_copy(out=out, in_=in_), rd, wr)

    def RED(self, out, in_, op, rd, wr):
        self.S.op("dve", lambda e: e.tensor_reduce(out=out, in_=in_, axis=AX.X, op=op), rd, wr)

    def RECIP(self, out, in_, rd, wr):
        self.S.op("dve", lambda e: e.reciprocal(out=out, in_=in_), rd, wr)

    def MEMSET(self, eng, ap, val, wr):
        self.S.op(eng, lambda e: e.memset(ap, val), [], wr)

    def rstd(self, out, in_, rd, wr, eps_ap):
        self.ACT(out, in_, AF.Sqrt, rd, wr, bias=eps_ap, scale=1.0)
        self.RECIP(out, out, wr, wr)

    def dbg(self, name, src, ap, shape, dt=F32, psum=False):
        if not self.debug or name in self._dbg:
            return
        self._dbg.add(name)
        d = self.nc.dram_tensor("dbg_" + name, list(shape), dt, kind="ExternalOutput").ap()
        if psum:
            tmp = self.dbgtmp[len(self._dbg) % 4]
            self.CP("dve", tmp[0:shape[0], 0:shape[1]], ap, [src], [tmp])
            self.S.dma("sp", d, tmp[0:shape[0], 0:shape[1]], [tmp], [])
        else:
            self.S.dma("sp", d, ap, [src], [])

    def build(self):
        nc = self.nc
        NT = self.ntok
        L = self.n_layers
        dr = {}

        def din(name, shape, dt=F32):
            dr[name] = nc.dram_tensor(name, list(shape), dt, kind="ExternalInput").ap()
            return dr[name]

        x_in = din("x", [NT, D])
        w_in = din("w_in", [DEPTH, D, PROJ])
        w_o = din("w_o", [DEPTH, D, D])
        ln1_g = din("ln1_g", [DEPTH, D]); ln1_b = din("ln1_b", [DEPTH, D])
        ln2_g = din("ln2_g", [DEPTH, D]); ln2_b = din("ln2_b", [DEPTH, D])
        cp_in = din("conv_cp", [DEPTH, 128, 2, 34])
        conv_pw = din("conv_pw", [DEPTH, 256, 256])
        diff_lambda = din("diff_lambda", [DEPTH, 256])
        diff_subln_g = din("diff_subln_g", [DEPTH, 128])
        dcp_in = din("dn_cp", [DEPTH, 128, 6, 5])
        dn_a_log = din("dn_a_log", [DEPTH, 8])
        dn_dt_bias = din("dn_dt_bias", [DEPTH, 8])
        dn_norm_g = din("dn_norm_g", [DEPTH, 64])
        nd = (L + 1) // 2
        nm = L // 2
        ffn_w1 = din("ffn_w1", [nd, D, DFF]); ffn_w3 = din("ffn_w3", [nd, D, DFF]); ffn_w2 = din("ffn_w2", [nd, DFF, D])
        if nm > 0:
            router_w = din("router_w", [nm, 128, 8, NEXP])
            moe_w1 = din("moe_w1", [nm, NEXP, D, DFFE]); moe_w3 = din("moe_w3", [nm, NEXP, D, DFFE])
            moe_w2 = din("moe_w2", [nm, NEXP, DFFE, D])
        c_ident = din("c_ident", [128, 128])
        c_masks = din("c_masks", [6, 128, 128])
        c_rope = din("c_rope", [2, 128, SEQ])
        y_out = nc.dram_tensor("y", [NT, D], F32, kind="ExternalOutput").ap()
        import os as _os
        ik = "ExternalOutput" if _os.environ.get("KDBG") else "Internal"
        xres = nc.dram_tensor("xres", [NT, D], F32, kind=ik).ap()
        xT = nc.dram_tensor("xT", [128, 8, NT], BF16, kind=ik).ap()
        self.ycT = nc.dram_tensor("ycT", [128, 8, NT], BF16, kind=ik).ap()

        with contextlib.ExitStack() as es:
            S = self.S = Sched(nc, es)
            self.psum = [TT(es.enter_context(nc.psum_tensor("psb%d" % i, [128, 512], F32))) for i in range(8)]
            self.psi = 0
            ident = self.tile(es, [128, 128], F32, "ident")
            identb = self.tile(es, [128, 128], BF16, "identb")
            onesm = self.tile(es, [128, 128], F32, "onesm")
            ones256 = self.tile(es, [128, 128], F32, "ones256")
            epsc = self.tile(es, [128, 2], F32, "epsc")
            S.dma("sp", ident[:], c_ident, [], [ident])
            S.dma("pool", identb[:], c_ident, [], [identb])
            self.MEMSET("dve", onesm[:], 1.0, [onesm])
            self.MEMSET("dve", ones256[:], 1.0 / 256.0, [ones256])
            self.MEMSET("dve", epsc[:, 0:1], EPS, [epsc])
            self.MEMSET("dve", epsc[:, 1:2], 1e-6, [epsc])
            self.ident, self.identb, self.onesm, self.ones256, self.epsc = ident, identb, onesm, ones256, epsc
            self.comb = self.tile(es, [128, NT // 128, NEXP], F32, "comb")
            self._xtbufs = [self.tile(es, [128, 8, 128], BF16, "xtb") for _ in range(2)] + \
                           [self.tile(es, [128, 8, 128], F32, "xtf") for _ in range(2)] + \
                           [self.tile(es, [128, 16], F32, "rt") for _ in range(2)]
            self.dr = dr
            self.xres, self.xT, self.y_out = xres, xT, y_out
            self.debug = bool(_os.environ.get("KDBG"))
            self.kgate = _os.environ.get("KGATE", "")
            self.ks1 = _os.environ.get("KS1", "")
            self._dbg = set()
            if self.debug:
                self.dbgtmp = [self.tile(es, [128, 512], F32, "dbgtmp") for _ in range(4)]

            with contextlib.ExitStack() as es2:
                bufs = [self.tile(es2, [128, D], F32, "pxt") for _ in range(2)]
                for tt in range(NT // 128):
                    xt = bufs[tt % 2]
                    S.dma("sp", xt[:], x_in[tt * 128:(tt + 1) * 128, :], [], [xt])
                    self.emit_xT(es2, xt, tt, None, 0)
                S.barrier()

            import os as _os
            self.kstop = _os.environ.get("KSTOP", "")
            for l in range(L):
                last = (l == L - 1)
                if self.kstop == "pro":
                    break
                for s in range(self.nseq):
                    self.mixer(l, s)
                    if self.kstop:
                        break
                if self.kstop:
                    break
                if "noffn" in self.kgate and l % 2 == 1:
                    continue
                self.ffn(l, last)
                if "stop0" in self.kgate:
                    break
            S.barrier()
            S.finish(es)

    def emit_xT(self, es, xt, tt, router, l):
        S = self.S
        xtb = self._xtbufs[tt % 2]
        xtf = self._xtbufs[2 + tt % 2]
        rt = self._xtbufs[4 + tt % 2]
        for k in range(8):
            p = self.ps()
            self.TR(p[:, 0:128], xt[:, k * 128:(k + 1) * 128], self.ident[:], [xt, self.ident], [p])
            if router is not None:
                self.CP("act", xtf[:, k, :], p[:, 0:128], [p], [xtf])
                self.CP("pool", xtb[:, k, :], xtf[:, k, :], [xtf], [xtb])
            else:
                self.CP("act" if k % 2 else "dve", xtb[:, k, :], p[:, 0:128], [p], [xtb])
        S.dma("sp", self.xT[:, :, tt * 128:(tt + 1) * 128], xtb[:], [xtb], [])
        if router is not None and "norouter" not in self.kgate:
            wr = router
            p = self.ps()
            for k in range(8):
                self.MM(p[:, 0:8], xtf[:, k, :], wr[:, k, :], k == 0, k == 7, [xtf, wr], [p])
            lg = rt[:, 0:8]
            self.CP("dve", lg, p[:, 0:8], [p], [rt])
            m1 = rt[:, 8:9]; m2 = rt[:, 9:10]; tmp = rt[:, 10:11]
            cm = self.comb[:, tt, :]
            self.RED(m1, lg, ALU.max, [rt], [rt])
            self.TS("dve", cm, lg, m1, -1e30, ALU.is_equal, ALU.mult, [rt], [self.comb])
            self.TT_("dve", cm, cm, lg, ALU.add, [rt, self.comb], [self.comb])
            self.RED(m2, cm, ALU.max, [self.comb], [rt])
            self.TS("dve", cm, lg, m2, None, ALU.is_ge, None, [rt], [self.comb])
            self.TS("dve", tmp, m1, -1.0, None, ALU.mult, None, [rt], [rt])
            ex = rt[:, 11:12]
            self.ACT(ex, m2, AF.Exp, [rt], [rt], bias=tmp, scale=1.0)
            self.TS("dve", ex, ex, 1.0, None, ALU.add, None, [rt], [rt])
            self.RECIP(ex, ex, [rt], [rt])
            self.ACT(lg, lg, AF.Exp, [rt], [rt], bias=tmp, scale=1.0)
            self.STT(cm, lg, ex, cm, ALU.mult, ALU.mult, [rt, self.comb], [self.comb])

    def layer_norm_tile(self, t, out, gt, bt, st):
        self.RED(st[:, 0:1], t[:], ALU.add, [t], [st])
        self.TS("dve", st[:, 1:2], st[:, 0:1], -1.0 / D, None, ALU.mult, None, [st], [st])
        self.TS("dve", out[:], t[:], st[:, 1:2], None, ALU.add, None, [t, st], [out])
        self.ACT(t[:], out[:], AF.Square, [out], [t])
        self.RED(st[:, 2:3], t[:], ALU.add, [t], [st])
        self.ACT(st[:, 3:4], st[:, 2:3], AF.Sqrt, [st, self.epsc], [st], bias=self.epsc[:, 0:1], scale=1.0 / D)
        self.RECIP(st[:, 3:4], st[:, 3:4], [st], [st])
        self.STT(out[:], out[:], st[:, 3:4], gt[:], ALU.mult, ALU.mult, [out, st, gt], [out])
        self.TT_("pool", out[:], out[:], bt[:], ALU.add, [out, bt], [out])

    def mixer(self, l, s):
        S = self.S
        dr = self.dr
        t0 = s * SEQ
        lam_init = 0.8 - 0.6 * math.exp(-0.3 * l)
        w_in = dr["w_in"][l]
        self.t0 = t0
        with contextlib.ExitStack() as es:
            xTs = self.tile(es, [128, 8, SEQ], BF16, "xTs")
            for k in range(8):
                S.dma("sp", xTs[:, k, :], self.xT[:, k, t0:t0 + SEQ], [], [xTs])
            ks = self.kstop
            ks1 = self.ks1 if l >= 1 else ""
            if ks in ("", "conv", "out") and (not ks1 or "conv" in ks1):
                self.conv_group(l, xTs, None, w_in)
            S.barrier()
            if ks in ("", "attn", "out") and (not ks1 or "attn" in ks1):
                self.attn_group(l, xTs, None, w_in, lam_init)
            S.barrier()
            if ks in ("", "dn", "out") and (not ks1 or "dn" in ks1):
                self.dn_group(l, xTs, None, w_in)
            S.barrier()
        if self.kstop in ("conv", "attn", "dn"):
            return
        if ks1 and "out" not in ks1:
            return
        with contextlib.ExitStack() as es:
            ycat = self.tile(es, [128, 8, SEQ], BF16, "ycat")
            for k in range(8):
                S.dma("sp", ycat[:, k, :], self.ycT[:, k, t0:t0 + SEQ], [], [ycat])
            self.out_proj(l, s, ycat)
            S.barrier()

    def load_w(self, t, src, rd=()):
        self.S.dma("pool", t, src.rearrange("(k p) m -> p k m", p=128), list(rd), [])

    def conv_group(self, l, xTs, ycat, w_in):
        S = self.S
        dr = self.dr
        with contextlib.ExitStack() as es:
            wsec = self.tile(es, [128, 8, 512], BF16, "wconv")
            S.dma("pool", wsec[:], w_in[:, C_CONV:C_CONV + 512].rearrange("(k p) m -> p k m", p=128), [], [wsec])
            cp = self.tile(es, [128, 2, 34], F32, "cp")
            S.dma("sp", cp[:], dr["conv_cp"][l], [], [cp])
            wpw = self.tile(es, [128, 2, 256], BF16, "wpw")
            S.dma("pool", wpw[:], dr["conv_pw"][l].rearrange("(k p) m -> p k m", p=128), [], [wpw])
            cpad = [self.tile(es, [128, SEQ + 30], F32, "cpad") for _ in range(2)]
            acc = [self.tile(es, [128, SEQ], F32, "cacc") for _ in range(2)]
            accb = [self.tile(es, [128, SEQ], F32, "caccb") for _ in range(2)]
            sig = [self.tile(es, [128, 512], F32, "sig") for _ in range(2)]
            for j in range(2):
                self.MEMSET("pool", cpad[j][:, 0:15], 0.0, [cpad[j]])
                self.MEMSET("pool", cpad[j][:, SEQ + 15:SEQ + 30], 0.0, [cpad[j]])
            for j in range(2):
                for tb in range(4):
                    pl = self.ps(); pg = self.ps()
                    for k in range(8):
                        self.MM(pl[:, :], wsec[:, k, j * 128:(j + 1) * 128], xTs[:, k, tb * 512:(tb + 1) * 512], k == 0, k == 7, [wsec, xTs], [pl])
                    for k in range(8):
                        self.MM(pg[:, :], wsec[:, k, 256 + j * 128:256 + (j + 1) * 128], xTs[:, k, tb * 512:(tb + 1) * 512], k == 0, k == 7, [wsec, xTs], [pg])
                    sg = sig[tb % 2]
                    self.ACT(sg[:], pg[:, :], AF.Sigmoid, [pg], [sg])
                    self.TT_("dve", cpad[j][:, 15 + tb * 512:15 + (tb + 1) * 512], pl[:, :], sg[:], ALU.mult, [pl, sg], [cpad[j]])
            for j in range(2):
                a = acc[j]
                self.TS("dve", a[:], cpad[j][:, 0:SEQ], cp[:, j, 0:1], None, ALU.mult, None, [cpad[j], cp], [a])
                for w in range(1, 31):
                    self.STT(a[:], cpad[j][:, w:w + SEQ], cp[:, j, w:w + 1], a[:], ALU.mult, ALU.add, [cpad[j], cp, a], [a])
                self.TS("pool", accb[j][:], a[:], cp[:, j, 31:32], None, ALU.add, None, [a, cp], [accb[j]])
                self.ACT(a[:], accb[j][:], AF.Square, [accb[j]], [a])
            ysil = [[self.tile(es, [128, 512], BF16, "ysil") for _ in range(2)] for _ in range(2)]
            m2 = self.tile(es, [128, 512], F32, "m2")
            rs = self.tile(es, [128, 512], F32, "rs")
            tmp = [self.tile(es, [128, 512], F32, "ctmp") for _ in range(2)]
            yst = [self.tile(es, [128, 512], BF16, "cyst") for _ in range(4)]
            for tb in range(4):
                sl = slice(tb * 512, (tb + 1) * 512)
                pm = self.ps(); pq = self.ps()
                for j in range(2):
                    self.MM(pm[:, :], self.ones256[:], accb[j][:, sl], j == 0, j == 1, [self.ones256, accb[j]], [pm])
                for j in range(2):
                    self.MM(pq[:, :], self.ones256[:], acc[j][:, sl], j == 0, j == 1, [self.ones256, acc[j]], [pq])
                self.ACT(m2[:], pm[:, :], AF.Square, [pm], [m2])
                self.TT_("dve", rs[:], pq[:, :], m2[:], ALU.subtract, [pq, m2], [rs])
                self.rstd(rs[:], rs[:], [rs, self.epsc], [rs], self.epsc[:, 0:1])
                for j in range(2):
                    t = tmp[j]
                    self.TT_("dve", t[:], accb[j][:, sl], pm[:, :], ALU.subtract, [accb[j], pm], [t])
                    self.TT_("pool", t[:], t[:], rs[:], ALU.mult, [t, rs], [t])
                    self.ACT(ysil[tb % 2][j][:], t[:], AF.Silu, [t, cp], [ysil[tb % 2][j]], bias=cp[:, j, 33:34], scale=cp[:, j, 32:33])
                for co in range(2):
                    po = self.ps()
                    for j in range(2):
                        self.MM(po[:, :], wpw[:, j, co * 128:(co + 1) * 128], ysil[tb % 2][j][:], j == 0, j == 1, [wpw, ysil[tb % 2][j]], [po])
                    ys = yst[(tb * 2 + co) % 4]
                    self.CP("dve", ys[:], po[:, :], [po], [ys])
                    S.dma("sp", self.ycT[:, co, self.t0 + tb * 512:self.t0 + (tb + 1) * 512], ys[:], [ys], [])

    def attn_group(self, l, xTs, ycat, w_in, lam_init):
        S = self.S
        dr = self.dr
        with contextlib.ExitStack() as es:
            rope = self.tile(es, [128, 2, SEQ], F32, "rope")
            S.dma("sp", rope[:, 0, :], dr["c_rope"][0], [], [rope])
            S.dma("sp", rope[:, 1, :], dr["c_rope"][1], [], [rope])
            wv = self.tile(es, [128, 8, 512], BF16, "wv")
            S.dma("pool", wv[:], w_in[:, C_V:C_V + 512].rearrange("(k p) m -> p k m", p=128), [], [wv])
            lamt = self.tile(es, [128, 256], F32, "lamt")
            S.dma("sp", lamt[:], dr["diff_lambda"][l:l + 1, :].partition_broadcast(128), [], [lamt])
            subg = self.tile(es, [128, 128], F32, "subg")
            S.dma("sp", subg[:], dr["diff_subln_g"][l:l + 1, :].partition_broadcast(128), [], [subg])
            lsm = self.tile(es, [128, 8], F32, "lsm")
            prod = self.tile(es, [128, 2, 64], F32, "lprod")
            lv = lamt[:].rearrange("p (a d) -> p a d", a=4)
            self.TT_("dve", prod[:, 0, :], lv[:, 0, :], lv[:, 1, :], ALU.mult, [lamt], [prod])
            self.TT_("dve", prod[:, 1, :], lv[:, 2, :], lv[:, 3, :], ALU.mult, [lamt], [prod])
            self.RED(lsm[:, 0:2], prod[:], ALU.add, [prod], [lsm])
            self.ACT(lsm[:, 2:4], lsm[:, 0:2], AF.Exp, [lsm], [lsm])
            self.TT_("dve", lsm[:, 4:5], lsm[:, 3:4], lsm[:, 2:3], ALU.subtract, [lsm], [lsm])
            self.TS("dve", lsm[:, 4:5], lsm[:, 4:5], -lam_init, None, ALU.add, None, [lsm], [lsm])
            neglam = lsm[:, 4:5]
            self.TS("dve", subg[:], subg[:], 1.0 - lam_init, None, ALU.mult, None, [subg], [subg])
            vall = self.tile(es, [128, 16, 4, 130], BF16, "vall")
            self.MEMSET("pool", vall[:, :, :, 128:129], 1.0, [vall])
            for kt in range(16):
                p = self.ps()
                for k in range(8):
                    self.MM(p[:, :], xTs[:, k, kt * 128:(kt + 1) * 128], wv[:, k, :], k == 0, k == 7, [xTs, wv], [p])
                self.CP("dve" if kt % 2 else "act", vall[:, kt, :, 0:128], p[:, :].rearrange("p (h e) -> p h e", h=4), [p], [vall])
            wqk = [self.tile(es, [128, 4, 8, 128], BF16, "wqk") for _ in range(2)]
            qrot = [self.tile(es, [128, SEQ], BF16, "qrot") for _ in range(2)]
            krot = [self.tile(es, [128, SEQ], BF16, "krot") for _ in range(2)]
            rtmp = [self.tile(es, [128, 512], F32, "rtmp") for _ in range(4)]
            PT = [[self.tile(es, [128, 512], BF16, "PT") for _ in range(16)] for _ in range(2)]
            osb = [self.tile(es, [128, 128], F32, "osb") for _ in range(2)]
            ost = [self.tile(es, [128, 8], F32, "ost") for _ in range(2)]
            junk = self.tile(es, [128, 128], F32, "ojunk")
            ayst = [self.tile(es, [128, 512], BF16, "ayst") for _ in range(2)]
            for h in range(4):
                w = wqk[h % 2]
                S.dma("pool", w[:, 0, :, :], w_in[:, C_Q + h * 128:C_Q + (h + 1) * 128].rearrange("(k p) m -> p k m", p=128), [], [w])
                S.dma("pool", w[:, 1, :, :], w_in[:, C_K + h * 128:C_K + (h + 1) * 128].rearrange("(k p) m -> p k m", p=128), [], [w])
                for a in range(2):
                    src = w[:, a, :, :].rearrange("p k (c t j) -> p k c t j", c=2, t=2)
                    dst = w[:, 2 + a, :, :].rearrange("p k (c t j) -> p k c t j", c=2, t=2)
                    for c in range(2):
                        self.CP("pool", dst[:, :, c, 0, :], src[:, :, c, 1, :], [w], [w])
                        self.CP("pool", dst[:, :, c, 1, :], src[:, :, c, 0, :], [w], [w])
                qr = qrot[h % 2]; kr = krot[h % 2]
                ri = 0
                for a, dstt in ((0, qr), (1, kr)):
                    for tb in range(4):
                        sl = slice(tb * 512, (tb + 1) * 512)
                        p0 = self.ps(); p1 = self.ps()
                        for k in range(8):
                            self.MM(p0[:, :], w[:, a, k, :], xTs[:, k, sl], k == 0, k == 7, [w, xTs], [p0])
                        for k in range(8):
                            self.MM(p1[:, :], w[:, 2 + a, k, :], xTs[:, k, sl], k == 0, k == 7, [w, xTs], [p1])
                        ta = rtmp[ri % 4]; tb_ = rtmp[(ri + 1) % 4]; ri += 2
                        self.TT_("dve", ta[:], p0[:, :], rope[:, 0, sl], ALU.mult, [p0, rope], [ta])
                        self.TT_("dve", tb_[:], p1[:, :], rope[:, 1, sl], ALU.mult, [p1, rope], [tb_])
                        self.TT_("pool", dstt[:, sl], ta[:], tb_[:], ALU.add, [ta, tb_], [dstt])
                self.dbg("qr", qr, qr[:], [128, SEQ], BF16)
                self.dbg("kr", kr, kr[:], [128, SEQ], BF16)
                self.dbg("vall", vall, vall[:, 0, :, :], [128, 4, 130], BF16)
                for qb in range(4):
                    qs = slice(qb * 512, (qb + 1) * 512)
                    for kt in range(16):
                        for c in range(2):
                            p = self.ps()
                            self.MM(p[:, :], kr[c * 64:(c + 1) * 64, kt * 128:(kt + 1) * 128], qr[c * 64:(c + 1) * 64, qs], True, True, [kr, qr], [p])
                            self.ACT(PT[c][kt][:], p[:, :], AF.Exp, [p], [PT[c][kt]], scale=0.125)
                    for qt in range(4):
                        qi = qb * 4 + qt
                        po = self.ps()
                        for c in range(2):
                            for kt in range(16):
                                self.MM(po[:, c * 256:c * 256 + 129], PT[c][kt][:, qt * 128:(qt + 1) * 128], vall[:, kt, h, 0:129], kt == 0, kt == 15, [PT[c][kt], vall], [po])
                        st = ost[qi % 2]; ob = osb[qi % 2]
                        self.dbg("pt", PT[0][0], PT[0][0][:], [128, 512], BF16)
                        self.dbg("po", po, po[:, :], [128, 512], F32, psum=True)
                        self.CP("dve", st[:, 0:1], po[:, 128:129], [po], [st])
                        self.CP("dve", st[:, 1:2], po[:, 256 + 128:256 + 129], [po], [st])
                        self.RECIP(st[:, 0:2], st[:, 0:2], [st], [st])
                        self.TT_("dve", st[:, 1:2], st[:, 1:2], neglam, ALU.mult, [st, lsm], [st])
                        self.TS("dve", ob[:], po[:, 0:128], st[:, 0:1], None, ALU.mult, None, [po, st], [ob])
                        self.STT(ob[:], po[:, 256:256 + 128], st[:, 1:2], ob[:], ALU.mult, ALU.add, [po, st, ob], [ob])
                        self.dbg("ob0", ob, ob[:], [128, 128], F32)
                        self.ACT(junk[:], ob[:], AF.Square, [ob], [junk])
                        self.RED(st[:, 2:3], junk[:], ALU.add, [junk], [st])
                        self.ACT(st[:, 3:4], st[:, 2:3], AF.Sqrt, [st, self.epsc], [st], bias=self.epsc[:, 0:1], scale=1.0 / 128.0)
                        self.RECIP(st[:, 3:4], st[:, 3:4], [st], [st])
                        self.STT(ob[:], ob[:], st[:, 3:4], subg[:], ALU.mult, ALU.mult, [ob, st, subg], [ob])
                        self.dbg("ob1", ob, ob[:], [128, 128], F32)
                        self.dbg("st", st, st[:], [128, 8], F32)
                        self.dbg("subg", subg, subg[:], [128, 128], F32)
                        self.dbg("lsm", lsm, lsm[:], [128, 8], F32)
                        pt = self.ps()
                        self.TR(pt[:, 0:128], ob[:], self.ident[:], [ob, self.ident], [pt])
                        ys = ayst[qb % 2]
                        self.CP("act", ys[:, qt * 128:(qt + 1) * 128], pt[:, 0:128], [pt], [ys])
                        if qt == 3:
                            S.dma("sp", self.ycT[:, 2 + h, self.t0 + qb * 512:self.t0 + (qb + 1) * 512], ys[:], [ys], [])

    def dn_group(self, l, xTs, ycat, w_in):
        S = self.S
        dr = self.dr
        NCH = 16
        with contextlib.ExitStack() as es:
            ident, identb = self.ident, self.identb
            masks = self.tile(es, [128, 6, 128], F32, "masks")
            for i in range(6):
                S.dma("sp", masks[:, i, :], dr["c_masks"][i], [], [masks])
            qkv = self.tile(es, [128, NCH, 768], BF16, "qkvtm")
            zba = self.tile(es, [128, NCH, 272], F32, "zba")
            qkfm = self.tile(es, [128, 4, NCH, 128], BF16, "qkfm")
            gb = self.tile(es, [128, NCH, 16], F32, "gb")
            with contextlib.ExitStack() as es1:
                wdn = self.tile(es1, [128, 8, 768], BF16, "wdn")
                S.dma("pool", wdn[:], w_in[:, C_DN:C_DN + 768].rearrange("(k p) m -> p k m", p=128), [], [wdn])
                wz = self.tile(es1, [128, 8, 272], BF16, "wz")
                S.dma("pool", wz[:], w_in[:, C_Z:C_Z + 272].rearrange("(k p) m -> p k m", p=128), [], [wz])
                dcp = self.tile(es1, [128, 6, 5], F32, "dcp")
                S.dma("sp", dcp[:], dr["dn_cp"][l], [], [dcp])
                rows = self.tile(es1, [128, 16], F32, "dnrows")
                S.dma("sp", rows[:, 0:8], dr["dn_a_log"][l:l + 1, :].partition_broadcast(128), [], [rows])
                S.dma("sp", rows[:, 8:16], dr["dn_dt_bias"][l:l + 1, :].partition_broadcast(128), [], [rows])
                dpad = [self.tile(es1, [128, SEQ + 4], F32, "dpad")]
                dacc = [self.tile(es1, [128, SEQ], F32, "dacc")]
                qf32 = self.tile(es1, [128, NCH, 512], F32, "qf32")
                for j in range(1):
                    self.MEMSET("pool", dpad[j][:, 0:2], 0.0, [dpad[j]])
                    self.MEMSET("pool", dpad[j][:, SEQ + 2:SEQ + 4], 0.0, [dpad[j]])
                for ch in range(6):
                    dp = dpad[0]; da = dacc[0]
                    for tb in range(4):
                        p = self.ps()
                        for k in range(8):
                            self.MM(p[:, :], wdn[:, k, ch * 128:(ch + 1) * 128], xTs[:, k, tb * 512:(tb + 1) * 512], k == 0, k == 7, [wdn, xTs], [p])
                        self.CP("act", dp[:, 2 + tb * 512:2 + (tb + 1) * 512], p[:, :], [p], [dp])
                    self.TS("dve", da[:], dp[:, 0:SEQ], dcp[:, ch, 0:1], None, ALU.mult, None, [dp, dcp], [da])
                    for w in range(1, 5):
                        self.STT(da[:], dp[:, w:w + SEQ], dcp[:, ch, w:w + 1], da[:], ALU.mult, ALU.add, [dp, dcp, da], [da])
                    self.ACT(da[:], da[:], AF.Silu, [da], [da])
                    for tt in range(NCH):
                        p = self.ps()
                        self.TR(p[:, 0:128], da[:, tt * 128:(tt + 1) * 128], ident[:], [da, ident], [p])
                        if ch < 4:
                            self.CP("dve" if tt % 2 else "act", qf32[:, tt, ch * 128:(ch + 1) * 128], p[:, 0:128], [p], [qf32])
                        else:
                            self.CP("dve" if tt % 2 else "act", qkv[:, tt, ch * 128:(ch + 1) * 128], p[:, 0:128], [p], [qkv])
                for tt in range(NCH):
                    p = self.ps()
                    for k in range(8):
                        self.MM(p[:, 0:272], xTs[:, k, tt * 128:(tt + 1) * 128], wz[:, k, :], k == 0, k == 7, [xTs, wz], [p])
                    self.CP("dve" if tt % 2 else "act", zba[:, tt, :], p[:, 0:272], [p], [zba])
                sqt = [self.tile(es1, [128, 512], F32, "dsqt") for _ in range(2)]
                ss = self.tile(es1, [128, NCH, 8], F32, "dss")
                for tt in range(NCH):
                    self.ACT(sqt[tt % 2][:], qf32[:, tt, :], AF.Square, [qf32], [sqt[tt % 2]])
                    self.RED(ss[:, tt, :], sqt[tt % 2][:].rearrange("p (h d) -> p h d", h=8), ALU.add, [sqt[tt % 2]], [ss])
                self.rstd(ss[:], ss[:], [ss, self.epsc], [ss], self.epsc[:, 1:2])
                self.TS("dve", ss[:, :, 0:4], ss[:, :, 0:4], 0.125, None, ALU.mult, None, [ss], [ss])
                for tt in range(NCH):
                    self.TT_("dve", qf32[:, tt, :].rearrange("p (h d) -> p h d", h=8), qf32[:, tt, :].rearrange("p (h d) -> p h d", h=8),
                             ss[:, tt, :].unsqueeze(2).to_broadcast([128, 8, 64]), ALU.mult, [qf32, ss], [qf32])
                self.CP("pool", qkv[:, :, 0:512], qf32[:], [qf32], [qkv])
                for tt in range(NCH):
                    for i in range(4):
                        p = self.ps()
                        self.TR(p[:, 0:128], qf32[:, tt, i * 128:(i + 1) * 128], ident[:], [qf32, ident], [p])
                        self.CP("dve" if i % 2 else "act", qkfm[:, i, tt, :], p[:, 0:128], [p], [qkfm])
                self.ACT(gb[:, :, 0:8], zba[:, :, 256:264], AF.Sigmoid, [zba], [gb])
                self.TT_("dve", gb[:, :, 8:16], zba[:, :, 264:272], rows[:, 8:16].unsqueeze(1).to_broadcast([128, NCH, 8]), ALU.add, [zba, rows], [gb])
                self.ACT(gb[:, :, 8:16], gb[:, :, 8:16], AF.Exp, [gb], [gb])
                self.ACT(gb[:, :, 8:16], gb[:, :, 8:16], AF.Ln, [gb], [gb], bias=1.0)
                self.ACT(rows[:, 0:8], rows[:, 0:8], AF.Exp, [rows], [rows])
                self.STT(gb[:, :, 8:16], gb[:, :, 8:16], -1.0, rows[:, 0:8].unsqueeze(1).to_broadcast([128, NCH, 8]), ALU.mult, ALU.mult, [gb, rows], [gb])
                S.barrier()
            import os as _os
            kdn = _os.environ.get("KDN", "")
            if kdn == "A":
                return
            ng = self.tile(es, [128, 64], F32, "dng")
            S.dma("sp", ng[:], dr["dn_norm_g"][l:l + 1, :].partition_broadcast(128), [], [ng])
            sz = zba
            self.ACT(zba[:, :, 0:256], zba[:, :, 0:256], AF.Silu, [zba], [zba])
            ydst = self.tile(es, [128, 2, SEQ], BF16, "ydst")
            MT = [self.tile(es, [64, NCH, 64], BF16, "MT") for _ in range(2)]
            Bm = [self.tile(es, [64, NCH, 64], F32, "Bm") for _ in range(2)]
            PTs = [self.tile(es, [64, NCH, 128], BF16, "PTs") for _ in range(2)]
            QKm = [self.tile(es, [128, NCH, 128], BF16, "QKm") for _ in range(2)]
            Ua = [self.tile(es, [128, NCH, 64], BF16, "Ua") for _ in range(2)]
            Sa = [self.tile(es, [64, NCH, 64], BF16, "Sa") for _ in range(2)]
            NR = 2

            def wt(shape, dt, name):
                return [self.tile(es, shape, dt, name) for _ in range(NR)]
            gL = wt([128, 128], F32, "gL"); sc = wt([128, 8], F32, "dsc"); E = wt([128, 128], F32, "dE")
            Gi = wt([128, 128], F32, "Gi"); Gs = wt([128, 128], F32, "Gs")
            X32 = wt([128, 128], F32, "X32")
            Pk = [wt([128, 128], F32, "Pk%d" % i) for i in range(7)]
            PkT = [wt([128, 128], F32, "PkT%d" % i) for i in range(6)]
            Z = [wt([128, 128], F32, "Z%d" % i) for i in range(2)]
            UW = wt([128, 128], BF16, "UW"); nW = wt([128, 64], BF16, "nW")
            Kd = wt([128, 64], BF16, "Kd"); Qd = wt([128, 64], BF16, "Qd")
            osum = self.tile(es, [128, 8], F32, "dosum")
            ojunk = self.tile(es, [128, 256], F32, "dojunk")
            it = 0
            for h in range(4):
                hb = (h % 2) * 64
                qi_ = h // 2
                for d in range(2):
                    Ld = masks[:, d, :]
                    Mi = masks[:, 2 + 2 * d, :]
                    Ms = masks[:, 3 + 2 * d, :]
                    lastc = 127 if d == 0 else 0
                    for n in range(NCH):
                        r = it % NR
                        it += 1
                        gcol = gb[:, n, 8 + d * 4 + h:8 + d * 4 + h + 1]
                        bcol = gb[:, n, d * 4 + h:d * 4 + h + 1]
                        kfm = qkfm[hb:hb + 64, 2 + qi_, n, :]
                        qfm = qkfm[hb:hb + 64, qi_, n, :]
                        q_tm = qkv[:, n, h * 64:(h + 1) * 64]
                        k_tm = qkv[:, n, 256 + h * 64:256 + (h + 1) * 64]
                        v_tm = qkv[:, n, 512 + h * 64:512 + (h + 1) * 64]
                        s_ = sc[r]
                        self.TS("pool", gL[r][:], Ld, gcol, None, ALU.mult, None, [masks, gb], [gL[r]])
                        pg = self.ps()
                        self.MM(pg[:, 0:128], self.onesm[:], gL[r][:], True, True, [self.onesm, gL[r]], [pg])
                        self.MM(pg[:, 128:129], Ld, gcol, True, True, [masks, gb], [pg])
                        self.TS("dve", s_[:, 0:1], pg[:, 128:129], -1.0, None, ALU.mult, None, [pg], [s_])
                        self.CP("dve", s_[:, 4:5], pg[:, lastc:lastc + 1], [pg], [s_])
                        self.TS("dve", E[r][:], pg[:, 0:128], s_[:, 0:1], 0.0, ALU.add, ALU.min, [pg, s_], [E[r]])
                        self.ACT(E[r][:], E[r][:], AF.Exp, [E[r]], [E[r]])
                        self.TT_("pool", Gi[r][:], E[r][:], Mi, ALU.mult, [E[r], masks], [Gi[r]])
                        self.TT_("pool", Gs[r][:], E[r][:], Ms, ALU.mult, [E[r], masks], [Gs[r]])
                        self.ACT(s_[:, 1:2], s_[:, 0:1], AF.Exp, [s_], [s_], scale=-1.0)
                        self.ACT(s_[:, 2:3], s_[:, 4:5], AF.Exp, [s_], [s_], bias=s_[:, 0:1], scale=1.0)
                        self.ACT(s_[:, 3:4], s_[:, 4:5], AF.Exp, [s_], [s_])
                        if kdn == "B1":
                            return
                        pk = self.ps()
                        self.MM(pk[:, 0:128], kfm, kfm, True, True, [qkfm], [pk])
                        self.MM(pk[:, 128:256], kfm, qfm, True, True, [qkfm], [pk])
                        self.STT(X32[r][:], pk[:, 0:128], bcol, Gs[r][:], ALU.mult, ALU.mult, [pk, gb, Gs[r]], [X32[r]])
                        self.TT_("dve", QKm[d][:, n, :], pk[:, 128:256], Gi[r][:], ALU.mult, [pk, Gi[r]], [QKm[d]])
                        self.CP("pool", Pk[0][r][:], X32[r][:], [X32[r]], [Pk[0][r]])
                        px = self.ps()
                        self.TR(px[:, 0:128], X32[r][:], ident[:], [X32[r], ident], [px])
                        self.CP("act", PkT[0][r][:], px[:, 0:128], [px], [PkT[0][r]])
                        self.CP("pool", Z[0][r][:, 0:64], v_tm, [qkv], [Z[0][r]])
                        self.TS("pool", Z[0][r][:, 64:128], k_tm, s_[:, 1:2], None, ALU.mult, None, [qkv, s_], [Z[0][r]])
                        self.TS("pool", Kd[r][:], k_tm, s_[:, 2:3], None, ALU.mult, None, [qkv, s_], [Kd[r]])
                        self.TS("pool", Qd[r][:], q_tm, s_[:, 1:2], None, ALU.mult, None, [qkv, s_], [Qd[r]])
                        if kdn == "B3":
                            return
                        zc = 0
                        for k in range(7):
                            pz = self.ps()
                            self.MM(pz[:, 0:128], Pk[k][r][:], Z[zc][r][:], True, True, [Pk[k][r], Z[zc][r]], [pz])
                            self.TT_("dve", Z[1 - zc][r][:], Z[zc][r][:], pz[:, 0:128], ALU.subtract if k == 0 else ALU.add, [Z[zc][r], pz], [Z[1 - zc][r]])
                            zc = 1 - zc
                            if k < 6:
                                pp = self.ps()
                                self.MM(pp[:, 0:128], PkT[k][r][:], Pk[k][r][:], True, True, [PkT[k][r], Pk[k][r]], [pp])
                                self.CP("act", Pk[k + 1][r][:], pp[:, 0:128], [pp], [Pk[k + 1][r]])
                                if k < 5:
                                    self.MM(pp[:, 128:256], Pk[k][r][:], PkT[k][r][:], True, True, [PkT[k][r], Pk[k][r]], [pp])
                                    self.CP("act", PkT[k + 1][r][:], pp[:, 128:256], [pp], [PkT[k + 1][r]])
                        Zf = Z[zc][r]
                        if kdn == "B5":
                            return
                        self.TS("dve", UW[r][:], Zf[:], bcol, None, ALU.mult, None, [Zf, gb], [UW[r]])
                        self.TS("pool", nW[r][:], UW[r][:, 64:128], -1.0, None, ALU.mult, None, [UW[r]], [nW[r]])
                        self.CP("pool", Ua[d][:, n, :], UW[r][:, 0:64], [UW[r]], [Ua[d]])
                        if kdn == "B6":
                            return
                        pm = self.ps()
                        self.MM(pm[0:64, 0:64], UW[r][:, 64:128], Kd[r][:], True, True, [UW[r], Kd[r]], [pm])
                        self.MM(pm[0:64, 64:128], Kd[r][:], UW[r][:, 0:64], True, True, [UW[r], Kd[r]], [pm])
                        if kdn == "B7":
                            return
                        self.STT(MT[d][:, n, :], ident[0:64, 0:64], s_[0:64, 3:4], pm[0:64, 0:64], ALU.mult, ALU.subtract, [ident, s_, pm], [MT[d]])
                        if kdn == "B8":
                            return
                        self.CP("dve", Bm[d][:, n, :], pm[0:64, 64:128], [pm], [Bm[d]])
                        if kdn == "B0":
                            return
                        pq = self.ps()
                        self.MM(pq[0:64, 0:128], Qd[r][:], identb[:], True, False, [Qd[r], identb], [pq])
                        self.MM(pq[0:64, 0:128], nW[r][:], QKm[d][:, n, :], False, True, [nW[r], QKm[d]], [pq])
                        self.CP("dve", PTs[d][:, n, :], pq[0:64, 0:128], [pq], [PTs[d]])
                    order = list(range(NCH)) if d == 0 else list(range(NCH - 1, -1, -1))
                    self.MEMSET("pool", Sa[d][:, order[0], :], 0.0, [Sa[d]])
                    for i in range(NCH - 1):
                        n = order[i]; nn = order[i + 1]
                        pss = self.ps()
                        self.MM(pss[0:64, 0:64], MT[d][:, n, :], Sa[d][:, n, :], True, True, [MT[d], Sa[d]], [pss])
                        self.TT_("dve", Sa[d][:, nn, :], pss[0:64, 0:64], Bm[d][:, n, :], ALU.add, [pss, Bm[d]], [Sa[d]])
                for n in range(NCH):
                    if h == 0:
                        pass
                    po = self.ps()
                    for d in range(2):
                        self.MM(po[:, 0:64], PTs[d][:, n, :], Sa[d][:, n, :], d == 0, False, [PTs[d], Sa[d]], [po])
                        self.MM(po[:, 0:64], QKm[d][:, n, :], Ua[d][:, n, :], False, d == 1, [QKm[d], Ua[d]], [po])
                    yv = sz[:, n, h * 64:(h + 1) * 64]
                    oc = ojunk[:, 0:64]
                    self.CP("dve", oc, po[:, 0:64], [po], [ojunk])
                    self.ACT(ojunk[:, 64:128], oc, AF.Square, [ojunk], [ojunk])
                    self.RED(osum[:, 0:1], ojunk[:, 64:128], ALU.add, [ojunk], [osum])
                    self.ACT(osum[:, 1:2], osum[:, 0:1], AF.Sqrt, [osum, self.epsc], [osum], bias=self.epsc[:, 0:1], scale=1.0 / 64.0)
                    self.RECIP(osum[:, 1:2], osum[:, 1:2], [osum], [osum])
                    self.STT(oc, oc, osum[:, 1:2], ng[:], ALU.mult, ALU.mult, [ojunk, osum, ng], [ojunk])
                    self.TT_("dve", yv, yv, oc, ALU.mult, [sz, ojunk], [sz])
            for n in range(NCH):
                for c in range(2):
                    p = self.ps()
                    self.TR(p[:, 0:128], sz[:, n, c * 128:(c + 1) * 128], ident[:], [sz, ident], [p])
                    self.CP("act" if c else "dve", ydst[:, c, n * 128:(n + 1) * 128], p[:, 0:128], [p], [ydst])
            for c in range(2):
                S.dma("sp", self.ycT[:, 6 + c, self.t0:self.t0 + SEQ], ydst[:, c, :], [ydst], [])

    def out_proj(self, l, s, ycat):
        S = self.S
        dr = self.dr
        moe = (l % 2 == 1)
        with contextlib.ExitStack() as es:
            wo = self.tile(es, [128, 8, D], BF16, "wo")
            S.dma("pool", wo[:], dr["w_o"][l].rearrange("(k p) m -> p k m", p=128), [], [wo])
            gt = self.tile(es, [128, D], F32, "l1g"); bt = self.tile(es, [128, D], F32, "l1b")
            S.dma("sp", gt[:], dr["ln1_g"][l:l + 1, :].partition_broadcast(128), [], [gt])
            S.dma("sp", bt[:], dr["ln1_b"][l:l + 1, :].partition_broadcast(128), [], [bt])
            router = None
            if moe and "nortile" not in self.kgate:
                router = self.tile(es, [128, 8, NEXP], F32, "wr")
                if "nordma" in self.kgate:
                    self.MEMSET("pool", router[:], 0.0, [router])
                else:
                    S.dma("sp", router[:], dr["router_w"][l // 2], [], [router])
            xin = [self.tile(es, [128, D], F32, "xin") for _ in range(2)]
            tt_ = [self.tile(es, [128, D], F32, "tsum") for _ in range(2)]
            xo = [self.tile(es, [128, D], F32, "xo") for _ in range(2)]
            st = [self.tile(es, [128, 8], F32, "lnst") for _ in range(2)]
            src = dr["x"] if l == 0 else self.xres
            for i in range(SEQ // 128):
                tg = s * (SEQ // 128) + i
                xi = xin[i % 2]; t = tt_[i % 2]; o = xo[i % 2]
                S.dma("sp", xi[:], src[tg * 128:(tg + 1) * 128, :], [], [xi])
                for half in range(2):
                    p = self.ps()
                    for k in range(8):
                        self.MM(p[:, :], ycat[:, k, i * 128:(i + 1) * 128], wo[:, k, half * 512:(half + 1) * 512], k == 0, k == 7, [ycat, wo], [p])
                    self.STT(t[:, half * 512:(half + 1) * 512], xi[:, half * 512:(half + 1) * 512], ALPHA, p[:, :], ALU.mult, ALU.add, [xi, p], [t])
                self.layer_norm_tile(t, o, gt, bt, st[i % 2])
                S.dma("sp", self.xres[tg * 128:(tg + 1) * 128, :], o[:], [o], [])
                self.emit_xT(es, o, tg, router, l)

    def ffn(self, l, last):
        S = self.S
        dr = self.dr
        moe = (l % 2 == 1)
        j = l // 2
        NT = self.ntok
        TB = 1024
        G = 4
        nff = (DFFE if moe else DFF) // 128
        groups = [(c0, min(G, nff - c0)) for c0 in range(0, nff, G)]
        nexp = NEXP if moe else 1
        with contextlib.ExitStack() as es:
            gt = self.tile(es, [128, D], F32, "l2g"); bt = self.tile(es, [128, D], F32, "l2b")
            S.dma("sp", gt[:], dr["ln2_g"][l:l + 1, :].partition_broadcast(128), [], [gt])
            S.dma("sp", bt[:], dr["ln2_b"][l:l + 1, :].partition_broadcast(128), [], [bt])
            xTb = self.tile(es, [128, 8, TB], BF16, "xTb")
            hT = [self.tile(es, [128, G, TB], BF16, "hT") for _ in range(2)]
            w1g = [self.tile(es, [128, 8, G * 128], BF16, "w1g") for _ in range(2)]
            w3g = [self.tile(es, [128, 8, G * 128], BF16, "w3g") for _ in range(2)]
            w2g = [self.tile(es, [128, G, D], BF16, "w2g") for _ in range(2)]
            acc = self.tile(es, [128, TB // 128, D], F32, "facc")
            sil = [self.tile(es, [128, 512], F32, "sil") for _ in range(3)]
            xin = [self.tile(es, [128, D], F32, "fxin") for _ in range(2)]
            xo = [self.tile(es, [128, D], F32, "fxo") for _ in range(2)]
            st = [self.tile(es, [128, 8], F32, "flnst") for _ in range(2)]
            gi = 0
            si = 0
            for tb in range(NT // TB):
                t0 = tb * TB
                for k in range(8):
                    S.dma("sp", xTb[:, k, :], self.xT[:, k, t0:t0 + TB], [], [xTb])
                first = True
                for e in range(nexp):
                    if moe:
                        W1 = dr["moe_w1"][j, e]; W3 = dr["moe_w3"][j, e]; W2 = dr["moe_w2"][j, e]
                    else:
                        W1 = dr["ffn_w1"][j]; W3 = dr["ffn_w3"][j]; W2 = dr["ffn_w2"][j]
                    for (c0, gn) in groups:
                        r = gi % 2
                        gi += 1
                        S.dma("pool", w1g[r][:, :, 0:gn * 128], W1[:, c0 * 128:(c0 + gn) * 128].rearrange("(k p) m -> p k m", p=128), [], [w1g[r]])
                        S.dma("pool", w3g[r][:, :, 0:gn * 128], W3[:, c0 * 128:(c0 + gn) * 128].rearrange("(k p) m -> p k m", p=128), [], [w3g[r]])
                        S.dma("pool", w2g[r][:, 0:gn, :], W2[c0 * 128:(c0 + gn) * 128, :].rearrange("(c p) m -> p c m", p=128), [], [w2g[r]])
                        for c in range(gn):
                            for hb in range(TB // 512):
                                ts_ = slice(hb * 512, (hb + 1) * 512)
                                pa = self.ps(); pb = self.ps()
                                for k in range(8):
                                    self.MM(pa[:, :], w1g[r][:, k, c * 128:(c + 1) * 128], xTb[:, k, ts_], k == 0, k == 7, [w1g[r], xTb], [pa])
                                for k in range(8):
                                    self.MM(pb[:, :], w3g[r][:, k, c * 128:(c + 1) * 128], xTb[:, k, ts_], k == 0, k == 7, [w3g[r], xTb], [pb])
                                sl_ = sil[si % 3]
                                si += 1
                                self.ACT(sl_[:], pa[:, :], AF.Silu, [pa], [sl_])
                                self.TT_("dve", hT[r][:, c, ts_], pb[:, :], sl_[:], ALU.mult, [pb, sl_], [hT[r]])
                        for tt in range(TB // 128):
                            tg = tb * (TB // 128) + tt
                            for half in range(2):
                                p = self.ps()
                                for c in range(gn):
                                    self.MM(p[:, :], hT[r][:, c, tt * 128:(tt + 1) * 128], w2g[r][:, c, half * 512:(half + 1) * 512], c == 0, c == gn - 1, [hT[r], w2g[r]], [p])
                                a_ = acc[:, tt, half * 512:(half + 1) * 512]
                                if moe:
                                    cw = self.comb[:, tg, e:e + 1]
                                    if first:
                                        self.TS("dve", a_, p[:, :], cw, None, ALU.mult, None, [p, self.comb], [acc])
                                    else:
                                        self.STT(a_, p[:, :], cw, a_, ALU.mult, ALU.add, [p, self.comb, acc], [acc])
                                else:
                                    if first:
                                        self.CP("dve", a_, p[:, :], [p], [acc])
                                    else:
                                        self.TT_("dve", a_, p[:, :], a_, ALU.add, [p, acc], [acc])
                        first = False
                for tt in range(TB // 128):
                    tg = tb * (TB // 128) + tt
                    xi = xin[tt % 2]; o = xo[tt % 2]
                    S.dma("sp", xi[:], self.xres[tg * 128:(tg + 1) * 128, :], [], [xi])
                    self.STT(xi[:], xi[:], ALPHA, acc[:, tt, :], ALU.mult, ALU.add, [xi, acc], [xi])
                    self.layer_norm_tile(xi, o, gt, bt, st[tt % 2])
                    if last:
                        S.dma("sp", self.y_out[tg * 128:(tg + 1) * 128, :], o[:], [o], [])
                    else:
                        S.dma("sp", self.xres[tg * 128:(tg + 1) * 128, :], o[:], [o], [])
                        self.emit_xT(es, o, tg, None, l)
            S.barrier()


def _consts():
    ident = np.eye(128, dtype=np.float32)
    p = np.arange(128)[:, None]
    f = np.arange(128)[None, :]
    Lf = (p <= f).astype(np.float32)
    Lb = (p >= f).astype(np.float32)
    masks = np.stack([Lf, Lb, (p <= f), (p < f), (p >= f), (p > f)]).astype(np.float32)
    inv = (10000.0 ** (-np.arange(0, 64, 2, dtype=np.float32) / 64.0)).astype(np.float32)
    ang = np.arange(SEQ, dtype=np.float32)[:, None] * inv[None, :]
    cos = np.cos(ang).astype(np.float32).T
    sin = np.sin(ang).astype(np.float32).T
    cosT = np.concatenate([cos, cos, cos, cos], 0)
    sinT = np.concatenate([-sin, sin, -sin, sin], 0)
    rope = np.stack([cosT, sinT]).astype(np.float32)
    return ident, masks, rope


_CACHE = {}
_LAST = None


def run(inputs, n_cores=8, n_layers=DEPTH, nseq=2):
    key = (n_cores, n_layers, nseq)
    nc = bass.Bass("TRN2", target_bir_lowering=False)
    Builder(nc, nseq, n_layers).build()
    ident, masks, rope = _consts()
    f = lambda a: np.ascontiguousarray(np.asarray(a, dtype=np.float32))
    x = f(inputs["x"])
    cdw = f(inputs["conv_dw"])
    cp = np.concatenate([cdw.transpose(0, 2, 1), f(inputs["conv_dw_b"])[:, :, None], f(inputs["conv_ln_g"])[:, :, None],
                         f(inputs["conv_ln_b"])[:, :, None]], axis=2)
    cp = np.ascontiguousarray(cp.reshape(DEPTH, 2, 128, 34).transpose(0, 2, 1, 3))
    dcp = f(inputs["dn_conv"]).transpose(0, 2, 1)
    dcp = np.ascontiguousarray(dcp.reshape(DEPTH, 6, 128, 5).transpose(0, 2, 1, 3))
    shared = {
        "w_in": f(inputs["w_in"]), "w_o": f(inputs["w_o"]),
        "ln1_g": f(inputs["ln1_g"]), "ln1_b": f(inputs["ln1_b"]), "ln2_g": f(inputs["ln2_g"]), "ln2_b": f(inputs["ln2_b"]),
        "conv_cp": cp, "conv_pw": f(inputs["conv_pw"]),
        "diff_lambda": f(inputs["diff_lambda"]).reshape(DEPTH, 256), "diff_subln_g": f(inputs["diff_subln_g"]),
        "dn_cp": dcp, "dn_a_log": f(inputs["dn_a_log"]).reshape(DEPTH, 8), "dn_dt_bias": f(inputs["dn_dt_bias"]).reshape(DEPTH, 8),
        "dn_norm_g": f(inputs["dn_norm_g"]),
        "c_ident": ident, "c_masks": masks, "c_rope": rope,
    }
    nd = (n_layers + 1) // 2
    nm = n_layers // 2
    for k_ in ("ffn_w1", "ffn_w3", "ffn_w2"):
        shared[k_] = f(inputs[k_])[:nd]
    if nm > 0:
        for k_ in ("moe_w1", "moe_w3", "moe_w2"):
            shared[k_] = f(inputs[k_])[:nm]
        shared["router_w"] = np.ascontiguousarray(f(inputs["router_w"])[:nm].reshape(nm, 8, 128, NEXP).transpose(0, 2, 1, 3))
    in_maps = []
    for c in range(n_cores):
        m = dict(shared)
        m["x"] = np.ascontiguousarray(x[c * nseq:(c + 1) * nseq].reshape(nseq * SEQ, D))
        in_maps.append(m)
    res = run_bass_kernel_spmd(nc, in_maps, core_ids=list(range(n_cores)))
    global _LAST
    _LAST = res.results
    out = np.stack([res.results[c]["y"].reshape(nseq, SEQ, D) for c in range(n_cores)], 0)
    return out.reshape(n_cores * nseq, SEQ, D).astype(np.float32)


def kernel(**inputs):
    return run(inputs, n_cores=8, n_layers=DEPTH, nseq=2)
```

```python
import contextlib
import math
import numpy as np
import concourse.bass as bass
import concourse.mybir as mybir
from concourse.bass_utils import run_bass_kernel_spmd

F32 = mybir.dt.float32
BF16 = mybir.dt.bfloat16
AF = mybir.ActivationFunctionType
ALU = mybir.AluOpType
AX = mybir.AxisListType

D = 1024
SEQ = 2048
DEPTH = 4
PROJ = 3088
DFF = 2816
DFFE = 3584
NEXP = 8
ALPHA = (2 * DEPTH) ** 0.25
EPS = 1e-5
C_CONV, C_Q, C_K, C_V, C_DN, C_Z, C_B, C_A = 0, 512, 1024, 1536, 2048, 2816, 3072, 3080


class Buf:
    __slots__ = ("w", "r")

    def __init__(self):
        self.w = None
        self.r = {}


class TT:
    __slots__ = ("t", "b")

    def __init__(self, t):
        self.t = t
        self.b = Buf()

    def __getitem__(self, k):
        return self.t[k]


class Sched:
    ENGS = ("pe", "act", "dve", "pool", "sp")
    NDS = 48

    def __init__(self, nc, es):
        self.nc = nc
        self.sems = {e: es.enter_context(nc.semaphore("pg_" + e)) for e in self.ENGS}
        self.cnt = {e: 0 for e in self.ENGS}
        self.seen = {e: {} for e in self.ENGS}
        self.ops = {e: [] for e in self.ENGS}
        self.dsems = [es.enter_context(nc.semaphore("dq%d" % i)) for i in range(self.NDS)]
        self.dval = [0] * self.NDS
        self.dnext = 0
        self.same_engine_sync = True

    def _collect(self, eng, reads, writes):
        deps = {}

        def add(tok):
            if tok is None:
                return
            k, v = tok
            if k == eng and (eng == "pe" or not self.same_engine_sync):
                return
            if deps.get(k, 0) < v:
                deps[k] = v

        for b in reads:
            add(b.w)
        for b in writes:
            add(b.w)
            for k, v in b.r.items():
                add((k, v))
        waits = []
        seen = self.seen[eng]
        for k, v in deps.items():
            if seen.get(k, 0) >= v:
                continue
            seen[k] = v
            waits.append((k, v))
        return waits

    def _semof(self, k):
        return self.sems[k] if isinstance(k, str) else self.dsems[k]

    def op(self, eng, fn, reads=(), writes=()):
        reads = [x.b if isinstance(x, TT) else x for x in reads]
        writes = [x.b if isinstance(x, TT) else x for x in writes]
        waits = self._collect(eng, reads, writes)
        self.cnt[eng] += 1
        tok = (eng, self.cnt[eng])
        self.ops[eng].append((waits, fn, (self.sems[eng], 1)))
        for b in writes:
            b.w = tok
            b.r = {}
        for b in reads:
            if b.r.get(eng, 0) < tok[1]:
                b.r[eng] = tok[1]
        return tok

    def dma(self, q, out, in_, reads=(), writes=(), slow=False):
        reads = [x.b if isinstance(x, TT) else x for x in reads]
        writes = [x.b if isinstance(x, TT) else x for x in writes]
        waits = self._collect(q, reads, writes)
        i = self.dnext
        self.dnext = (self.dnext + 1) % self.NDS
        seen = self.seen[q]
        if self.dval[i] > 0 and seen.get(i, 0) < self.dval[i]:
            seen[i] = self.dval[i]
            waits.append((i, self.dval[i]))
        self.dval[i] += 16
        tok = (i, self.dval[i])
        if slow:
            fn = lambda e, o=out, a=in_: e.dma_start(out=o, in_=a, allow_slow_non_contiguous=True)
        else:
            fn = lambda e, o=out, a=in_: e.dma_start(out=o, in_=a)
        self.ops[q].append((waits, fn, (self.dsems[i], 16)))
        for b in writes:
            b.w = tok
            b.r = {}
        for b in reads:
            if b.r.get(i, 0) < tok[1]:
                b.r[i] = tok[1]
        return tok

    def barrier(self):
        for e in self.ENGS:
            waits = []
            seen = self.seen[e]
            for k in self.ENGS:
                if k != e and self.cnt[k] > seen.get(k, 0):
                    seen[k] = self.cnt[k]
                    waits.append((k, self.cnt[k]))
            for i in range(self.NDS):
                if self.dval[i] > seen.get(i, 0):
                    seen[i] = self.dval[i]
                    waits.append((i, self.dval[i]))
            if waits:
                self.ops[e].append((waits, None, None))

    def finish(self, es):
        nc = self.nc
        block = es.enter_context(nc.Block())

        def run(ename):
            def body(e):
                for waits, fn, inc in self.ops[ename]:
                    for k, v in waits:
                        e.wait_ge(self._semof(k), v)
                    if fn is not None:
                        fn(e).then_inc(inc[0], inc[1])
            return body

        block.tensor(run("pe"))
        block.scalar(run("act"))
        block.vector(run("dve"))
        block.gpsimd(run("pool"))
        block.sync(run("sp"))


class Builder:
    def __init__(self, nc, nseq, n_layers):
        self.nc = nc
        self.nseq = nseq
        self.ntok = nseq * SEQ
        self.n_layers = n_layers
        self.uid = 0

    def tile(self, es, shape, dt=F32, name="t"):
        self.uid += 1
        return TT(es.enter_context(self.nc.sbuf_tensor("%s_%d" % (name, self.uid), list(shape), dt)))

    def ps(self):
        p = self.psum[self.psi]
        self.psi = (self.psi + 1) % len(self.psum)
        return p

    def MM(self, out, lhsT, rhs, start, stop, rd, wr):
        self.S.op("pe", lambda e: e.matmul(out, lhsT=lhsT, rhs=rhs, start=start, stop=stop), rd, wr)

    def TR(self, out, in_, ident, rd, wr):
        self.S.op("pe", lambda e: e.transpose(out, in_, ident), rd, wr)

    def ACT(self, out, in_, func, rd, wr, bias=None, scale=None, accum=None):
        kw = {}
        if bias is not None:
            kw["bias"] = bias
        if scale is not None:
            kw["scale"] = scale
        if accum is not None:
            kw["accum_out"] = accum
        self.S.op("act", lambda e: e.activation(out=out, in_=in_, func=func, **kw), rd, wr)

    def TT_(self, eng, out, in0, in1, op, rd, wr):
        self.S.op(eng, lambda e: e.tensor_tensor(out=out, in0=in0, in1=in1, op=op), rd, wr)

    def TS(self, eng, out, in0, s1, s2, op0, op1, rd, wr):
        if op1 is None:
            self.S.op(eng, lambda e: e.tensor_scalar(out=out, in0=in0, scalar1=s1, scalar2=None, op0=op0), rd, wr)
        else:
            self.S.op(eng, lambda e: e.tensor_scalar(out=out, in0=in0, scalar1=s1, scalar2=s2, op0=op0, op1=op1), rd, wr)

    def STT(self, out, in0, scalar, in1, op0, op1, rd, wr):
        self.S.op("dve", lambda e: e.scalar_tensor_tensor(out=out, in0=in0, scalar=scalar, in1=in1, op0=op0, op1=op1), rd, wr)

    def CP(self, eng, out, in_, rd, wr):
        if eng == "act":
            self.S.op("act", lambda e: e.activation(out=out, in_=in_, func=AF.Copy), rd, wr)
        else:
            self.S.op(eng, lambda e: e.tensor_copy(out=out, in_=in_), rd, wr)

    def RED(self, out, in_, op, rd, wr):
        self.S.op("dve", lambda e: e.tensor_reduce(out=out, in_=in_, axis=AX.X, op=op), rd, wr)

    def RECIP(self, out, in_, rd, wr):
        self.S.op("dve", lambda e: e.reciprocal(out=out, in_=in_), rd, wr)

    def MEMSET(self, eng, ap, val, wr):
        self.S.op(eng, lambda e: e.memset(ap, val), [], wr)

    def rstd(self, out, in_, rd, wr, eps_ap):
        self.ACT(out, in_, AF.Sqrt, rd, wr, bias=eps_ap, scale=1.0)
        self.RECIP(out, out, wr, wr)

    def dbg(self, name, src, ap, shape, dt=F32, psum=False):
        if not self.debug or name in self._dbg:
            return
        self._dbg.add(name)
        d = self.nc.dram_tensor("dbg_" + name, list(shape), dt, kind="ExternalOutput").ap()
        if psum:
            tmp = self.dbgtmp[len(self._dbg) % 4]
            self.CP("dve", tmp[0:shape[0], 0:shape[1]], ap, [src], [tmp])
            self.S.dma("sp", d, tmp[0:shape[0], 0:shape[1]], [tmp], [])
        else:
            self.S.dma("sp", d, ap, [src], [])

    def build(self):
        nc = self.nc
        NT = self.ntok
        L = self.n_layers
        dr = {}

        def din(name, shape, dt=F32):
            dr[name] = nc.dram_tensor(name, list(shape), dt, kind="ExternalInput").ap()
            return dr[name]

        x_in = din("x", [NT, D])
        w_in = din("w_in", [DEPTH, D, PROJ])
        w_o = din("w_o", [DEPTH, D, D])
        ln1_g = din("ln1_g", [DEPTH, D]); ln1_b = din("ln1_b", [DEPTH, D])
        ln2_g = din("ln2_g", [DEPTH, D]); ln2_b = din("ln2_b", [DEPTH, D])
        cp_in = din("conv_cp", [DEPTH, 128, 2, 34])
        conv_pw = din("conv_pw", [DEPTH, 256, 256])
        diff_lambda = din("diff_lambda", [DEPTH, 256])
        diff_subln_g = din("diff_subln_g", [DEPTH, 128])
        dcp_in = din("dn_cp", [DEPTH, 128, 6, 5])
        dn_a_log = din("dn_a_log", [DEPTH, 8])
        dn_dt_bias = din("dn_dt_bias", [DEPTH, 8])
        dn_norm_g = din("dn_norm_g", [DEPTH, 64])
        nd = (L + 1) // 2
        nm = L // 2
        ffn_w1 = din("ffn_w1", [nd, D, DFF]); ffn_w3 = din("ffn_w3", [nd, D, DFF]); ffn_w2 = din("ffn_w2", [nd, DFF, D])
        if nm > 0:
            router_w = din("router_w", [nm, 128, 8, NEXP])
            moe_w1 = din("moe_w1", [nm, NEXP, D, DFFE]); moe_w3 = din("moe_w3", [nm, NEXP, D, DFFE])
            moe_w2 = din("moe_w2", [nm, NEXP, DFFE, D])
        c_ident = din("c_ident", [128, 128])
        c_masks = din("c_masks", [6, 128, 128])
        c_rope = din("c_rope", [2, 128, SEQ])
        y_out = nc.dram_tensor("y", [NT, D], F32, kind="ExternalOutput").ap()
        import os as _os
        ik = "ExternalOutput" if _os.environ.get("KDBG") else "Internal"
        xres = nc.dram_tensor("xres", [NT, D], F32, kind=ik).ap()
        xT = nc.dram_tensor("xT", [128, 8, NT], BF16, kind=ik).ap()
        self.ycT = nc.dram_tensor("ycT", [128, 8, NT], BF16, kind=ik).ap()

        with contextlib.ExitStack() as es:
            S = self.S = Sched(nc, es)
            self.psum = [TT(es.enter_context(nc.psum_tensor("psb%d" % i, [128, 512], F32))) for i in range(8)]
            self.psi = 0
            ident = self.tile(es, [128, 128], F32, "ident")
            identb = self.tile(es, [128, 128], BF16, "identb")
            onesm = self.tile(es, [128, 128], F32, "onesm")
            ones256 = self.tile(es, [128, 128], F32, "ones256")
            epsc = self.tile(es, [128, 2], F32, "epsc")
            S.dma("sp", ident[:], c_ident, [], [ident])
            S.dma("pool", identb[:], c_ident, [], [identb])
            self.MEMSET("dve", onesm[:], 1.0, [onesm])
            self.MEMSET("dve", ones256[:], 1.0 / 256.0, [ones256])
            self.MEMSET("dve", epsc[:, 0:1], EPS, [epsc])
            self.MEMSET("dve", epsc[:, 1:2], 1e-6, [epsc])
            self.ident, self.identb, self.onesm, self.ones256, self.epsc = ident, identb, onesm, ones256, epsc
            self.comb = self.tile(es, [128, NT // 128, NEXP], F32, "comb")
            self._xtbufs = [self.tile(es, [128, 8, 128], BF16, "xtb") for _ in range(2)] + \
                           [self.tile(es, [128, 8, 128], F32, "xtf") for _ in range(2)] + \
                           [self.tile(es, [128, 16], F32, "rt") for _ in range(2)]
            self.dr = dr
            self.xres, self.xT, self.y_out = xres, xT, y_out
            self.debug = bool(_os.environ.get("KDBG"))
            self.kgate = _os.environ.get("KGATE", "")
            self.ks1 = _os.environ.get("KS1", "")
            self._dbg = set()
            if self.debug:
                self.dbgtmp = [self.tile(es, [128, 512], F32, "dbgtmp") for _ in range(4)]

            with contextlib.ExitStack() as es2:
                bufs = [self.tile(es2, [128, D], F32, "pxt") for _ in range(2)]
                for tt in range(NT // 128):
                    xt = bufs[tt % 2]
                    S.dma("sp", xt[:], x_in[tt * 128:(tt + 1) * 128, :], [], [xt])
                    self.emit_xT(es2, xt, tt, None, 0)
                S.barrier()

            import os as _os
            self.kstop = _os.environ.get("KSTOP", "")
            for l in range(L):
                last = (l == L - 1)
                if self.kstop == "pro":
                    break
                for s in range(self.nseq):
                    self.mixer(l, s)
                    if self.kstop:
                        break
                if self.kstop:
                    break
                if "noffn" in self.kgate and l % 2 == 1:
                    continue
                self.ffn(l, last)
                if "stop0" in self.kgate:
                    break
            S.barrier()
            S.finish(es)

    def emit_xT(self, es, xt, tt, router, l):
        S = self.S
        xtb = self._xtbufs[tt % 2]
        xtf = self._xtbufs[2 + tt % 2]
        rt = self._xtbufs[4 + tt % 2]
        for k in range(8):
            p = self.ps()
            self.TR(p[:, 0:128], xt[:, k * 128:(k + 1) * 128], self.ident[:], [xt, self.ident], [p])
            if router is not None:
                self.CP("act", xtf[:, k, :], p[:, 0:128], [p], [xtf])
                self.CP("pool", xtb[:, k, :], xtf[:, k, :], [xtf], [xtb])
            else:
                self.CP("act" if k % 2 else "dve", xtb[:, k, :], p[:, 0:128], [p], [xtb])
        S.dma("sp", self.xT[:, :, tt * 128:(tt + 1) * 128], xtb[:], [xtb], [])
        if router is not None and "norouter" not in self.kgate:
            wr = router
            p = self.ps()
            for k in range(8):
                self.MM(p[:, 0:8], xtf[:, k, :], wr[:, k, :], k == 0, k == 7, [xtf, wr], [p])
            lg = rt[:, 0:8]
            self.CP("dve", lg, p[:, 0:8], [p], [rt])
            m1 = rt[:, 8:9]; m2 = rt[:, 9:10]; tmp = rt[:, 10:11]
            cm = self.comb[:, tt, :]
            self.RED(m1, lg, ALU.max, [rt], [rt])
            self.TS("dve", cm, lg, m1, -1e30, ALU.is_equal, ALU.mult, [rt], [self.comb])
            self.TT_("dve", cm, cm, lg, ALU.add, [rt, self.comb], [self.comb])
            self.RED(m2, cm, ALU.max, [self.comb], [rt])
            self.TS("dve", cm, lg, m2, None, ALU.is_ge, None, [rt], [self.comb])
            self.TS("dve", tmp, m1, -1.0, None, ALU.mult, None, [rt], [rt])
            ex = rt[:, 11:12]
            self.ACT(ex, m2, AF.Exp, [rt], [rt], bias=tmp, scale=1.0)
            self.TS("dve", ex, ex, 1.0, None, ALU.add, None, [rt], [rt])
            self.RECIP(ex, ex, [rt], [rt])
            self.ACT(lg, lg, AF.Exp, [rt], [rt], bias=tmp, scale=1.0)
            self.STT(cm, lg, ex, cm, ALU.mult, ALU.mult, [rt, self.comb], [self.comb])

    def layer_norm_tile(self, t, out, gt, bt, st):
        self.RED(st[:, 0:1], t[:], ALU.add, [t], [st])
        self.TS("dve", st[:, 1:2], st[:, 0:1], -1.0 / D, None, ALU.mult, None, [st], [st])
        self.TS("dve", out[:], t[:], st[:, 1:2], None, ALU.add, None, [t, st], [out])
        self.ACT(t[:], out[:], AF.Square, [out], [t])
        self.RED(st[:, 2:3], t[:], ALU.add, [t], [st])
        self.ACT(st[:, 3:4], st[:, 2:3], AF.Sqrt, [st, self.epsc], [st], bias=self.epsc[:, 0:1], scale=1.0 / D)
        self.RECIP(st[:, 3:4], st[:, 3:4], [st], [st])
        self.STT(out[:], out[:], st[:, 3:4], gt[:], ALU.mult, ALU.mult, [out, st, gt], [out])
        self.TT_("pool", out[:], out[:], bt[:], ALU.add, [out, bt], [out])

    def mixer(self, l, s):
        S = self.S
        dr = self.dr
        t0 = s * SEQ
        lam_init = 0.8 - 0.6 * math.exp(-0.3 * l)
        w_in = dr["w_in"][l]
        self.t0 = t0
        with contextlib.ExitStack() as es:
            xTs = self.tile(es, [128, 8, SEQ], BF16, "xTs")
            for k in range(8):
                S.dma("sp", xTs[:, k, :], self.xT[:, k, t0:t0 + SEQ], [], [xTs])
            ks = self.kstop
            ks1 = self.ks1 if l >= 1 else ""
            if ks in ("", "conv", "out") and (not ks1 or "conv" in ks1):
                self.conv_group(l, xTs, None, w_in)
            S.barrier()
            if ks in ("", "attn", "out") and (not ks1 or "attn" in ks1):
                self.attn_group(l, xTs, None, w_in, lam_init)
            S.barrier()
            if ks in ("", "dn", "out") and (not ks1 or "dn" in ks1):
                self.dn_group(l, xTs, None, w_in)
            S.barrier()
        if self.kstop in ("conv", "attn", "dn"):
            return
        if ks1 and "out" not in ks1:
            return
        with contextlib.ExitStack() as es:
            ycat = self.tile(es, [128, 8, SEQ], BF16, "ycat")
            for k in range(8):
                S.dma("sp", ycat[:, k, :], self.ycT[:, k, t0:t0 + SEQ], [], [ycat])
            self.out_proj(l, s, ycat)
            S.barrier()

    def load_w(self, t, src, rd=()):
        self.S.dma("pool", t, src.rearrange("(k p) m -> p k m", p=128), list(rd), [])

    def conv_group(self, l, xTs, ycat, w_in):
        S = self.S
        dr = self.dr
        with contextlib.ExitStack() as es:
            wsec = self.tile(es, [128, 8, 512], BF16, "wconv")
            S.dma("pool", wsec[:], w_in[:, C_CONV:C_CONV + 512].rearrange("(k p) m -> p k m", p=128), [], [wsec])
            cp = self.tile(es, [128, 2, 34], F32, "cp")
            S.dma("sp", cp[:], dr["conv_cp"][l], [], [cp])
            wpw = self.tile(es, [128, 2, 256], BF16, "wpw")
            S.dma("pool", wpw[:], dr["conv_pw"][l].rearrange("(k p) m -> p k m", p=128), [], [wpw])
            cpad = [self.tile(es, [128, SEQ + 30], F32, "cpad") for _ in range(2)]
            acc = [self.tile(es, [128, SEQ], F32, "cacc") for _ in range(2)]
            accb = [self.tile(es, [128, SEQ], F32, "caccb") for _ in range(2)]
            sig = [self.tile(es, [128, 512], F32, "sig") for _ in range(2)]
            for j in range(2):
                self.MEMSET("pool", cpad[j][:, 0:15], 0.0, [cpad[j]])
                self.MEMSET("pool", cpad[j][:, SEQ + 15:SEQ + 30], 0.0, [cpad[j]])
            for j in range(2):
                for tb in range(4):
                    pl = self.ps(); pg = self.ps()
                    for k in range(8):
                        self.MM(pl[:, :], wsec[:, k, j * 128:(j + 1) * 128], xTs[:, k, tb * 512:(tb + 1) * 512], k == 0, k == 7, [wsec, xTs], [pl])
                    for k in range(8):
                        self.MM(pg[:, :], wsec[:, k, 256 + j * 128:256 + (j + 1) * 128], xTs[:, k, tb * 512:(tb + 1) * 512], k == 0, k == 7, [wsec, xTs], [pg])
                    sg = sig[tb % 2]
                    self.ACT(sg[:], pg[:, :], AF.Sigmoid, [pg], [sg])
                    self.TT_("dve", cpad[j][:, 15 + tb * 512:15 + (tb + 1) * 512], pl[:, :], sg[:], ALU.mult, [pl, sg], [cpad[j]])
            for j in range(2):
                a = acc[j]
                self.TS("dve", a[:], cpad[j][:, 0:SEQ], cp[:, j, 0:1], None, ALU.mult, None, [cpad[j], cp], [a])
                for w in range(1, 31):
                    self.STT(a[:], cpad[j][:, w:w + SEQ], cp[:, j, w:w + 1], a[:], ALU.mult, ALU.add, [cpad[j], cp, a], [a])
                self.TS("pool", accb[j][:], a[:], cp[:, j, 31:32], None, ALU.add, None, [a, cp], [accb[j]])
                self.ACT(a[:], accb[j][:], AF.Square, [accb[j]], [a])
            ysil = [[self.tile(es, [128, 512], BF16, "ysil") for _ in range(2)] for _ in range(2)]
            m2 = self.tile(es, [128, 512], F32, "m2")
            rs = self.tile(es, [128, 512], F32, "rs")
            tmp = [self.tile(es, [128, 512], F32, "ctmp") for _ in range(2)]
            yst = [self.tile(es, [128, 512], BF16, "cyst") for _ in range(4)]
            for tb in range(4):
                sl = slice(tb * 512, (tb + 1) * 512)
                pm = self.ps(); pq = self.ps()
                for j in range(2):
                    self.MM(pm[:, :], self.ones256[:], accb[j][:, sl], j == 0, j == 1, [self.ones256, accb[j]], [pm])
                for j in range(2):
                    self.MM(pq[:, :], self.ones256[:], acc[j][:, sl], j == 0, j == 1, [self.ones256, acc[j]], [pq])
                self.ACT(m2[:], pm[:, :], AF.Square, [pm], [m2])
                self.TT_("dve", rs[:], pq[:, :], m2[:], ALU.subtract, [pq, m2], [rs])
                self.rstd(rs[:], rs[:], [rs, self.epsc], [rs], self.epsc[:, 0:1])
                for j in range(2):
                    t = tmp[j]
                    self.TT_("dve", t[:], accb[j][:, sl], pm[:, :], ALU.subtract, [accb[j], pm], [t])
                    self.TT_("pool", t[:], t[:], rs[:], ALU.mult, [t, rs], [t])
                    self.ACT(ysil[tb % 2][j][:], t[:], AF.Silu, [t, cp], [ysil[tb % 2][j]], bias=cp[:, j, 33:34], scale=cp[:, j, 32:33])
                for co in range(2):
                    po = self.ps()
                    for j in range(2):
                        self.MM(po[:, :], wpw[:, j, co * 128:(co + 1) * 128], ysil[tb % 2][j][:], j == 0, j == 1, [wpw, ysil[tb % 2][j]], [po])
                    ys = yst[(tb * 2 + co) % 4]
                    self.CP("dve", ys[:], po[:, :], [po], [ys])
                    S.dma("sp", self.ycT[:, co, self.t0 + tb * 512:self.t0 + (tb + 1) * 512], ys[:], [ys], [])

    def attn_group(self, l, xTs, ycat, w_in, lam_init):
        S = self.S
        dr = self.dr
        with contextlib.ExitStack() as es:
            rope = self.tile(es, [128, 2, SEQ], F32, "rope")
            S.dma("sp", rope[:, 0, :], dr["c_rope"][0], [], [rope])
            S.dma("sp", rope[:, 1, :], dr["c_rope"][1], [], [rope])
            wv = self.tile(es, [128, 8, 512], BF16, "wv")
            S.dma("pool", wv[:], w_in[:, C_V:C_V + 512].rearrange("(k p) m -> p k m", p=128), [], [wv])
            lamt = self.tile(es, [128, 256], F32, "lamt")
            S.dma("sp", lamt[:], dr["diff_lambda"][l:l + 1, :].partition_broadcast(128), [], [lamt])
            subg = self.tile(es, [128, 128], F32, "subg")
            S.dma("sp", subg[:], dr["diff_subln_g"][l:l + 1, :].partition_broadcast(128), [], [subg])
            lsm = self.tile(es, [128, 8], F32, "lsm")
            prod = self.tile(es, [128, 2, 64], F32, "lprod")
            lv = lamt[:].rearrange("p (a d) -> p a d", a=4)
            self.TT_("dve", prod[:, 0, :], lv[:, 0, :], lv[:, 1, :], ALU.mult, [lamt], [prod])
            self.TT_("dve", prod[:, 1, :], lv[:, 2, :], lv[:, 3, :], ALU.mult, [lamt], [prod])
            self.RED(lsm[:, 0:2], prod[:], ALU.add, [prod], [lsm])
            self.ACT(lsm[:, 2:4], lsm[:, 0:2], AF.Exp, [lsm], [lsm])
            self.TT_("dve", lsm[:, 4:5], lsm[:, 3:4], lsm[:, 2:3], ALU.subtract, [lsm], [lsm])
            self.TS("dve", lsm[:, 4:5], lsm[:, 4:5], -lam_init, None, ALU.add, None, [lsm], [lsm])
            neglam = lsm[:, 4:5]
            self.TS("dve", subg[:], subg[:], 1.0 - lam_init, None, ALU.mult, None, [subg], [subg])
            vall = self.tile(es, [128, 16, 4, 130], BF16, "vall")
            self.MEMSET("pool", vall[:, :, :, 128:129], 1.0, [vall])
            for kt in range(16):
                p = self.ps()
                for k in range(8):
                    self.MM(p[:, :], xTs[:, k, kt * 128:(kt + 1) * 128], wv[:, k, :], k == 0, k == 7, [xTs, wv], [p])
                self.CP("dve" if kt % 2 else "act", vall[:, kt, :, 0:128], p[:, :].rearrange("p (h e) -> p h e", h=4), [p], [vall])
            wqk = [self.tile(es, [128, 4, 8, 128], BF16, "wqk") for _ in range(2)]
            qrot = [self.tile(es, [128, SEQ], BF16, "qrot") for _ in range(2)]
            krot = [self.tile(es, [128, SEQ], BF16, "krot") for _ in range(2)]
            rtmp = [self.tile(es, [128, 512], F32, "rtmp") for _ in range(4)]
            PT = [[self.tile(es, [128, 512], BF16, "PT") for _ in range(16)] for _ in range(2)]
            osb = [self.tile(es, [128, 128], F32, "osb") for _ in range(2)]
            ost = [self.tile(es, [128, 8], F32, "ost") for _ in range(2)]
            junk = self.tile(es, [128, 128], F32, "ojunk")
            ayst = [self.tile(es, [128, 512], BF16, "ayst") for _ in range(2)]
            for h in range(4):
                w = wqk[h % 2]
                S.dma("pool", w[:, 0, :, :], w_in[:, C_Q + h * 128:C_Q + (h + 1) * 128].rearrange("(k p) m -> p k m", p=128), [], [w])
                S.dma("pool", w[:, 1, :, :], w_in[:, C_K + h * 128:C_K + (h + 1) * 128].rearrange("(k p) m -> p k m", p=128), [], [w])
                for a in range(2):
                    src = w[:, a, :, :].rearrange("p k (c t j) -> p k c t j", c=2, t=2)
                    dst = w[:, 2 + a, :, :].rearrange("p k (c t j) -> p k c t j", c=2, t=2)
                    for c in range(2):
                        self.CP("pool", dst[:, :, c, 0, :], src[:, :, c, 1, :], [w], [w])
                        self.CP("pool", dst[:, :, c, 1, :], src[:, :, c, 0, :], [w], [w])
                qr = qrot[h % 2]; kr = krot[h % 2]
                ri = 0
                for a, dstt in ((0, qr), (1, kr)):
                    for tb in range(4):
                        sl = slice(tb * 512, (tb + 1) * 512)
                        p0 = self.ps(); p1 = self.ps()
                        for k in range(8):
                            self.MM(p0[:, :], w[:, a, k, :], xTs[:, k, sl], k == 0, k == 7, [w, xTs], [p0])
                        for k in range(8):
                            self.MM(p1[:, :], w[:, 2 + a, k, :], xTs[:, k, sl], k == 0, k == 7, [w, xTs], [p1])
                        ta = rtmp[ri % 4]; tb_ = rtmp[(ri + 1) % 4]; ri += 2
                        self.TT_("dve", ta[:], p0[:, :], rope[:, 0, sl], ALU.mult, [p0, rope], [ta])
                        self.TT_("dve", tb_[:], p1[:, :], rope[:, 1, sl], ALU.mult, [p1, rope], [tb_])
                        self.TT_("pool", dstt[:, sl], ta[:], tb_[:], ALU.add, [ta, tb_], [dstt])
                self.dbg("qr", qr, qr[:], [128, SEQ], BF16)
                self.dbg("kr", kr, kr[:], [128, SEQ], BF16)
                self.dbg("vall", vall, vall[:, 0, :, :], [128, 4, 130], BF16)
                for qb in range(4):
                    qs = slice(qb * 512, (qb + 1) * 512)
                    for kt in range(16):
                        for c in range(2):
                            p = self.ps()
                            self.MM(p[:, :], kr[c * 64:(c + 1) * 64, kt * 128:(kt + 1) * 128], qr[c * 64:(c + 1) * 64, qs], True, True, [kr, qr], [p])
                            self.ACT(PT[c][kt][:], p[:, :], AF.Exp, [p], [PT[c][kt]], scale=0.125)
                    for qt in range(4):
                        qi = qb * 4 + qt
                        po = self.ps()
                        for c in range(2):
                            for kt in range(16):
                                self.MM(po[:, c * 256:c * 256 + 129], PT[c][kt][:, qt * 128:(qt + 1) * 128], vall[:, kt, h, 0:129], kt == 0, kt == 15, [PT[c][kt], vall], [po])
                        st = ost[qi % 2]; ob = osb[qi % 2]
                        self.dbg("pt", PT[0][0], PT[0][0][:], [128, 512], BF16)
                        self.dbg("po", po, po[:, :], [128, 512], F32, psum=True)
                        self.CP("dve", st[:, 0:1], po[:, 128:129], [po], [st])
                        self.CP("dve", st[:, 1:2], po[:, 256 + 128:256 + 129], [po], [st])
                        self.RECIP(st[:, 0:2], st[:, 0:2], [st], [st])
                        self.TT_("dve", st[:, 1:2], st[:, 1:2], neglam, ALU.mult, [st, lsm], [st])
                        self.TS("dve", ob[:], po[:, 0:128], st[:, 0:1], None, ALU.mult, None, [po, st], [ob])
                        self.STT(ob[:], po[:, 256:256 + 128], st[:, 1:2], ob[:], ALU.mult, ALU.add, [po, st, ob], [ob])
                        self.dbg("ob0", ob, ob[:], [128, 128], F32)
                        self.ACT(junk[:], ob[:], AF.Square, [ob], [junk])
                        self.RED(st[:, 2:3], junk[:], ALU.add, [junk], [st])
                        self.ACT(st[:, 3:4], st[:, 2:3], AF.Sqrt, [st, self.epsc], [st], bias=self.epsc[:, 0:1], scale=1.0 / 128.0)
                        self.RECIP(st[:, 3:4], st[:, 3:4], [st], [st])
                        self.STT(ob[:], ob[:], st[:, 3:4], subg[:], ALU.mult, ALU.mult, [ob, st, subg], [ob])
                        self.dbg("ob1", ob, ob[:], [128, 128], F32)
                        self.dbg("st", st, st[:], [128, 8], F32)
                        self.dbg("subg", subg, subg[:], [128, 128], F32)
                        self.dbg("lsm", lsm, lsm[:], [128, 8], F32)
                        pt = self.ps()
                        self.TR(pt[:, 0:128], ob[:], self.ident[:], [ob, self.ident], [pt])
                        ys = ayst[qb % 2]
                        self.CP("act", ys[:, qt * 128:(qt + 1) * 128], pt[:, 0:128], [pt], [ys])
                        if qt == 3:
                            S.dma("sp", self.ycT[:, 2 + h, self.t0 + qb * 512:self.t0 + (qb + 1) * 512], ys[:], [ys], [])

    def dn_group(self, l, xTs, ycat, w_in):
        S = self.S
        dr = self.dr
        NCH = 16
        with contextlib.ExitStack() as es:
            ident, identb = self.ident, self.identb
            masks = self.tile(es, [128, 6, 128], F32, "masks")
            for i in range(6):
                S.dma("sp", masks[:, i, :], dr["c_masks"][i], [], [masks])
            qkv = self.tile(es, [128, NCH, 768], BF16, "qkvtm")
            zba = self.tile(es, [128, NCH, 272], F32, "zba")
            qkfm = self.tile(es, [128, 4, NCH, 128], BF16, "qkfm")
            gb = self.tile(es, [128, NCH, 16], F32, "gb")
            with contextlib.ExitStack() as es1:
                wdn = self.tile(es1, [128, 8, 768], BF16, "wdn")
                S.dma("pool", wdn[:], w_in[:, C_DN:C_DN + 768].rearrange("(k p) m -> p k m", p=128), [], [wdn])
                wz = self.tile(es1, [128, 8, 272], BF16, "wz")
                S.dma("pool", wz[:], w_in[:, C_Z:C_Z + 272].rearrange("(k p) m -> p k m", p=128), [], [wz])
                dcp = self.tile(es1, [128, 6, 5], F32, "dcp")
                S.dma("sp", dcp[:], dr["dn_cp"][l], [], [dcp])
                rows = self.tile(es1, [128, 16], F32, "dnrows")
                S.dma("sp", rows[:, 0:8], dr["dn_a_log"][l:l + 1, :].partition_broadcast(128), [], [rows])
                S.dma("sp", rows[:, 8:16], dr["dn_dt_bias"][l:l + 1, :].partition_broadcast(128), [], [rows])
                dpad = [self.tile(es1, [128, SEQ + 4], F32, "dpad")]
                dacc = [self.tile(es1, [128, SEQ], F32, "dacc")]
                qf32 = self.tile(es1, [128, NCH, 512], F32, "qf32")
                for j in range(1):
                    self.MEMSET("pool", dpad[j][:, 0:2], 0.0, [dpad[j]])
                    self.MEMSET("pool", dpad[j][:, SEQ + 2:SEQ + 4], 0.0, [dpad[j]])
                for ch in range(6):
                    dp = dpad[0]; da = dacc[0]
                    for tb in range(4):
                        p = self.ps()
                        for k in range(8):
                            self.MM(p[:, :], wdn[:, k, ch * 128:(ch + 1) * 128], xTs[:, k, tb * 512:(tb + 1) * 512], k == 0, k == 7, [wdn, xTs], [p])
                        self.CP("act", dp[:, 2 + tb * 512:2 + (tb + 1) * 512], p[:, :], [p], [dp])
                    self.TS("dve", da[:], dp[:, 0:SEQ], dcp[:, ch, 0:1], None, ALU.mult, None, [dp, dcp], [da])
                    for w in range(1, 5):
                        self.STT(da[:], dp[:, w:w + SEQ], dcp[:, ch, w:w + 1], da[:], ALU.mult, ALU.add, [dp, dcp, da], [da])
                    self.ACT(da[:], da[:], AF.Silu, [da], [da])
                    for tt in range(NCH):
                        p = self.ps()
                        self.TR(p[:, 0:128], da[:, tt * 128:(tt + 1) * 128], ident[:], [da, ident], [p])
                        if ch < 4:
                            self.CP("dve" if tt % 2 else "act", qf32[:, tt, ch * 128:(ch + 1) * 128], p[:, 0:128], [p], [qf32])
                        else:
                            self.CP("dve" if tt % 2 else "act", qkv[:, tt, ch * 128:(ch + 1) * 128], p[:, 0:128], [p], [qkv])
                for tt in range(NCH):
                    p = self.ps()
                    for k in range(8):
                        self.MM(p[:, 0:272], xTs[:, k, tt * 128:(tt + 1) * 128], wz[:, k, :], k == 0, k == 7, [xTs, wz], [p])
                    self.CP("dve" if tt % 2 else "act", zba[:, tt, :], p[:, 0:272], [p], [zba])
                sqt = [self.tile(es1, [128, 512], F32, "dsqt") for _ in range(2)]
                ss = self.tile(es1, [128, NCH, 8], F32, "dss")
                for tt in range(NCH):
                    self.ACT(sqt[tt % 2][:], qf32[:, tt, :], AF.Square, [qf32], [sqt[tt % 2]])
                    self.RED(ss[:, tt, :], sqt[tt % 2][:].rearrange("p (h d) -> p h d", h=8), ALU.add, [sqt[tt % 2]], [ss])
                self.rstd(ss[:], ss[:], [ss, self.epsc], [ss], self.epsc[:, 1:2])
                self.TS("dve", ss[:, :, 0:4], ss[:, :, 0:4], 0.125, None, ALU.mult, None, [ss], [ss])
                for tt in range(NCH):
                    self.TT_("dve", qf32[:, tt, :].rearrange("p (h d) -> p h d", h=8), qf32[:, tt, :].rearrange("p (h d) -> p h d", h=8),
                             ss[:, tt, :].unsqueeze(2).to_broadcast([128, 8, 64]), ALU.mult, [qf32, ss], [qf32])
                self.CP("pool", qkv[:, :, 0:512], qf32[:], [qf32], [qkv])
                for tt in range(NCH):
                    for i in range(4):
                        p = self.ps()
                        self.TR(p[:, 0:128], qf32[:, tt, i * 128:(i + 1) * 128], ident[:], [qf32, ident], [p])
                        self.CP("dve" if i % 2 else "act", qkfm[:, i, tt, :], p[:, 0:128], [p], [qkfm])
                self.ACT(gb[:, :, 0:8], zba[:, :, 256:264], AF.Sigmoid, [zba], [gb])
                self.TT_("dve", gb[:, :, 8:16], zba[:, :, 264:272], rows[:, 8:16].unsqueeze(1).to_broadcast([128, NCH, 8]), ALU.add, [zba, rows], [gb])
                self.ACT(gb[:, :, 8:16], gb[:, :, 8:16], AF.Exp, [gb], [gb])
                self.ACT(gb[:, :, 8:16], gb[:, :, 8:16], AF.Ln, [gb], [gb], bias=1.0)
                self.ACT(rows[:, 0:8], rows[:, 0:8], AF.Exp, [rows], [rows])
                self.STT(gb[:, :, 8:16], gb[:, :, 8:16], -1.0, rows[:, 0:8].unsqueeze(1).to_broadcast([128, NCH, 8]), ALU.mult, ALU.mult, [gb, rows], [gb])
                S.barrier()
            import os as _os
            kdn = _os.environ.get("KDN", "")
            if kdn == "A":
                return
            ng = self.tile(es, [128, 64], F32, "dng")
            S.dma("sp", ng[:], dr["dn_norm_g"][l:l + 1, :].partition_broadcast(128), [], [ng])
            sz = zba
            self.ACT(zba[:, :, 0:256], zba[:, :, 0:256], AF.Silu, [zba], [zba])
            ydst = self.tile(es, [128, 2, SEQ], BF16, "ydst")
            MT = [self.tile(es, [64, NCH, 64], BF16, "MT") for _ in range(2)]
            Bm = [self.tile(es, [64, NCH, 64], F32, "Bm") for _ in range(2)]
            PTs = [self.tile(es, [64, NCH, 128], BF16, "PTs") for _ in range(2)]
            QKm = [self.tile(es, [128, NCH, 128], BF16, "QKm") for _ in range(2)]
            Ua = [self.tile(es, [128, NCH, 64], BF16, "Ua") for _ in range(2)]
            Sa = [self.tile(es, [64, NCH, 64], BF16, "Sa") for _ in range(2)]
            G = 4
            NR = G

            def wt(shape, dt, name):
                return [self.tile(es, shape, dt, name) for _ in range(NR)]
            gL = wt([128, 128], F32, "gL"); sc = wt([128, 8], F32, "dsc"); E = wt([128, 128], F32, "dE")
            Gi = wt([128, 128], F32, "Gi"); Gs = wt([128, 128], F32, "Gs")
            PP = [wt([128, 256], F32, "PP%d" % i) for i in range(2)]
            Z = [wt([128, 128], F32, "Z%d" % i) for i in range(2)]
            UW = wt([128, 128], BF16, "UW"); nW = wt([128, 64], BF16, "nW")
            Kd = wt([128, 64], BF16, "Kd"); Qd = wt([128, 64], BF16, "Qd")
            osum = self.tile(es, [128, 2, NCH], F32, "dosum")
            ojunk = self.tile(es, [128, 2, NCH * 64], F32, "dojunk")

            def lockstep(gens):
                gens = list(gens)
                while gens:
                    alive = []
                    for g_ in gens:
                        try:
                            next(g_)
                            alive.append(g_)
                        except StopIteration:
                            pass
                    gens = alive

            def chain(h, d, n, r):
                hb = (h % 2) * 64
                qi_ = h // 2
                Ld = masks[:, d, :]
                Mi = masks[:, 2 + 2 * d, :]
                Ms = masks[:, 3 + 2 * d, :]
                lastc = 127 if d == 0 else 0
                gcol = gb[:, n, 8 + d * 4 + h:8 + d * 4 + h + 1]
                bcol = gb[:, n, d * 4 + h:d * 4 + h + 1]
                kfm = qkfm[hb:hb + 64, 2 + qi_, n, :]
                qfm = qkfm[hb:hb + 64, qi_, n, :]
                q_tm = qkv[:, n, h * 64:(h + 1) * 64]
                k_tm = qkv[:, n, 256 + h * 64:256 + (h + 1) * 64]
                v_tm = qkv[:, n, 512 + h * 64:512 + (h + 1) * 64]
                s_ = sc[r]
                P0 = PP[0][r]
                self.TS("pool", gL[r][:], Ld, gcol, None, ALU.mult, None, [masks, gb], [gL[r]])
                yield
                pg = self.ps()
                self.MM(pg[:, 0:128], self.onesm[:], gL[r][:], True, True, [self.onesm, gL[r]], [pg])
                self.MM(pg[:, 128:129], Ld, gcol, True, True, [masks, gb], [pg])
                pk = self.ps()
                self.MM(pk[:, 0:128], kfm, kfm, True, True, [qkfm], [pk])
                self.MM(pk[:, 128:256], kfm, qfm, True, True, [qkfm], [pk])
                yield
                self.TS("dve", s_[:, 0:1], pg[:, 128:129], -1.0, None, ALU.mult, None, [pg], [s_])
                self.CP("dve", s_[:, 4:5], pg[:, lastc:lastc + 1], [pg], [s_])
                self.TS("dve", E[r][:], pg[:, 0:128], s_[:, 0:1], 0.0, ALU.add, ALU.min, [pg, s_], [E[r]])
                yield
                self.ACT(E[r][:], E[r][:], AF.Exp, [E[r]], [E[r]])
                self.ACT(s_[:, 1:2], s_[:, 0:1], AF.Exp, [s_], [s_], scale=-1.0)
                self.ACT(s_[:, 2:3], s_[:, 4:5], AF.Exp, [s_], [s_], bias=s_[:, 0:1], scale=1.0)
                self.ACT(s_[:, 3:4], s_[:, 4:5], AF.Exp, [s_], [s_])
                yield
                self.TT_("pool", Gs[r][:], E[r][:], Ms, ALU.mult, [E[r], masks], [Gs[r]])
                self.TT_("pool", Gi[r][:], E[r][:], Mi, ALU.mult, [E[r], masks], [Gi[r]])
                self.CP("pool", Z[0][r][:, 0:64], v_tm, [qkv], [Z[0][r]])
                self.TS("pool", Z[0][r][:, 64:128], k_tm, s_[:, 1:2], None, ALU.mult, None, [qkv, s_], [Z[0][r]])
                self.TS("pool", Kd[r][:], k_tm, s_[:, 2:3], None, ALU.mult, None, [qkv, s_], [Kd[r]])
                self.TS("pool", Qd[r][:], q_tm, s_[:, 1:2], None, ALU.mult, None, [qkv, s_], [Qd[r]])
                yield
                self.STT(P0[:, 0:128], pk[:, 0:128], bcol, Gs[r][:], ALU.mult, ALU.mult, [pk, gb, Gs[r]], [P0])
                self.TT_("dve", QKm[d][:, n, :], pk[:, 128:256], Gi[r][:], ALU.mult, [pk, Gi[r]], [QKm[d]])
                yield
                px = self.ps()
                self.TR(px[:, 0:128], P0[:, 0:128], ident[:], [P0, ident], [px])
                yield
                self.CP("act", P0[:, 128:256], px[:, 0:128], [px], [P0])
                yield
                zc = 0
                for k in range(7):
                    cur = PP[k % 2][r]; nxt = PP[(k + 1) % 2][r]
                    pz = self.ps()
                    self.MM(pz[:, 0:128], cur[:, 0:128], Z[zc][r][:], True, True, [cur, Z[zc][r]], [pz])
                    if k < 6:
                        pp = self.ps()
                        self.MM(pp[:, 0:128], cur[:, 128:256], cur[:, 0:128], True, True, [cur], [pp])
                        if k < 5:
                            self.MM(pp[:, 128:256], cur[:, 0:128], cur[:, 128:256], True, True, [cur], [pp])
                    yield
                    self.TT_("dve", Z[1 - zc][r][:], Z[zc][r][:], pz[:, 0:128], ALU.subtract if k == 0 else ALU.add, [Z[zc][r], pz], [Z[1 - zc][r]])
                    zc = 1 - zc
                    if k < 5:
                        self.CP("act", nxt[:, 0:256], pp[:, 0:256], [pp], [nxt])
                    elif k == 5:
                        self.CP("act", nxt[:, 0:128], pp[:, 0:128], [pp], [nxt])
                    yield
                Zf = Z[zc][r]
                self.TS("dve", UW[r][:], Zf[:], bcol, None, ALU.mult, None, [Zf, gb], [UW[r]])
                yield
                self.TS("pool", nW[r][:], UW[r][:, 64:128], -1.0, None, ALU.mult, None, [UW[r]], [nW[r]])
                self.CP("pool", Ua[d][:, n, :], UW[r][:, 0:64], [UW[r]], [Ua[d]])
                pm = self.ps()
                self.MM(pm[0:64, 0:64], UW[r][:, 64:128], Kd[r][:], True, True, [UW[r], Kd[r]], [pm])
                self.MM(pm[0:64, 64:128], Kd[r][:], UW[r][:, 0:64], True, True, [UW[r], Kd[r]], [pm])
                yield
                self.STT(MT[d][:, n, :], ident[0:64, 0:64], s_[0:64, 3:4], pm[0:64, 0:64], ALU.mult, ALU.subtract, [ident, s_, pm], [MT[d]])
                self.CP("dve", Bm[d][:, n, :], pm[0:64, 64:128], [pm], [Bm[d]])
                pq = self.ps()
                self.MM(pq[0:64, 0:128], Qd[r][:], identb[:], True, False, [Qd[r], identb], [pq])
                self.MM(pq[0:64, 0:128], nW[r][:], QKm[d][:, n, :], False, True, [nW[r], QKm[d]], [pq])
                yield
                self.CP("dve", PTs[d][:, n, :], pq[0:64, 0:128], [pq], [PTs[d]])

            def scan(d):
                order = list(range(NCH)) if d == 0 else list(range(NCH - 1, -1, -1))
                self.MEMSET("pool", Sa[d][:, order[0], :], 0.0, [Sa[d]])
                yield
                for i in range(NCH - 1):
                    n = order[i]; nn = order[i + 1]
                    pss = self.ps()
                    self.MM(pss[0:64, 0:64], MT[d][:, n, :], Sa[d][:, n, :], True, True, [MT[d], Sa[d]], [pss])
                    yield
                    self.TT_("dve", Sa[d][:, nn, :], pss[0:64, 0:64], Bm[d][:, n, :], ALU.add, [pss, Bm[d]], [Sa[d]])
                    yield

            for h in range(4):
                for d in range(2):
                    for n0 in range(0, NCH, G):
                        lockstep([chain(h, d, n0 + g_, g_) for g_ in range(G)])
                lockstep([scan(0), scan(1)])
                oc = ojunk[:, 0, :]; sq = ojunk[:, 1, :]
                for half in range(2):
                    po = self.ps()
                    for j in range(8):
                        n = half * 8 + j
                        for d in range(2):
                            self.MM(po[:, j * 64:(j + 1) * 64], PTs[d][:, n, :], Sa[d][:, n, :], d == 0, False, [PTs[d], Sa[d]], [po])
                            self.MM(po[:, j * 64:(j + 1) * 64], QKm[d][:, n, :], Ua[d][:, n, :], False, d == 1, [QKm[d], Ua[d]], [po])
                    self.CP("dve", ojunk[:, 0, half * 512:(half + 1) * 512], po[:, :], [po], [ojunk])
                oc3 = oc.rearrange("p (n e) -> p n e", e=64)
                self.ACT(sq, oc, AF.Square, [ojunk], [ojunk])
                self.RED(osum[:, 0, :], sq.rearrange("p (n e) -> p n e", e=64), ALU.add, [ojunk], [osum])
                self.ACT(osum[:, 1, :], osum[:, 0, :], AF.Sqrt, [osum, self.epsc], [osum], bias=self.epsc[:, 0:1], scale=1.0 / 64.0)
                self.RECIP(osum[:, 1, :], osum[:, 1, :], [osum], [osum])
                self.TT_("dve", oc3, oc3, osum[:, 1, :].unsqueeze(2).to_broadcast([128, NCH, 64]), ALU.mult, [ojunk, osum], [ojunk])
                self.TT_("dve", oc3, oc3, ng[:].unsqueeze(1).to_broadcast([128, NCH, 64]), ALU.mult, [ojunk, ng], [ojunk])
                yv = sz[:, :, h * 64:(h + 1) * 64]
                self.TT_("dve", yv, yv, oc3, ALU.mult, [sz, ojunk], [sz])
            for n in range(NCH):
                for c in range(2):
                    p = self.ps()
                    self.TR(p[:, 0:128], sz[:, n, c * 128:(c + 1) * 128], ident[:], [sz, ident], [p])
                    self.CP("act" if c else "dve", ydst[:, c, n * 128:(n + 1) * 128], p[:, 0:128], [p], [ydst])
            for c in range(2):
                S.dma("sp", self.ycT[:, 6 + c, self.t0:self.t0 + SEQ], ydst[:, c, :], [ydst], [])

    def out_proj(self, l, s, ycat):
        S = self.S
        dr = self.dr
        moe = (l % 2 == 1)
        with contextlib.ExitStack() as es:
            wo = self.tile(es, [128, 8, D], BF16, "wo")
            S.dma("pool", wo[:], dr["w_o"][l].rearrange("(k p) m -> p k m", p=128), [], [wo])
            gt = self.tile(es, [128, D], F32, "l1g"); bt = self.tile(es, [128, D], F32, "l1b")
            S.dma("sp", gt[:], dr["ln1_g"][l:l + 1, :].partition_broadcast(128), [], [gt])
            S.dma("sp", bt[:], dr["ln1_b"][l:l + 1, :].partition_broadcast(128), [], [bt])
            router = None
            if moe and "nortile" not in self.kgate:
                router = self.tile(es, [128, 8, NEXP], F32, "wr")
                if "nordma" in self.kgate:
                    self.MEMSET("pool", router[:], 0.0, [router])
                else:
                    S.dma("sp", router[:], dr["router_w"][l // 2], [], [router])
            xin = [self.tile(es, [128, D], F32, "xin") for _ in range(2)]
            tt_ = [self.tile(es, [128, D], F32, "tsum") for _ in range(2)]
            xo = [self.tile(es, [128, D], F32, "xo") for _ in range(2)]
            st = [self.tile(es, [128, 8], F32, "lnst") for _ in range(2)]
            src = dr["x"] if l == 0 else self.xres
            for i in range(SEQ // 128):
                tg = s * (SEQ // 128) + i
                xi = xin[i % 2]; t = tt_[i % 2]; o = xo[i % 2]
                S.dma("sp", xi[:], src[tg * 128:(tg + 1) * 128, :], [], [xi])
                for half in range(2):
                    p = self.ps()
                    for k in range(8):
                        self.MM(p[:, :], ycat[:, k, i * 128:(i + 1) * 128], wo[:, k, half * 512:(half + 1) * 512], k == 0, k == 7, [ycat, wo], [p])
                    self.STT(t[:, half * 512:(half + 1) * 512], xi[:, half * 512:(half + 1) * 512], ALPHA, p[:, :], ALU.mult, ALU.add, [xi, p], [t])
                self.layer_norm_tile(t, o, gt, bt, st[i % 2])
                S.dma("sp", self.xres[tg * 128:(tg + 1) * 128, :], o[:], [o], [])
                self.emit_xT(es, o, tg, router, l)

    def ffn(self, l, last):
        S = self.S
        dr = self.dr
        moe = (l % 2 == 1)
        j = l // 2
        NT = self.ntok
        TB = 1024
        G = 4
        nff = (DFFE if moe else DFF) // 128
        groups = [(c0, min(G, nff - c0)) for c0 in range(0, nff, G)]
        nexp = NEXP if moe else 1
        with contextlib.ExitStack() as es:
            gt = self.tile(es, [128, D], F32, "l2g"); bt = self.tile(es, [128, D], F32, "l2b")
            S.dma("sp", gt[:], dr["ln2_g"][l:l + 1, :].partition_broadcast(128), [], [gt])
            S.dma("sp", bt[:], dr["ln2_b"][l:l + 1, :].partition_broadcast(128), [], [bt])
            xTb = self.tile(es, [128, 8, TB], BF16, "xTb")
            hT = [self.tile(es, [128, G, TB], BF16, "hT") for _ in range(2)]
            w1g = [self.tile(es, [128, 8, G * 128], BF16, "w1g") for _ in range(2)]
            w3g = [self.tile(es, [128, 8, G * 128], BF16, "w3g") for _ in range(2)]
            w2g = [self.tile(es, [128, G, D], BF16, "w2g") for _ in range(2)]
            acc = self.tile(es, [128, TB // 128, D], F32, "facc")
            sil = [self.tile(es, [128, 512], F32, "sil") for _ in range(3)]
            xin = [self.tile(es, [128, D], F32, "fxin") for _ in range(2)]
            xo = [self.tile(es, [128, D], F32, "fxo") for _ in range(2)]
            st = [self.tile(es, [128, 8], F32, "flnst") for _ in range(2)]
            gi = 0
            si = 0
            for tb in range(NT // TB):
                t0 = tb * TB
                for k in range(8):
                    S.dma("sp", xTb[:, k, :], self.xT[:, k, t0:t0 + TB], [], [xTb])
                first = True
                for e in range(nexp):
                    if moe:
                        W1 = dr["moe_w1"][j, e]; W3 = dr["moe_w3"][j, e]; W2 = dr["moe_w2"][j, e]
                    else:
                        W1 = dr["ffn_w1"][j]; W3 = dr["ffn_w3"][j]; W2 = dr["ffn_w2"][j]
                    for (c0, gn) in groups:
                        r = gi % 2
                        gi += 1
                        S.dma("pool", w1g[r][:, :, 0:gn * 128], W1[:, c0 * 128:(c0 + gn) * 128].rearrange("(k p) m -> p k m", p=128), [], [w1g[r]])
                        S.dma("pool", w3g[r][:, :, 0:gn * 128], W3[:, c0 * 128:(c0 + gn) * 128].rearrange("(k p) m -> p k m", p=128), [], [w3g[r]])
                        S.dma("pool", w2g[r][:, 0:gn, :], W2[c0 * 128:(c0 + gn) * 128, :].rearrange("(c p) m -> p c m", p=128), [], [w2g[r]])
                        for c in range(gn):
                            for hb in range(TB // 512):
                                ts_ = slice(hb * 512, (hb + 1) * 512)
                                pa = self.ps(); pb = self.ps()
                                for k in range(8):
                                    self.MM(pa[:, :], w1g[r][:, k, c * 128:(c + 1) * 128], xTb[:, k, ts_], k == 0, k == 7, [w1g[r], xTb], [pa])
                                for k in range(8):
                                    self.MM(pb[:, :], w3g[r][:, k, c * 128:(c + 1) * 128], xTb[:, k, ts_], k == 0, k == 7, [w3g[r], xTb], [pb])
                                sl_ = sil[si % 3]
                                si += 1
                                self.ACT(sl_[:], pa[:, :], AF.Silu, [pa], [sl_])
                                self.TT_("dve", hT[r][:, c, ts_], pb[:, :], sl_[:], ALU.mult, [pb, sl_], [hT[r]])
                        for tt in range(TB // 128):
                            tg = tb * (TB // 128) + tt
                            for half in range(2):
                                p = self.ps()
                                for c in range(gn):
                                    self.MM(p[:, :], hT[r][:, c, tt * 128:(tt + 1) * 128], w2g[r][:, c, half * 512:(half + 1) * 512], c == 0, c == gn - 1, [hT[r], w2g[r]], [p])
                                a_ = acc[:, tt, half * 512:(half + 1) * 512]
                                if moe:
                                    cw = self.comb[:, tg, e:e + 1]
                                    if first:
                                        self.TS("dve", a_, p[:, :], cw, None, ALU.mult, None, [p, self.comb], [acc])
                                    else:
                                        self.STT(a_, p[:, :], cw, a_, ALU.mult, ALU.add, [p, self.comb, acc], [acc])
                                else:
                                    if first:
                                        self.CP("dve", a_, p[:, :], [p], [acc])
                                    else:
                                        self.TT_("dve", a_, p[:, :], a_, ALU.add, [p, acc], [acc])
                        first = False
                for tt in range(TB // 128):
                    tg = tb * (TB // 128) + tt
                    xi = xin[tt % 2]; o = xo[tt % 2]
                    S.dma("sp", xi[:], self.xres[tg * 128:(tg + 1) * 128, :], [], [xi])
                    self.STT(xi[:], xi[:], ALPHA, acc[:, tt, :], ALU.mult, ALU.add, [xi, acc], [xi])
                    self.layer_norm_tile(xi, o, gt, bt, st[tt % 2])
                    if last:
                        S.dma("sp", self.y_out[tg * 128:(tg + 1) * 128, :], o[:], [o], [])
                    else:
                        S.dma("sp", self.xres[tg * 128:(tg + 1) * 128, :], o[:], [o], [])
                        self.emit_xT(es, o, tg, None, l)
            S.barrier()


def _consts():
    ident = np.eye(128, dtype=np.float32)
    p = np.arange(128)[:, None]
    f = np.arange(128)[None, :]
    Lf = (p <= f).astype(np.float32)
    Lb = (p >= f).astype(np.float32)
    masks = np.stack([Lf, Lb, (p <= f), (p < f), (p >= f), (p > f)]).astype(np.float32)
    inv = (10000.0 ** (-np.arange(0, 64, 2, dtype=np.float32) / 64.0)).astype(np.float32)
    ang = np.arange(SEQ, dtype=np.float32)[:, None] * inv[None, :]
    cos = np.cos(ang).astype(np.float32).T
    sin = np.sin(ang).astype(np.float32).T
    cosT = np.concatenate([cos, cos, cos, cos], 0)
    sinT = np.concatenate([-sin, sin, -sin, sin], 0)
    rope = np.stack([cosT, sinT]).astype(np.float32)
    return ident, masks, rope


_CACHE = {}
_LAST = None


def run(inputs, n_cores=8, n_layers=DEPTH, nseq=2):
    key = (n_cores, n_layers, nseq)
    nc = bass.Bass("TRN2", target_bir_lowering=False)
    Builder(nc, nseq, n_layers).build()
    ident, masks, rope = _consts()
    f = lambda a: np.ascontiguousarray(np.asarray(a, dtype=np.float32))
    x = f(inputs["x"])
    cdw = f(inputs["conv_dw"])
    cp = np.concatenate([cdw.transpose(0, 2, 1), f(inputs["conv_dw_b"])[:, :, None], f(inputs["conv_ln_g"])[:, :, None],
                         f(inputs["conv_ln_b"])[:, :, None]], axis=2)
    cp = np.ascontiguousarray(cp.reshape(DEPTH, 2, 128, 34).transpose(0, 2, 1, 3))
    dcp = f(inputs["dn_conv"]).transpose(0, 2, 1)
    dcp = np.ascontiguousarray(dcp.reshape(DEPTH, 6, 128, 5).transpose(0, 2, 1, 3))
    shared = {
        "w_in": f(inputs["w_in"]), "w_o": f(inputs["w_o"]),
        "ln1_g": f(inputs["ln1_g"]), "ln1_b": f(inputs["ln1_b"]), "ln2_g": f(inputs["ln2_g"]), "ln2_b": f(inputs["ln2_b"]),
        "conv_cp": cp, "conv_pw": f(inputs["conv_pw"]),
        "diff_lambda": f(inputs["diff_lambda"]).reshape(DEPTH, 256), "diff_subln_g": f(inputs["diff_subln_g"]),
        "dn_cp": dcp, "dn_a_log": f(inputs["dn_a_log"]).reshape(DEPTH, 8), "dn_dt_bias": f(inputs["dn_dt_bias"]).reshape(DEPTH, 8),
        "dn_norm_g": f(inputs["dn_norm_g"]),
        "c_ident": ident, "c_masks": masks, "c_rope": rope,
    }
    nd = (n_layers + 1) // 2
    nm = n_layers // 2
    for k_ in ("ffn_w1", "ffn_w3", "ffn_w2"):
        shared[k_] = f(inputs[k_])[:nd]
    if nm > 0:
        for k_ in ("moe_w1", "moe_w3", "moe_w2"):
            shared[k_] = f(inputs[k_])[:nm]
        shared["router_w"] = np.ascontiguousarray(f(inputs["router_w"])[:nm].reshape(nm, 8, 128, NEXP).transpose(0, 2, 1, 3))
    in_maps = []
    for c in range(n_cores):
        m = dict(shared)
        m["x"] = np.ascontiguousarray(x[c * nseq:(c + 1) * nseq].reshape(nseq * SEQ, D))
        in_maps.append(m)
    import os as _os
    if _os.environ.get("KTRACE"):
        res = run_bass_kernel_spmd(nc, in_maps, core_ids=list(range(n_cores)), trace=True)
        print("EXEC_TIME_NS", res.exec_time_ns, flush=True)
    else:
        res = run_bass_kernel_spmd(nc, in_maps, core_ids=list(range(n_cores)))
    global _LAST
    _LAST = res.results
    out = np.stack([res.results[c]["y"].reshape(nseq, SEQ, D) for c in range(n_cores)], 0)
    return out.reshape(n_cores * nseq, SEQ, D).astype(np.float32)


def kernel(**inputs):
    return run(inputs, n_cores=8, n_layers=DEPTH, nseq=2)
```

```python
import contextlib
import math
import numpy as np
import concourse.bass as bass
import concourse.mybir as mybir
from concourse.bass_utils import run_bass_kernel_spmd

F32 = mybir.dt.float32
BF16 = mybir.dt.bfloat16
AF = mybir.ActivationFunctionType
ALU = mybir.AluOpType
AX = mybir.AxisListType

D = 1024
SEQ = 2048
DEPTH = 4
PROJ = 3088
DFF = 2816
DFFE = 3584
NEXP = 8
ALPHA = (2 * DEPTH) ** 0.25
EPS = 1e-5
C_CONV, C_Q, C_K, C_V, C_DN, C_Z, C_B, C_A = 0, 512, 1024, 1536, 2048, 2816, 3072, 3080


class Buf:
    __slots__ = ("w", "r")

    def __init__(self):
        self.w = None
        self.r = {}


class TT:
    __slots__ = ("t", "b")

    def __init__(self, t):
        self.t = t
        self.b = Buf()

    def __getitem__(self, k):
        return self.t[k]


class Sched:
    ENGS = ("pe", "act", "dve", "pool", "sp")
    NDS = 48

    def __init__(self, nc, es):
        self.nc = nc
        self.sems = {e: es.enter_context(nc.semaphore("pg_" + e)) for e in self.ENGS}
        self.cnt = {e: 0 for e in self.ENGS}
        self.seen = {e: {} for e in self.ENGS}
        self.ops = {e: [] for e in self.ENGS}
        self.dsems = [es.enter_context(nc.semaphore("dq%d" % i)) for i in range(self.NDS)]
        self.dval = [0] * self.NDS
        self.dnext = 0
        self.same_engine_sync = True

    def _collect(self, eng, reads, writes):
        deps = {}

        def add(tok):
            if tok is None:
                return
            k, v = tok
            if k == eng and (eng == "pe" or not self.same_engine_sync):
                return
            if deps.get(k, 0) < v:
                deps[k] = v

        for b in reads:
            add(b.w)
        for b in writes:
            add(b.w)
            for k, v in b.r.items():
                add((k, v))
        waits = []
        seen = self.seen[eng]
        for k, v in deps.items():
            if seen.get(k, 0) >= v:
                continue
            seen[k] = v
            waits.append((k, v))
        return waits

    def _semof(self, k):
        return self.sems[k] if isinstance(k, str) else self.dsems[k]

    def op(self, eng, fn, reads=(), writes=()):
        reads = [x.b if isinstance(x, TT) else x for x in reads]
        writes = [x.b if isinstance(x, TT) else x for x in writes]
        waits = self._collect(eng, reads, writes)
        self.cnt[eng] += 1
        tok = (eng, self.cnt[eng])
        self.ops[eng].append((waits, fn, (self.sems[eng], 1)))
        for b in writes:
            b.w = tok
            b.r = {}
        for b in reads:
            if b.r.get(eng, 0) < tok[1]:
                b.r[eng] = tok[1]
        return tok

    def dma(self, q, out, in_, reads=(), writes=(), slow=False):
        reads = [x.b if isinstance(x, TT) else x for x in reads]
        writes = [x.b if isinstance(x, TT) else x for x in writes]
        waits = self._collect(q, reads, writes)
        i = self.dnext
        self.dnext = (self.dnext + 1) % self.NDS
        seen = self.seen[q]
        if self.dval[i] > 0 and seen.get(i, 0) < self.dval[i]:
            seen[i] = self.dval[i]
            waits.append((i, self.dval[i]))
        self.dval[i] += 16
        tok = (i, self.dval[i])
        if slow:
            fn = lambda e, o=out, a=in_: e.dma_start(out=o, in_=a, allow_slow_non_contiguous=True)
        else:
            fn = lambda e, o=out, a=in_: e.dma_start(out=o, in_=a)
        self.ops[q].append((waits, fn, (self.dsems[i], 16)))
        for b in writes:
            b.w = tok
            b.r = {}
        for b in reads:
            if b.r.get(i, 0) < tok[1]:
                b.r[i] = tok[1]
        return tok

    def barrier(self):
        for e in self.ENGS:
            waits = []
            seen = self.seen[e]
            for k in self.ENGS:
                if k != e and self.cnt[k] > seen.get(k, 0):
                    seen[k] = self.cnt[k]
                    waits.append((k, self.cnt[k]))
            for i in range(self.NDS):
                if self.dval[i] > seen.get(i, 0):
                    seen[i] = self.dval[i]
                    waits.append((i, self.dval[i]))
            if waits:
                self.ops[e].append((waits, None, None))

    def finish(self, es):
        nc = self.nc
        block = es.enter_context(nc.Block())

        def run(ename):
            def body(e):
                for waits, fn, inc in self.ops[ename]:
                    for k, v in waits:
                        e.wait_ge(self._semof(k), v)
                    if fn is not None:
                        fn(e).then_inc(inc[0], inc[1])
            return body

        block.tensor(run("pe"))
        block.scalar(run("act"))
        block.vector(run("dve"))
        block.gpsimd(run("pool"))
        block.sync(run("sp"))


class Builder:
    def __init__(self, nc, nseq, n_layers):
        self.nc = nc
        self.nseq = nseq
        self.ntok = nseq * SEQ
        self.n_layers = n_layers
        self.uid = 0

    def tile(self, es, shape, dt=F32, name="t"):
        self.uid += 1
        return TT(es.enter_context(self.nc.sbuf_tensor("%s_%d" % (name, self.uid), list(shape), dt)))

    def ps(self):
        p = self.psum[self.psi]
        self.psi = (self.psi + 1) % len(self.psum)
        return p

    def MM(self, out, lhsT, rhs, start, stop, rd, wr):
        self.S.op("pe", lambda e: e.matmul(out, lhsT=lhsT, rhs=rhs, start=start, stop=stop), rd, wr)

    def TR(self, out, in_, ident, rd, wr):
        self.S.op("pe", lambda e: e.transpose(out, in_, ident), rd, wr)

    def ACT(self, out, in_, func, rd, wr, bias=None, scale=None, accum=None):
        kw = {}
        if bias is not None:
            kw["bias"] = bias
        if scale is not None:
            kw["scale"] = scale
        if accum is not None:
            kw["accum_out"] = accum
        self.S.op("act", lambda e: e.activation(out=out, in_=in_, func=func, **kw), rd, wr)

    def TT_(self, eng, out, in0, in1, op, rd, wr):
        self.S.op(eng, lambda e: e.tensor_tensor(out=out, in0=in0, in1=in1, op=op), rd, wr)

    def TS(self, eng, out, in0, s1, s2, op0, op1, rd, wr):
        if op1 is None:
            self.S.op(eng, lambda e: e.tensor_scalar(out=out, in0=in0, scalar1=s1, scalar2=None, op0=op0), rd, wr)
        else:
            self.S.op(eng, lambda e: e.tensor_scalar(out=out, in0=in0, scalar1=s1, scalar2=s2, op0=op0, op1=op1), rd, wr)

    def STT(self, out, in0, scalar, in1, op0, op1, rd, wr):
        self.S.op("dve", lambda e: e.scalar_tensor_tensor(out=out, in0=in0, scalar=scalar, in1=in1, op0=op0, op1=op1), rd, wr)

    def CP(self, eng, out, in_, rd, wr):
        if eng == "act":
            self.S.op("act", lambda e: e.activation(out=out, in_=in_, func=AF.Copy), rd, wr)
        else:
            self.S.op(eng, lambda e: e.tensor_copy(out=out, in_=in_), rd, wr)

    def RED(self, out, in_, op, rd, wr):
        self.S.op("dve", lambda e: e.tensor_reduce(out=out, in_=in_, axis=AX.X, op=op), rd, wr)

    def RECIP(self, out, in_, rd, wr):
        self.S.op("dve", lambda e: e.reciprocal(out=out, in_=in_), rd, wr)

    def MEMSET(self, eng, ap, val, wr):
        self.S.op(eng, lambda e: e.memset(ap, val), [], wr)

    def rstd(self, out, in_, rd, wr, eps_ap):
        self.ACT(out, in_, AF.Sqrt, rd, wr, bias=eps_ap, scale=1.0)
        self.RECIP(out, out, wr, wr)

    def dbg(self, name, src, ap, shape, dt=F32, psum=False):
        if not self.debug or name in self._dbg:
            return
        self._dbg.add(name)
        d = self.nc.dram_tensor("dbg_" + name, list(shape), dt, kind="ExternalOutput").ap()
        if psum:
            tmp = self.dbgtmp[len(self._dbg) % 4]
            self.CP("dve", tmp[0:shape[0], 0:shape[1]], ap, [src], [tmp])
            self.S.dma("sp", d, tmp[0:shape[0], 0:shape[1]], [tmp], [])
        else:
            self.S.dma("sp", d, ap, [src], [])

    def build(self):
        nc = self.nc
        NT = self.ntok
        L = self.n_layers
        dr = {}

        def din(name, shape, dt=F32):
            dr[name] = nc.dram_tensor(name, list(shape), dt, kind="ExternalInput").ap()
            return dr[name]

        x_in = din("x", [NT, D])
        w_in = din("w_in", [DEPTH, D, PROJ])
        w_o = din("w_o", [DEPTH, D, D])
        ln1_g = din("ln1_g", [DEPTH, D]); ln1_b = din("ln1_b", [DEPTH, D])
        ln2_g = din("ln2_g", [DEPTH, D]); ln2_b = din("ln2_b", [DEPTH, D])
        cp_in = din("conv_cp", [DEPTH, 128, 2, 34])
        conv_pw = din("conv_pw", [DEPTH, 256, 256])
        diff_lambda = din("diff_lambda", [DEPTH, 256])
        diff_subln_g = din("diff_subln_g", [DEPTH, 128])
        dcp_in = din("dn_cp", [DEPTH, 128, 6, 5])
        dn_a_log = din("dn_a_log", [DEPTH, 8])
        dn_dt_bias = din("dn_dt_bias", [DEPTH, 8])
        dn_norm_g = din("dn_norm_g", [DEPTH, 64])
        nd = (L + 1) // 2
        nm = L // 2
        ffn_w1 = din("ffn_w1", [nd, D, DFF]); ffn_w3 = din("ffn_w3", [nd, D, DFF]); ffn_w2 = din("ffn_w2", [nd, DFF, D])
        if nm > 0:
            router_w = din("router_w", [nm, 128, 8, NEXP])
            moe_w1 = din("moe_w1", [nm, NEXP, D, DFFE]); moe_w3 = din("moe_w3", [nm, NEXP, D, DFFE])
            moe_w2 = din("moe_w2", [nm, NEXP, DFFE, D])
        c_ident = din("c_ident", [128, 128])
        c_masks = din("c_masks", [6, 128, 128])
        c_rope = din("c_rope", [2, 128, SEQ])
        y_out = nc.dram_tensor("y", [NT, D], F32, kind="ExternalOutput").ap()
        import os as _os
        ik = "ExternalOutput" if _os.environ.get("KDBG") else "Internal"
        xres = nc.dram_tensor("xres", [NT, D], F32, kind=ik).ap()
        xT = nc.dram_tensor("xT", [128, 8, NT], BF16, kind=ik).ap()
        self.ycT = nc.dram_tensor("ycT", [128, 8, NT], BF16, kind=ik).ap()

        with contextlib.ExitStack() as es:
            S = self.S = Sched(nc, es)
            self.psum = [TT(es.enter_context(nc.psum_tensor("psb%d" % i, [128, 512], F32))) for i in range(8)]
            self.psi = 0
            ident = self.tile(es, [128, 128], F32, "ident")
            identb = self.tile(es, [128, 128], BF16, "identb")
            onesm = self.tile(es, [128, 128], F32, "onesm")
            ones256 = self.tile(es, [128, 128], F32, "ones256")
            epsc = self.tile(es, [128, 2], F32, "epsc")
            S.dma("sp", ident[:], c_ident, [], [ident])
            S.dma("pool", identb[:], c_ident, [], [identb])
            self.MEMSET("dve", onesm[:], 1.0, [onesm])
            self.MEMSET("dve", ones256[:], 1.0 / 256.0, [ones256])
            self.MEMSET("dve", epsc[:, 0:1], EPS, [epsc])
            self.MEMSET("dve", epsc[:, 1:2], 1e-6, [epsc])
            self.ident, self.identb, self.onesm, self.ones256, self.epsc = ident, identb, onesm, ones256, epsc
            self.comb = self.tile(es, [128, NT // 128, NEXP], F32, "comb")
            self._xtbufs = [self.tile(es, [128, 8, 128], BF16, "xtb") for _ in range(2)] + \
                           [self.tile(es, [128, 8, 128], F32, "xtf") for _ in range(2)] + \
                           [self.tile(es, [128, 16], F32, "rt") for _ in range(2)]
            self.dr = dr
            self.xres, self.xT, self.y_out = xres, xT, y_out
            self.debug = bool(_os.environ.get("KDBG"))
            self.kgate = _os.environ.get("KGATE", "")
            self.ks1 = _os.environ.get("KS1", "")
            self._dbg = set()
            if self.debug:
                self.dbgtmp = [self.tile(es, [128, 512], F32, "dbgtmp") for _ in range(4)]

            with contextlib.ExitStack() as es2:
                bufs = [self.tile(es2, [128, D], F32, "pxt") for _ in range(2)]
                for tt in range(NT // 128):
                    xt = bufs[tt % 2]
                    S.dma("sp", xt[:], x_in[tt * 128:(tt + 1) * 128, :], [], [xt])
                    self.emit_xT(es2, xt, tt, None, 0)
                S.barrier()

            import os as _os
            self.kstop = _os.environ.get("KSTOP", "")
            for l in range(L):
                last = (l == L - 1)
                if self.kstop == "pro":
                    break
                for s in range(self.nseq):
                    self.mixer(l, s)
                    if self.kstop:
                        break
                if self.kstop:
                    break
                if "noffn" in self.kgate and l % 2 == 1:
                    continue
                self.ffn(l, last)
                if "stop0" in self.kgate:
                    break
            S.barrier()
            S.finish(es)

    def emit_xT(self, es, xt, tt, router, l):
        S = self.S
        xtb = self._xtbufs[tt % 2]
        xtf = self._xtbufs[2 + tt % 2]
        rt = self._xtbufs[4 + tt % 2]
        for k in range(8):
            p = self.ps()
            self.TR(p[:, 0:128], xt[:, k * 128:(k + 1) * 128], self.ident[:], [xt, self.ident], [p])
            if router is not None:
                self.CP("act", xtf[:, k, :], p[:, 0:128], [p], [xtf])
                self.CP("pool", xtb[:, k, :], xtf[:, k, :], [xtf], [xtb])
            else:
                self.CP("act" if k % 2 else "dve", xtb[:, k, :], p[:, 0:128], [p], [xtb])
        S.dma("sp", self.xT[:, :, tt * 128:(tt + 1) * 128], xtb[:], [xtb], [])
        if router is not None and "norouter" not in self.kgate:
            wr = router
            p = self.ps()
            for k in range(8):
                self.MM(p[:, 0:8], xtf[:, k, :], wr[:, k, :], k == 0, k == 7, [xtf, wr], [p])
            lg = rt[:, 0:8]
            self.CP("dve", lg, p[:, 0:8], [p], [rt])
            m1 = rt[:, 8:9]; m2 = rt[:, 9:10]; tmp = rt[:, 10:11]
            cm = self.comb[:, tt, :]
            self.RED(m1, lg, ALU.max, [rt], [rt])
            self.TS("dve", cm, lg, m1, -1e30, ALU.is_equal, ALU.mult, [rt], [self.comb])
            self.TT_("dve", cm, cm, lg, ALU.add, [rt, self.comb], [self.comb])
            self.RED(m2, cm, ALU.max, [self.comb], [rt])
            self.TS("dve", cm, lg, m2, None, ALU.is_ge, None, [rt], [self.comb])
            self.TS("dve", tmp, m1, -1.0, None, ALU.mult, None, [rt], [rt])
            ex = rt[:, 11:12]
            self.ACT(ex, m2, AF.Exp, [rt], [rt], bias=tmp, scale=1.0)
            self.TS("dve", ex, ex, 1.0, None, ALU.add, None, [rt], [rt])
            self.RECIP(ex, ex, [rt], [rt])
            self.ACT(lg, lg, AF.Exp, [rt], [rt], bias=tmp, scale=1.0)
            self.STT(cm, lg, ex, cm, ALU.mult, ALU.mult, [rt, self.comb], [self.comb])

    def layer_norm_tile(self, t, out, gt, bt, st):
        self.RED(st[:, 0:1], t[:], ALU.add, [t], [st])
        self.TS("dve", st[:, 1:2], st[:, 0:1], -1.0 / D, None, ALU.mult, None, [st], [st])
        self.TS("dve", out[:], t[:], st[:, 1:2], None, ALU.add, None, [t, st], [out])
        self.ACT(t[:], out[:], AF.Square, [out], [t])
        self.RED(st[:, 2:3], t[:], ALU.add, [t], [st])
        self.ACT(st[:, 3:4], st[:, 2:3], AF.Sqrt, [st, self.epsc], [st], bias=self.epsc[:, 0:1], scale=1.0 / D)
        self.RECIP(st[:, 3:4], st[:, 3:4], [st], [st])
        self.STT(out[:], out[:], st[:, 3:4], gt[:], ALU.mult, ALU.mult, [out, st, gt], [out])
        self.TT_("pool", out[:], out[:], bt[:], ALU.add, [out, bt], [out])

    def layer_norm_gen(self, t, out, gt, bt, st):
        self.RED(st[:, 0:1], t[:], ALU.add, [t], [st])
        yield
        self.TS("dve", st[:, 1:2], st[:, 0:1], -1.0 / D, None, ALU.mult, None, [st], [st])
        yield
        self.TS("dve", out[:], t[:], st[:, 1:2], None, ALU.add, None, [t, st], [out])
        yield
        self.ACT(t[:], out[:], AF.Square, [out], [t])
        yield
        self.RED(st[:, 2:3], t[:], ALU.add, [t], [st])
        yield
        self.ACT(st[:, 3:4], st[:, 2:3], AF.Sqrt, [st, self.epsc], [st], bias=self.epsc[:, 0:1], scale=1.0 / D)
        yield
        self.RECIP(st[:, 3:4], st[:, 3:4], [st], [st])
        yield
        self.STT(out[:], out[:], st[:, 3:4], gt[:], ALU.mult, ALU.mult, [out, st, gt], [out])
        yield
        self.TT_("pool", out[:], out[:], bt[:], ALU.add, [out, bt], [out])
        yield

    @staticmethod
    def lockstep(gens):
        gens = list(gens)
        while gens:
            alive = []
            for g_ in gens:
                try:
                    next(g_)
                    alive.append(g_)
                except StopIteration:
                    pass
            gens = alive

    def mixer(self, l, s):
        S = self.S
        dr = self.dr
        t0 = s * SEQ
        lam_init = 0.8 - 0.6 * math.exp(-0.3 * l)
        w_in = dr["w_in"][l]
        self.t0 = t0
        with contextlib.ExitStack() as es:
            xTs = self.tile(es, [128, 8, SEQ], BF16, "xTs")
            for k in range(8):
                S.dma("sp", xTs[:, k, :], self.xT[:, k, t0:t0 + SEQ], [], [xTs])
            ks = self.kstop
            ks1 = self.ks1 if l >= 1 else ""
            if ks in ("", "conv", "out") and (not ks1 or "conv" in ks1):
                self.conv_group(l, xTs, None, w_in)
            S.barrier()
            if ks in ("", "attn", "out") and (not ks1 or "attn" in ks1):
                self.attn_group(l, xTs, None, w_in, lam_init)
            S.barrier()
            if ks in ("", "dn", "out") and (not ks1 or "dn" in ks1):
                self.dn_group(l, xTs, None, w_in)
            S.barrier()
        if self.kstop in ("conv", "attn", "dn"):
            return
        if ks1 and "out" not in ks1:
            return
        with contextlib.ExitStack() as es:
            ycat = self.tile(es, [128, 8, SEQ], BF16, "ycat")
            for k in range(8):
                S.dma("sp", ycat[:, k, :], self.ycT[:, k, t0:t0 + SEQ], [], [ycat])
            self.out_proj(l, s, ycat)
            S.barrier()

    def load_w(self, t, src, rd=()):
        self.S.dma("pool", t, src.rearrange("(k p) m -> p k m", p=128), list(rd), [])

    def conv_group(self, l, xTs, ycat, w_in):
        S = self.S
        dr = self.dr
        with contextlib.ExitStack() as es:
            wsec = self.tile(es, [128, 8, 512], BF16, "wconv")
            S.dma("pool", wsec[:], w_in[:, C_CONV:C_CONV + 512].rearrange("(k p) m -> p k m", p=128), [], [wsec])
            cp = self.tile(es, [128, 2, 34], F32, "cp")
            S.dma("sp", cp[:], dr["conv_cp"][l], [], [cp])
            wpw = self.tile(es, [128, 2, 256], BF16, "wpw")
            S.dma("pool", wpw[:], dr["conv_pw"][l].rearrange("(k p) m -> p k m", p=128), [], [wpw])
            cpad = [self.tile(es, [128, SEQ + 30], F32, "cpad") for _ in range(2)]
            acc = [self.tile(es, [128, SEQ], F32, "cacc") for _ in range(2)]
            accb = [self.tile(es, [128, SEQ], F32, "caccb") for _ in range(2)]
            sig = [self.tile(es, [128, 512], F32, "sig") for _ in range(2)]
            for j in range(2):
                self.MEMSET("pool", cpad[j][:, 0:15], 0.0, [cpad[j]])
                self.MEMSET("pool", cpad[j][:, SEQ + 15:SEQ + 30], 0.0, [cpad[j]])
            for j in range(2):
                for tb in range(4):
                    pl = self.ps(); pg = self.ps()
                    for k in range(8):
                        self.MM(pl[:, :], wsec[:, k, j * 128:(j + 1) * 128], xTs[:, k, tb * 512:(tb + 1) * 512], k == 0, k == 7, [wsec, xTs], [pl])
                    for k in range(8):
                        self.MM(pg[:, :], wsec[:, k, 256 + j * 128:256 + (j + 1) * 128], xTs[:, k, tb * 512:(tb + 1) * 512], k == 0, k == 7, [wsec, xTs], [pg])
                    sg = sig[tb % 2]
                    self.ACT(sg[:], pg[:, :], AF.Sigmoid, [pg], [sg])
                    self.TT_("dve", cpad[j][:, 15 + tb * 512:15 + (tb + 1) * 512], pl[:, :], sg[:], ALU.mult, [pl, sg], [cpad[j]])
            for j in range(2):
                a = acc[j]
                self.TS("dve", a[:], cpad[j][:, 0:SEQ], cp[:, j, 0:1], None, ALU.mult, None, [cpad[j], cp], [a])
                for w in range(1, 31):
                    self.STT(a[:], cpad[j][:, w:w + SEQ], cp[:, j, w:w + 1], a[:], ALU.mult, ALU.add, [cpad[j], cp, a], [a])
                self.TS("pool", accb[j][:], a[:], cp[:, j, 31:32], None, ALU.add, None, [a, cp], [accb[j]])
                self.ACT(a[:], accb[j][:], AF.Square, [accb[j]], [a])
            ysil = [[self.tile(es, [128, 512], BF16, "ysil") for _ in range(2)] for _ in range(2)]
            m2 = self.tile(es, [128, 512], F32, "m2")
            rs = self.tile(es, [128, 512], F32, "rs")
            tmp = [self.tile(es, [128, 512], F32, "ctmp") for _ in range(2)]
            yst = [self.tile(es, [128, 512], BF16, "cyst") for _ in range(4)]
            for tb in range(4):
                sl = slice(tb * 512, (tb + 1) * 512)
                pm = self.ps(); pq = self.ps()
                for j in range(2):
                    self.MM(pm[:, :], self.ones256[:], accb[j][:, sl], j == 0, j == 1, [self.ones256, accb[j]], [pm])
                for j in range(2):
                    self.MM(pq[:, :], self.ones256[:], acc[j][:, sl], j == 0, j == 1, [self.ones256, acc[j]], [pq])
                self.ACT(m2[:], pm[:, :], AF.Square, [pm], [m2])
                self.TT_("dve", rs[:], pq[:, :], m2[:], ALU.subtract, [pq, m2], [rs])
                self.rstd(rs[:], rs[:], [rs, self.epsc], [rs], self.epsc[:, 0:1])
                for j in range(2):
                    t = tmp[j]
                    self.TT_("dve", t[:], accb[j][:, sl], pm[:, :], ALU.subtract, [accb[j], pm], [t])
                    self.TT_("pool", t[:], t[:], rs[:], ALU.mult, [t, rs], [t])
                    self.ACT(ysil[tb % 2][j][:], t[:], AF.Silu, [t, cp], [ysil[tb % 2][j]], bias=cp[:, j, 33:34], scale=cp[:, j, 32:33])
                for co in range(2):
                    po = self.ps()
                    for j in range(2):
                        self.MM(po[:, :], wpw[:, j, co * 128:(co + 1) * 128], ysil[tb % 2][j][:], j == 0, j == 1, [wpw, ysil[tb % 2][j]], [po])
                    ys = yst[(tb * 2 + co) % 4]
                    self.CP("dve", ys[:], po[:, :], [po], [ys])
                    S.dma("sp", self.ycT[:, co, self.t0 + tb * 512:self.t0 + (tb + 1) * 512], ys[:], [ys], [])

    def attn_group(self, l, xTs, ycat, w_in, lam_init):
        S = self.S
        dr = self.dr
        with contextlib.ExitStack() as es:
            rope = self.tile(es, [128, 2, SEQ], F32, "rope")
            S.dma("sp", rope[:, 0, :], dr["c_rope"][0], [], [rope])
            S.dma("sp", rope[:, 1, :], dr["c_rope"][1], [], [rope])
            wv = self.tile(es, [128, 8, 512], BF16, "wv")
            S.dma("pool", wv[:], w_in[:, C_V:C_V + 512].rearrange("(k p) m -> p k m", p=128), [], [wv])
            lamt = self.tile(es, [128, 256], F32, "lamt")
            S.dma("sp", lamt[:], dr["diff_lambda"][l:l + 1, :].partition_broadcast(128), [], [lamt])
            subg = self.tile(es, [128, 128], F32, "subg")
            S.dma("sp", subg[:], dr["diff_subln_g"][l:l + 1, :].partition_broadcast(128), [], [subg])
            lsm = self.tile(es, [128, 8], F32, "lsm")
            prod = self.tile(es, [128, 2, 64], F32, "lprod")
            lv = lamt[:].rearrange("p (a d) -> p a d", a=4)
            self.TT_("dve", prod[:, 0, :], lv[:, 0, :], lv[:, 1, :], ALU.mult, [lamt], [prod])
            self.TT_("dve", prod[:, 1, :], lv[:, 2, :], lv[:, 3, :], ALU.mult, [lamt], [prod])
            self.RED(lsm[:, 0:2], prod[:], ALU.add, [prod], [lsm])
            self.ACT(lsm[:, 2:4], lsm[:, 0:2], AF.Exp, [lsm], [lsm])
            self.TT_("dve", lsm[:, 4:5], lsm[:, 3:4], lsm[:, 2:3], ALU.subtract, [lsm], [lsm])
            self.TS("dve", lsm[:, 4:5], lsm[:, 4:5], -lam_init, None, ALU.add, None, [lsm], [lsm])
            neglam = lsm[:, 4:5]
            self.TS("dve", subg[:], subg[:], 1.0 - lam_init, None, ALU.mult, None, [subg], [subg])
            vall = self.tile(es, [128, 16, 4, 130], BF16, "vall")
            self.MEMSET("pool", vall[:, :, :, 128:129], 1.0, [vall])
            for kt in range(16):
                p = self.ps()
                for k in range(8):
                    self.MM(p[:, :], xTs[:, k, kt * 128:(kt + 1) * 128], wv[:, k, :], k == 0, k == 7, [xTs, wv], [p])
                self.CP("dve" if kt % 2 else "act", vall[:, kt, :, 0:128], p[:, :].rearrange("p (h e) -> p h e", h=4), [p], [vall])
            wqk = [self.tile(es, [128, 4, 8, 128], BF16, "wqk") for _ in range(2)]
            qrot = [self.tile(es, [128, SEQ], BF16, "qrot") for _ in range(2)]
            krot = [self.tile(es, [128, SEQ], BF16, "krot") for _ in range(2)]
            rtmp = [self.tile(es, [128, 512], F32, "rtmp") for _ in range(4)]
            PT = [[self.tile(es, [128, 512], BF16, "PT") for _ in range(16)] for _ in range(2)]
            osb = [self.tile(es, [128, 128], F32, "osb") for _ in range(2)]
            ost = [self.tile(es, [128, 8], F32, "ost") for _ in range(2)]
            junk = self.tile(es, [128, 128], F32, "ojunk")
            ayst = [self.tile(es, [128, 512], BF16, "ayst") for _ in range(2)]
            for h in range(4):
                w = wqk[h % 2]
                S.dma("pool", w[:, 0, :, :], w_in[:, C_Q + h * 128:C_Q + (h + 1) * 128].rearrange("(k p) m -> p k m", p=128), [], [w])
                S.dma("pool", w[:, 1, :, :], w_in[:, C_K + h * 128:C_K + (h + 1) * 128].rearrange("(k p) m -> p k m", p=128), [], [w])
                for a in range(2):
                    src = w[:, a, :, :].rearrange("p k (c t j) -> p k c t j", c=2, t=2)
                    dst = w[:, 2 + a, :, :].rearrange("p k (c t j) -> p k c t j", c=2, t=2)
                    for c in range(2):
                        self.CP("pool", dst[:, :, c, 0, :], src[:, :, c, 1, :], [w], [w])
                        self.CP("pool", dst[:, :, c, 1, :], src[:, :, c, 0, :], [w], [w])
                qr = qrot[h % 2]; kr = krot[h % 2]
                ri = 0
                for a, dstt in ((0, qr), (1, kr)):
                    for tb in range(4):
                        sl = slice(tb * 512, (tb + 1) * 512)
                        p0 = self.ps(); p1 = self.ps()
                        for k in range(8):
                            self.MM(p0[:, :], w[:, a, k, :], xTs[:, k, sl], k == 0, k == 7, [w, xTs], [p0])
                        for k in range(8):
                            self.MM(p1[:, :], w[:, 2 + a, k, :], xTs[:, k, sl], k == 0, k == 7, [w, xTs], [p1])
                        ta = rtmp[ri % 4]; tb_ = rtmp[(ri + 1) % 4]; ri += 2
                        self.TT_("dve", ta[:], p0[:, :], rope[:, 0, sl], ALU.mult, [p0, rope], [ta])
                        self.TT_("dve", tb_[:], p1[:, :], rope[:, 1, sl], ALU.mult, [p1, rope], [tb_])
                        self.TT_("pool", dstt[:, sl], ta[:], tb_[:], ALU.add, [ta, tb_], [dstt])
                self.dbg("qr", qr, qr[:], [128, SEQ], BF16)
                self.dbg("kr", kr, kr[:], [128, SEQ], BF16)
                self.dbg("vall", vall, vall[:, 0, :, :], [128, 4, 130], BF16)
                for qb in range(4):
                    qs = slice(qb * 512, (qb + 1) * 512)
                    for kt in range(16):
                        for c in range(2):
                            p = self.ps()
                            self.MM(p[:, :], kr[c * 64:(c + 1) * 64, kt * 128:(kt + 1) * 128], qr[c * 64:(c + 1) * 64, qs], True, True, [kr, qr], [p])
                            self.ACT(PT[c][kt][:], p[:, :], AF.Exp, [p], [PT[c][kt]], scale=0.125)
                    for qt in range(4):
                        qi = qb * 4 + qt
                        po = self.ps()
                        for c in range(2):
                            for kt in range(16):
                                self.MM(po[:, c * 256:c * 256 + 129], PT[c][kt][:, qt * 128:(qt + 1) * 128], vall[:, kt, h, 0:129], kt == 0, kt == 15, [PT[c][kt], vall], [po])
                        st = ost[qi % 2]; ob = osb[qi % 2]
                        self.dbg("pt", PT[0][0], PT[0][0][:], [128, 512], BF16)
                        self.dbg("po", po, po[:, :], [128, 512], F32, psum=True)
                        self.CP("dve", st[:, 0:1], po[:, 128:129], [po], [st])
                        self.CP("dve", st[:, 1:2], po[:, 256 + 128:256 + 129], [po], [st])
                        self.RECIP(st[:, 0:2], st[:, 0:2], [st], [st])
                        self.TT_("dve", st[:, 1:2], st[:, 1:2], neglam, ALU.mult, [st, lsm], [st])
                        self.TS("dve", ob[:], po[:, 0:128], st[:, 0:1], None, ALU.mult, None, [po, st], [ob])
                        self.STT(ob[:], po[:, 256:256 + 128], st[:, 1:2], ob[:], ALU.mult, ALU.add, [po, st, ob], [ob])
                        self.dbg("ob0", ob, ob[:], [128, 128], F32)
                        self.ACT(junk[:], ob[:], AF.Square, [ob], [junk])
                        self.RED(st[:, 2:3], junk[:], ALU.add, [junk], [st])
                        self.ACT(st[:, 3:4], st[:, 2:3], AF.Sqrt, [st, self.epsc], [st], bias=self.epsc[:, 0:1], scale=1.0 / 128.0)
                        self.RECIP(st[:, 3:4], st[:, 3:4], [st], [st])
                        self.STT(ob[:], ob[:], st[:, 3:4], subg[:], ALU.mult, ALU.mult, [ob, st, subg], [ob])
                        self.dbg("ob1", ob, ob[:], [128, 128], F32)
                        self.dbg("st", st, st[:], [128, 8], F32)
                        self.dbg("subg", subg, subg[:], [128, 128], F32)
                        self.dbg("lsm", lsm, lsm[:], [128, 8], F32)
                        pt = self.ps()
                        self.TR(pt[:, 0:128], ob[:], self.ident[:], [ob, self.ident], [pt])
                        ys = ayst[qb % 2]
                        self.CP("act", ys[:, qt * 128:(qt + 1) * 128], pt[:, 0:128], [pt], [ys])
                        if qt == 3:
                            S.dma("sp", self.ycT[:, 2 + h, self.t0 + qb * 512:self.t0 + (qb + 1) * 512], ys[:], [ys], [])

    def dn_group(self, l, xTs, ycat, w_in):
        S = self.S
        dr = self.dr
        NCH = 16
        with contextlib.ExitStack() as es:
            ident, identb = self.ident, self.identb
            masks = self.tile(es, [128, 6, 128], F32, "masks")
            for i in range(6):
                S.dma("sp", masks[:, i, :], dr["c_masks"][i], [], [masks])
            qkv = self.tile(es, [128, NCH, 768], BF16, "qkvtm")
            zba = self.tile(es, [128, NCH, 272], F32, "zba")
            qkfm = self.tile(es, [128, 4, NCH, 128], BF16, "qkfm")
            gb = self.tile(es, [128, NCH, 16], F32, "gb")
            with contextlib.ExitStack() as es1:
                wdn = self.tile(es1, [128, 8, 768], BF16, "wdn")
                S.dma("pool", wdn[:], w_in[:, C_DN:C_DN + 768].rearrange("(k p) m -> p k m", p=128), [], [wdn])
                wz = self.tile(es1, [128, 8, 272], BF16, "wz")
                S.dma("pool", wz[:], w_in[:, C_Z:C_Z + 272].rearrange("(k p) m -> p k m", p=128), [], [wz])
                dcp = self.tile(es1, [128, 6, 5], F32, "dcp")
                S.dma("sp", dcp[:], dr["dn_cp"][l], [], [dcp])
                rows = self.tile(es1, [128, 16], F32, "dnrows")
                S.dma("sp", rows[:, 0:8], dr["dn_a_log"][l:l + 1, :].partition_broadcast(128), [], [rows])
                S.dma("sp", rows[:, 8:16], dr["dn_dt_bias"][l:l + 1, :].partition_broadcast(128), [], [rows])
                dpad = [self.tile(es1, [128, SEQ + 4], F32, "dpad")]
                dacc = [self.tile(es1, [128, SEQ], F32, "dacc")]
                qf32 = self.tile(es1, [128, NCH, 512], F32, "qf32")
                for j in range(1):
                    self.MEMSET("pool", dpad[j][:, 0:2], 0.0, [dpad[j]])
                    self.MEMSET("pool", dpad[j][:, SEQ + 2:SEQ + 4], 0.0, [dpad[j]])
                for ch in range(6):
                    dp = dpad[0]; da = dacc[0]
                    for tb in range(4):
                        p = self.ps()
                        for k in range(8):
                            self.MM(p[:, :], wdn[:, k, ch * 128:(ch + 1) * 128], xTs[:, k, tb * 512:(tb + 1) * 512], k == 0, k == 7, [wdn, xTs], [p])
                        self.CP("act", dp[:, 2 + tb * 512:2 + (tb + 1) * 512], p[:, :], [p], [dp])
                    self.TS("dve", da[:], dp[:, 0:SEQ], dcp[:, ch, 0:1], None, ALU.mult, None, [dp, dcp], [da])
                    for w in range(1, 5):
                        self.STT(da[:], dp[:, w:w + SEQ], dcp[:, ch, w:w + 1], da[:], ALU.mult, ALU.add, [dp, dcp, da], [da])
                    self.ACT(da[:], da[:], AF.Silu, [da], [da])
                    for tt in range(NCH):
                        p = self.ps()
                        self.TR(p[:, 0:128], da[:, tt * 128:(tt + 1) * 128], ident[:], [da, ident], [p])
                        if ch < 4:
                            self.CP("dve" if tt % 2 else "act", qf32[:, tt, ch * 128:(ch + 1) * 128], p[:, 0:128], [p], [qf32])
                        else:
                            self.CP("dve" if tt % 2 else "act", qkv[:, tt, ch * 128:(ch + 1) * 128], p[:, 0:128], [p], [qkv])
                for tt in range(NCH):
                    p = self.ps()
                    for k in range(8):
                        self.MM(p[:, 0:272], xTs[:, k, tt * 128:(tt + 1) * 128], wz[:, k, :], k == 0, k == 7, [xTs, wz], [p])
                    self.CP("dve" if tt % 2 else "act", zba[:, tt, :], p[:, 0:272], [p], [zba])
                sqt = [self.tile(es1, [128, 512], F32, "dsqt") for _ in range(2)]
                ss = self.tile(es1, [128, NCH, 8], F32, "dss")
                for tt in range(NCH):
                    self.ACT(sqt[tt % 2][:], qf32[:, tt, :], AF.Square, [qf32], [sqt[tt % 2]])
                    self.RED(ss[:, tt, :], sqt[tt % 2][:].rearrange("p (h d) -> p h d", h=8), ALU.add, [sqt[tt % 2]], [ss])
                self.rstd(ss[:], ss[:], [ss, self.epsc], [ss], self.epsc[:, 1:2])
                self.TS("dve", ss[:, :, 0:4], ss[:, :, 0:4], 0.125, None, ALU.mult, None, [ss], [ss])
                for tt in range(NCH):
                    self.TT_("dve", qf32[:, tt, :].rearrange("p (h d) -> p h d", h=8), qf32[:, tt, :].rearrange("p (h d) -> p h d", h=8),
                             ss[:, tt, :].unsqueeze(2).to_broadcast([128, 8, 64]), ALU.mult, [qf32, ss], [qf32])
                self.CP("pool", qkv[:, :, 0:512], qf32[:], [qf32], [qkv])
                for tt in range(NCH):
                    for i in range(4):
                        p = self.ps()
                        self.TR(p[:, 0:128], qf32[:, tt, i * 128:(i + 1) * 128], ident[:], [qf32, ident], [p])
                        self.CP("dve" if i % 2 else "act", qkfm[:, i, tt, :], p[:, 0:128], [p], [qkfm])
                self.ACT(gb[:, :, 0:8], zba[:, :, 256:264], AF.Sigmoid, [zba], [gb])
                self.TT_("dve", gb[:, :, 8:16], zba[:, :, 264:272], rows[:, 8:16].unsqueeze(1).to_broadcast([128, NCH, 8]), ALU.add, [zba, rows], [gb])
                self.ACT(gb[:, :, 8:16], gb[:, :, 8:16], AF.Exp, [gb], [gb])
                self.ACT(gb[:, :, 8:16], gb[:, :, 8:16], AF.Ln, [gb], [gb], bias=1.0)
                self.ACT(rows[:, 0:8], rows[:, 0:8], AF.Exp, [rows], [rows])
                self.STT(gb[:, :, 8:16], gb[:, :, 8:16], -1.0, rows[:, 0:8].unsqueeze(1).to_broadcast([128, NCH, 8]), ALU.mult, ALU.mult, [gb, rows], [gb])
                S.barrier()
            import os as _os
            kdn = _os.environ.get("KDN", "")
            if kdn == "A":
                return
            ng = self.tile(es, [128, 64], F32, "dng")
            S.dma("sp", ng[:], dr["dn_norm_g"][l:l + 1, :].partition_broadcast(128), [], [ng])
            sz = zba
            self.ACT(zba[:, :, 0:256], zba[:, :, 0:256], AF.Silu, [zba], [zba])
            ydst = self.tile(es, [128, 2, SEQ], BF16, "ydst")
            MT = [self.tile(es, [64, NCH, 64], BF16, "MT") for _ in range(2)]
            Bm = [self.tile(es, [64, NCH, 64], F32, "Bm") for _ in range(2)]
            PTs = [self.tile(es, [64, NCH, 128], BF16, "PTs") for _ in range(2)]
            QKm = [self.tile(es, [128, NCH, 128], BF16, "QKm") for _ in range(2)]
            Ua = [self.tile(es, [128, NCH, 64], BF16, "Ua") for _ in range(2)]
            Sa = [self.tile(es, [64, NCH, 64], BF16, "Sa") for _ in range(2)]
            G = 4
            NR = G

            def wt(shape, dt, name):
                return [self.tile(es, shape, dt, name) for _ in range(NR)]
            gL = wt([128, 128], F32, "gL"); sc = wt([128, 8], F32, "dsc"); E = wt([128, 128], F32, "dE")
            Gi = wt([128, 128], F32, "Gi"); Gs = wt([128, 128], F32, "Gs")
            PP = [wt([128, 256], F32, "PP%d" % i) for i in range(2)]
            Z = [wt([128, 128], F32, "Z%d" % i) for i in range(2)]
            UW = wt([128, 128], BF16, "UW"); nW = wt([128, 64], BF16, "nW")
            Kd = wt([128, 64], BF16, "Kd"); Qd = wt([128, 64], BF16, "Qd")
            osum = self.tile(es, [128, 2, NCH], F32, "dosum")
            ojunk = self.tile(es, [128, 2, NCH * 64], F32, "dojunk")

            def lockstep(gens):
                gens = list(gens)
                while gens:
                    alive = []
                    for g_ in gens:
                        try:
                            next(g_)
                            alive.append(g_)
                        except StopIteration:
                            pass
                    gens = alive

            def chain(h, d, n, r):
                hb = (h % 2) * 64
                qi_ = h // 2
                Ld = masks[:, d, :]
                Mi = masks[:, 2 + 2 * d, :]
                Ms = masks[:, 3 + 2 * d, :]
                lastc = 127 if d == 0 else 0
                gcol = gb[:, n, 8 + d * 4 + h:8 + d * 4 + h + 1]
                bcol = gb[:, n, d * 4 + h:d * 4 + h + 1]
                kfm = qkfm[hb:hb + 64, 2 + qi_, n, :]
                qfm = qkfm[hb:hb + 64, qi_, n, :]
                q_tm = qkv[:, n, h * 64:(h + 1) * 64]
                k_tm = qkv[:, n, 256 + h * 64:256 + (h + 1) * 64]
                v_tm = qkv[:, n, 512 + h * 64:512 + (h + 1) * 64]
                s_ = sc[r]
                P0 = PP[0][r]
                self.TS("pool", gL[r][:], Ld, gcol, None, ALU.mult, None, [masks, gb], [gL[r]])
                yield
                pg = self.ps()
                self.MM(pg[:, 0:128], self.onesm[:], gL[r][:], True, True, [self.onesm, gL[r]], [pg])
                self.MM(pg[:, 128:129], Ld, gcol, True, True, [masks, gb], [pg])
                pk = self.ps()
                self.MM(pk[:, 0:128], kfm, kfm, True, True, [qkfm], [pk])
                self.MM(pk[:, 128:256], kfm, qfm, True, True, [qkfm], [pk])
                yield
                self.TS("dve", s_[:, 0:1], pg[:, 128:129], -1.0, None, ALU.mult, None, [pg], [s_])
                self.CP("dve", s_[:, 4:5], pg[:, lastc:lastc + 1], [pg], [s_])
                self.TS("dve", E[r][:], pg[:, 0:128], s_[:, 0:1], 0.0, ALU.add, ALU.min, [pg, s_], [E[r]])
                yield
                self.ACT(E[r][:], E[r][:], AF.Exp, [E[r]], [E[r]])
                self.ACT(s_[:, 1:2], s_[:, 0:1], AF.Exp, [s_], [s_], scale=-1.0)
                self.ACT(s_[:, 2:3], s_[:, 4:5], AF.Exp, [s_], [s_], bias=s_[:, 0:1], scale=1.0)
                self.ACT(s_[:, 3:4], s_[:, 4:5], AF.Exp, [s_], [s_])
                yield
                self.TT_("pool", Gs[r][:], E[r][:], Ms, ALU.mult, [E[r], masks], [Gs[r]])
                self.TT_("pool", Gi[r][:], E[r][:], Mi, ALU.mult, [E[r], masks], [Gi[r]])
                self.CP("pool", Z[0][r][:, 0:64], v_tm, [qkv], [Z[0][r]])
                self.TS("pool", Z[0][r][:, 64:128], k_tm, s_[:, 1:2], None, ALU.mult, None, [qkv, s_], [Z[0][r]])
                self.TS("pool", Kd[r][:], k_tm, s_[:, 2:3], None, ALU.mult, None, [qkv, s_], [Kd[r]])
                self.TS("pool", Qd[r][:], q_tm, s_[:, 1:2], None, ALU.mult, None, [qkv, s_], [Qd[r]])
                yield
                self.STT(P0[:, 0:128], pk[:, 0:128], bcol, Gs[r][:], ALU.mult, ALU.mult, [pk, gb, Gs[r]], [P0])
                self.TT_("dve", QKm[d][:, n, :], pk[:, 128:256], Gi[r][:], ALU.mult, [pk, Gi[r]], [QKm[d]])
                yield
                px = self.ps()
                self.TR(px[:, 0:128], P0[:, 0:128], ident[:], [P0, ident], [px])
                yield
                self.CP("act", P0[:, 128:256], px[:, 0:128], [px], [P0])
                yield
                zc = 0
                for k in range(7):
                    cur = PP[k % 2][r]; nxt = PP[(k + 1) % 2][r]
                    pz = self.ps()
                    self.MM(pz[:, 0:128], cur[:, 0:128], Z[zc][r][:], True, True, [cur, Z[zc][r]], [pz])
                    if k < 6:
                        pp = self.ps()
                        self.MM(pp[:, 0:128], cur[:, 128:256], cur[:, 0:128], True, True, [cur], [pp])
                        if k < 5:
                            self.MM(pp[:, 128:256], cur[:, 0:128], cur[:, 128:256], True, True, [cur], [pp])
                    yield
                    self.TT_("dve", Z[1 - zc][r][:], Z[zc][r][:], pz[:, 0:128], ALU.subtract if k == 0 else ALU.add, [Z[zc][r], pz], [Z[1 - zc][r]])
                    zc = 1 - zc
                    if k < 5:
                        self.CP("act", nxt[:, 0:256], pp[:, 0:256], [pp], [nxt])
                    elif k == 5:
                        self.CP("act", nxt[:, 0:128], pp[:, 0:128], [pp], [nxt])
                    yield
                Zf = Z[zc][r]
                self.TS("dve", UW[r][:], Zf[:], bcol, None, ALU.mult, None, [Zf, gb], [UW[r]])
                yield
                self.TS("pool", nW[r][:], UW[r][:, 64:128], -1.0, None, ALU.mult, None, [UW[r]], [nW[r]])
                self.CP("pool", Ua[d][:, n, :], UW[r][:, 0:64], [UW[r]], [Ua[d]])
                pm = self.ps()
                self.MM(pm[0:64, 0:64], UW[r][:, 64:128], Kd[r][:], True, True, [UW[r], Kd[r]], [pm])
                self.MM(pm[0:64, 64:128], Kd[r][:], UW[r][:, 0:64], True, True, [UW[r], Kd[r]], [pm])
                yield
                self.STT(MT[d][:, n, :], ident[0:64, 0:64], s_[0:64, 3:4], pm[0:64, 0:64], ALU.mult, ALU.subtract, [ident, s_, pm], [MT[d]])
                self.CP("dve", Bm[d][:, n, :], pm[0:64, 64:128], [pm], [Bm[d]])
                pq = self.ps()
                self.MM(pq[0:64, 0:128], Qd[r][:], identb[:], True, False, [Qd[r], identb], [pq])
                self.MM(pq[0:64, 0:128], nW[r][:], QKm[d][:, n, :], False, True, [nW[r], QKm[d]], [pq])
                yield
                self.CP("dve", PTs[d][:, n, :], pq[0:64, 0:128], [pq], [PTs[d]])

            def scan(d):
                order = list(range(NCH)) if d == 0 else list(range(NCH - 1, -1, -1))
                self.MEMSET("pool", Sa[d][:, order[0], :], 0.0, [Sa[d]])
                yield
                for i in range(NCH - 1):
                    n = order[i]; nn = order[i + 1]
                    pss = self.ps()
                    self.MM(pss[0:64, 0:64], MT[d][:, n, :], Sa[d][:, n, :], True, True, [MT[d], Sa[d]], [pss])
                    yield
                    self.TT_("dve", Sa[d][:, nn, :], pss[0:64, 0:64], Bm[d][:, n, :], ALU.add, [pss, Bm[d]], [Sa[d]])
                    yield

            for h in range(4):
                for d in range(2):
                    for n0 in range(0, NCH, G):
                        lockstep([chain(h, d, n0 + g_, g_) for g_ in range(G)])
                lockstep([scan(0), scan(1)])
                oc = ojunk[:, 0, :]; sq = ojunk[:, 1, :]
                for half in range(2):
                    po = self.ps()
                    for j in range(8):
                        n = half * 8 + j
                        for d in range(2):
                            self.MM(po[:, j * 64:(j + 1) * 64], PTs[d][:, n, :], Sa[d][:, n, :], d == 0, False, [PTs[d], Sa[d]], [po])
                            self.MM(po[:, j * 64:(j + 1) * 64], QKm[d][:, n, :], Ua[d][:, n, :], False, d == 1, [QKm[d], Ua[d]], [po])
                    self.CP("dve", ojunk[:, 0, half * 512:(half + 1) * 512], po[:, :], [po], [ojunk])
                oc3 = oc.rearrange("p (n e) -> p n e", e=64)
                self.ACT(sq, oc, AF.Square, [ojunk], [ojunk])
                self.RED(osum[:, 0, :], sq.rearrange("p (n e) -> p n e", e=64), ALU.add, [ojunk], [osum])
                self.ACT(osum[:, 1, :], osum[:, 0, :], AF.Sqrt, [osum, self.epsc], [osum], bias=self.epsc[:, 0:1], scale=1.0 / 64.0)
                self.RECIP(osum[:, 1, :], osum[:, 1, :], [osum], [osum])
                self.TT_("dve", oc3, oc3, osum[:, 1, :].unsqueeze(2).to_broadcast([128, NCH, 64]), ALU.mult, [ojunk, osum], [ojunk])
                self.TT_("dve", oc3, oc3, ng[:].unsqueeze(1).to_broadcast([128, NCH, 64]), ALU.mult, [ojunk, ng], [ojunk])
                yv = sz[:, :, h * 64:(h + 1) * 64]
                self.TT_("dve", yv, yv, oc3, ALU.mult, [sz, ojunk], [sz])
            for n in range(NCH):
                for c in range(2):
                    p = self.ps()
                    self.TR(p[:, 0:128], sz[:, n, c * 128:(c + 1) * 128], ident[:], [sz, ident], [p])
                    self.CP("act" if c else "dve", ydst[:, c, n * 128:(n + 1) * 128], p[:, 0:128], [p], [ydst])
            for c in range(2):
                S.dma("sp", self.ycT[:, 6 + c, self.t0:self.t0 + SEQ], ydst[:, c, :], [ydst], [])

    def out_proj(self, l, s, ycat):
        S = self.S
        dr = self.dr
        moe = (l % 2 == 1)
        with contextlib.ExitStack() as es:
            wo = self.tile(es, [128, 8, D], BF16, "wo")
            S.dma("pool", wo[:], dr["w_o"][l].rearrange("(k p) m -> p k m", p=128), [], [wo])
            gt = self.tile(es, [128, D], F32, "l1g"); bt = self.tile(es, [128, D], F32, "l1b")
            S.dma("sp", gt[:], dr["ln1_g"][l:l + 1, :].partition_broadcast(128), [], [gt])
            S.dma("sp", bt[:], dr["ln1_b"][l:l + 1, :].partition_broadcast(128), [], [bt])
            router = None
            if moe and "nortile" not in self.kgate:
                router = self.tile(es, [128, 8, NEXP], F32, "wr")
                if "nordma" in self.kgate:
                    self.MEMSET("pool", router[:], 0.0, [router])
                else:
                    S.dma("sp", router[:], dr["router_w"][l // 2], [], [router])
            GT = 4
            xin = [self.tile(es, [128, D], F32, "xin") for _ in range(GT)]
            tt_ = [self.tile(es, [128, D], F32, "tsum") for _ in range(GT)]
            xo = [self.tile(es, [128, D], F32, "xo") for _ in range(GT)]
            st = [self.tile(es, [128, 8], F32, "lnst") for _ in range(GT)]
            src = dr["x"] if l == 0 else self.xres

            def tile_gen(i, r):
                tg = s * (SEQ // 128) + i
                xi = xin[r]; t = tt_[r]; o = xo[r]
                S.dma("sp", xi[:], src[tg * 128:(tg + 1) * 128, :], [], [xi])
                yield
                for half in range(2):
                    p = self.ps()
                    for k in range(8):
                        self.MM(p[:, :], ycat[:, k, i * 128:(i + 1) * 128], wo[:, k, half * 512:(half + 1) * 512], k == 0, k == 7, [ycat, wo], [p])
                    self.STT(t[:, half * 512:(half + 1) * 512], xi[:, half * 512:(half + 1) * 512], ALPHA, p[:, :], ALU.mult, ALU.add, [xi, p], [t])
                yield
                yield from self.layer_norm_gen(t, o, gt, bt, st[r])
                S.dma("sp", self.xres[tg * 128:(tg + 1) * 128, :], o[:], [o], [])
                self.emit_xT(es, o, tg, router, l)

            for i0 in range(0, SEQ // 128, GT):
                self.lockstep([tile_gen(i0 + g_, g_) for g_ in range(GT)])

    def ffn(self, l, last):
        S = self.S
        dr = self.dr
        moe = (l % 2 == 1)
        j = l // 2
        NT = self.ntok
        TB = 1024
        G = 4
        nff = (DFFE if moe else DFF) // 128
        groups = [(c0, min(G, nff - c0)) for c0 in range(0, nff, G)]
        nexp = NEXP if moe else 1
        with contextlib.ExitStack() as es:
            gt = self.tile(es, [128, D], F32, "l2g"); bt = self.tile(es, [128, D], F32, "l2b")
            S.dma("sp", gt[:], dr["ln2_g"][l:l + 1, :].partition_broadcast(128), [], [gt])
            S.dma("sp", bt[:], dr["ln2_b"][l:l + 1, :].partition_broadcast(128), [], [bt])
            xTb = self.tile(es, [128, 8, TB], BF16, "xTb")
            hT = [self.tile(es, [128, G, TB], BF16, "hT") for _ in range(2)]
            w1g = [self.tile(es, [128, 8, G * 128], BF16, "w1g") for _ in range(2)]
            w3g = [self.tile(es, [128, 8, G * 128], BF16, "w3g") for _ in range(2)]
            w2g = [self.tile(es, [128, G, D], BF16, "w2g") for _ in range(2)]
            acc = self.tile(es, [128, TB // 128, D], F32, "facc")
            sil = [self.tile(es, [128, 512], F32, "sil") for _ in range(3)]
            GT = 4
            xin = [self.tile(es, [128, D], F32, "fxin") for _ in range(GT)]
            xo = [self.tile(es, [128, D], F32, "fxo") for _ in range(GT)]
            st = [self.tile(es, [128, 8], F32, "flnst") for _ in range(GT)]
            gi = 0
            si = 0
            for tb in range(NT // TB):
                t0 = tb * TB
                for k in range(8):
                    S.dma("sp", xTb[:, k, :], self.xT[:, k, t0:t0 + TB], [], [xTb])
                first = True
                for e in range(nexp):
                    if moe:
                        W1 = dr["moe_w1"][j, e]; W3 = dr["moe_w3"][j, e]; W2 = dr["moe_w2"][j, e]
                    else:
                        W1 = dr["ffn_w1"][j]; W3 = dr["ffn_w3"][j]; W2 = dr["ffn_w2"][j]
                    for (c0, gn) in groups:
                        r = gi % 2
                        gi += 1
                        S.dma("pool", w1g[r][:, :, 0:gn * 128], W1[:, c0 * 128:(c0 + gn) * 128].rearrange("(k p) m -> p k m", p=128), [], [w1g[r]])
                        S.dma("pool", w3g[r][:, :, 0:gn * 128], W3[:, c0 * 128:(c0 + gn) * 128].rearrange("(k p) m -> p k m", p=128), [], [w3g[r]])
                        S.dma("pool", w2g[r][:, 0:gn, :], W2[c0 * 128:(c0 + gn) * 128, :].rearrange("(c p) m -> p c m", p=128), [], [w2g[r]])
                        for c in range(gn):
                            for hb in range(TB // 512):
                                ts_ = slice(hb * 512, (hb + 1) * 512)
                                pa = self.ps(); pb = self.ps()
                                for k in range(8):
                                    self.MM(pa[:, :], w1g[r][:, k, c * 128:(c + 1) * 128], xTb[:, k, ts_], k == 0, k == 7, [w1g[r], xTb], [pa])
                                for k in range(8):
                                    self.MM(pb[:, :], w3g[r][:, k, c * 128:(c + 1) * 128], xTb[:, k, ts_], k == 0, k == 7, [w3g[r], xTb], [pb])
                                sl_ = sil[si % 3]
                                si += 1
                                self.ACT(sl_[:], pa[:, :], AF.Silu, [pa], [sl_])
                                self.TT_("dve", hT[r][:, c, ts_], pb[:, :], sl_[:], ALU.mult, [pb, sl_], [hT[r]])
                        for tt in range(TB // 128):
                            tg = tb * (TB // 128) + tt
                            for half in range(2):
                                p = self.ps()
                                for c in range(gn):
                                    self.MM(p[:, :], hT[r][:, c, tt * 128:(tt + 1) * 128], w2g[r][:, c, half * 512:(half + 1) * 512], c == 0, c == gn - 1, [hT[r], w2g[r]], [p])
                                a_ = acc[:, tt, half * 512:(half + 1) * 512]
                                if moe:
                                    cw = self.comb[:, tg, e:e + 1]
                                    if first:
                                        self.TS("dve", a_, p[:, :], cw, None, ALU.mult, None, [p, self.comb], [acc])
                                    else:
                                        self.STT(a_, p[:, :], cw, a_, ALU.mult, ALU.add, [p, self.comb, acc], [acc])
                                else:
                                    if first:
                                        self.CP("dve", a_, p[:, :], [p], [acc])
                                    else:
                                        self.TT_("dve", a_, p[:, :], a_, ALU.add, [p, acc], [acc])
                        first = False
                def ep_gen(tt, r, tb=tb):
                    tg = tb * (TB // 128) + tt
                    xi = xin[r]; o = xo[r]
                    S.dma("sp", xi[:], self.xres[tg * 128:(tg + 1) * 128, :], [], [xi])
                    yield
                    self.STT(xi[:], xi[:], ALPHA, acc[:, tt, :], ALU.mult, ALU.add, [xi, acc], [xi])
                    yield
                    yield from self.layer_norm_gen(xi, o, gt, bt, st[r])
                    if last:
                        S.dma("sp", self.y_out[tg * 128:(tg + 1) * 128, :], o[:], [o], [])
                    else:
                        S.dma("sp", self.xres[tg * 128:(tg + 1) * 128, :], o[:], [o], [])
                        self.emit_xT(es, o, tg, None, l)

                for t0_ in range(0, TB // 128, GT):
                    self.lockstep([ep_gen(t0_ + g_, g_) for g_ in range(GT)])
            S.barrier()


def _consts():
    ident = np.eye(128, dtype=np.float32)
    p = np.arange(128)[:, None]
    f = np.arange(128)[None, :]
    Lf = (p <= f).astype(np.float32)
    Lb = (p >= f).astype(np.float32)
    masks = np.stack([Lf, Lb, (p <= f), (p < f), (p >= f), (p > f)]).astype(np.float32)
    inv = (10000.0 ** (-np.arange(0, 64, 2, dtype=np.float32) / 64.0)).astype(np.float32)
    ang = np.arange(SEQ, dtype=np.float32)[:, None] * inv[None, :]
    cos = np.cos(ang).astype(np.float32).T
    sin = np.sin(ang).astype(np.float32).T
    cosT = np.concatenate([cos, cos, cos, cos], 0)
    sinT = np.concatenate([-sin, sin, -sin, sin], 0)
    rope = np.stack([cosT, sinT]).astype(np.float32)
    return ident, masks, rope


_CACHE = {}
_LAST = None


def run(inputs, n_cores=8, n_layers=DEPTH, nseq=2):
    key = (n_cores, n_layers, nseq)
    nc = bass.Bass("TRN2", target_bir_lowering=False)
    Builder(nc, nseq, n_layers).build()
    ident, masks, rope = _consts()
    f = lambda a: np.ascontiguousarray(np.asarray(a, dtype=np.float32))
    x = f(inputs["x"])
    cdw = f(inputs["conv_dw"])
    cp = np.concatenate([cdw.transpose(0, 2, 1), f(inputs["conv_dw_b"])[:, :, None], f(inputs["conv_ln_g"])[:, :, None],
                         f(inputs["conv_ln_b"])[:, :, None]], axis=2)
    cp = np.ascontiguousarray(cp.reshape(DEPTH, 2, 128, 34).transpose(0, 2, 1, 3))
    dcp = f(inputs["dn_conv"]).transpose(0, 2, 1)
    dcp = np.ascontiguousarray(dcp.reshape(DEPTH, 6, 128, 5).transpose(0, 2, 1, 3))
    shared = {
        "w_in": f(inputs["w_in"]), "w_o": f(inputs["w_o"]),
        "ln1_g": f(inputs["ln1_g"]), "ln1_b": f(inputs["ln1_b"]), "ln2_g": f(inputs["ln2_g"]), "ln2_b": f(inputs["ln2_b"]),
        "conv_cp": cp, "conv_pw": f(inputs["conv_pw"]),
        "diff_lambda": f(inputs["diff_lambda"]).reshape(DEPTH, 256), "diff_subln_g": f(inputs["diff_subln_g"]),
        "dn_cp": dcp, "dn_a_log": f(inputs["dn_a_log"]).reshape(DEPTH, 8), "dn_dt_bias": f(inputs["dn_dt_bias"]).reshape(DEPTH, 8),
        "dn_norm_g": f(inputs["dn_norm_g"]),
        "c_ident": ident, "c_masks": masks, "c_rope": rope,
    }
    nd = (n_layers + 1) // 2
    nm = n_layers // 2
    for k_ in ("ffn_w1", "ffn_w3", "ffn_w2"):
        shared[k_] = f(inputs[k_])[:nd]
    if nm > 0:
        for k_ in ("moe_w1", "moe_w3", "moe_w2"):
            shared[k_] = f(inputs[k_])[:nm]
        shared["router_w"] = np.ascontiguousarray(f(inputs["router_w"])[:nm].reshape(nm, 8, 128, NEXP).transpose(0, 2, 1, 3))
    in_maps = []
    for c in range(n_cores):
        m = dict(shared)
        m["x"] = np.ascontiguousarray(x[c * nseq:(c + 1) * nseq].reshape(nseq * SEQ, D))
        in_maps.append(m)
    import os as _os
    if _os.environ.get("KTRACE"):
        res = run_bass_kernel_spmd(nc, in_maps, core_ids=list(range(n_cores)), trace=True)
        print("EXEC_TIME_NS", res.exec_time_ns, flush=True)
    else:
        res = run_bass_kernel_spmd(nc, in_maps, core_ids=list(range(n_cores)))
    global _LAST
    _LAST = res.results
    out = np.stack([res.results[c]["y"].reshape(nseq, SEQ, D) for c in range(n_cores)], 0)
    return out.reshape(n_cores * nseq, SEQ, D).astype(np.float32)


def kernel(**inputs):
    return run(inputs, n_cores=8, n_layers=DEPTH, nseq=2)
```

```python
import contextlib
import math
import numpy as np
import concourse.bass as bass
import concourse.mybir as mybir
from concourse.bass_utils import run_bass_kernel_spmd

F32 = mybir.dt.float32
BF16 = mybir.dt.bfloat16
AF = mybir.ActivationFunctionType
ALU = mybir.AluOpType
AX = mybir.AxisListType

D = 1024
SEQ = 2048
DEPTH = 4
PROJ = 3088
DFF = 2816
DFFE = 3584
NEXP = 8
ALPHA = (2 * DEPTH) ** 0.25
EPS = 1e-5
C_CONV, C_Q, C_K, C_V, C_DN, C_Z, C_B, C_A = 0, 512, 1024, 1536, 2048, 2816, 3072, 3080


class Buf:
    __slots__ = ("w", "r")

    def __init__(self):
        self.w = None
        self.r = {}


class TT:
    __slots__ = ("t", "b")

    def __init__(self, t):
        self.t = t
        self.b = Buf()

    def __getitem__(self, k):
        return self.t[k]


class Sched:
    ENGS = ("pe", "act", "dve", "pool", "sp")
    NDS = 48

    def __init__(self, nc, es):
        self.nc = nc
        self.sems = {e: es.enter_context(nc.semaphore("pg_" + e)) for e in self.ENGS}
        self.cnt = {e: 0 for e in self.ENGS}
        self.seen = {e: {} for e in self.ENGS}
        self.ops = {e: [] for e in self.ENGS}
        self.dsems = [es.enter_context(nc.semaphore("dq%d" % i)) for i in range(self.NDS)]
        self.dval = [0] * self.NDS
        self.dnext = 0
        self.same_engine_sync = True

    def _collect(self, eng, reads, writes):
        deps = {}

        def add(tok):
            if tok is None:
                return
            k, v = tok
            if k == eng and (eng == "pe" or not self.same_engine_sync):
                return
            if deps.get(k, 0) < v:
                deps[k] = v

        for b in reads:
            add(b.w)
        for b in writes:
            add(b.w)
            for k, v in b.r.items():
                add((k, v))
        waits = []
        seen = self.seen[eng]
        for k, v in deps.items():
            if seen.get(k, 0) >= v:
                continue
            seen[k] = v
            waits.append((k, v))
        return waits

    def _semof(self, k):
        return self.sems[k] if isinstance(k, str) else self.dsems[k]

    def op(self, eng, fn, reads=(), writes=()):
        reads = [x.b if isinstance(x, TT) else x for x in reads]
        writes = [x.b if isinstance(x, TT) else x for x in writes]
        waits = self._collect(eng, reads, writes)
        self.cnt[eng] += 1
        tok = (eng, self.cnt[eng])
        self.ops[eng].append((waits, fn, (self.sems[eng], 1)))
        for b in writes:
            b.w = tok
            b.r = {}
        for b in reads:
            if b.r.get(eng, 0) < tok[1]:
                b.r[eng] = tok[1]
        return tok

    def dma(self, q, out, in_, reads=(), writes=(), slow=False):
        reads = [x.b if isinstance(x, TT) else x for x in reads]
        writes = [x.b if isinstance(x, TT) else x for x in writes]
        waits = self._collect(q, reads, writes)
        i = self.dnext
        self.dnext = (self.dnext + 1) % self.NDS
        seen = self.seen[q]
        if self.dval[i] > 0 and seen.get(i, 0) < self.dval[i]:
            seen[i] = self.dval[i]
            waits.append((i, self.dval[i]))
        self.dval[i] += 16
        tok = (i, self.dval[i])
        if slow:
            fn = lambda e, o=out, a=in_: e.dma_start(out=o, in_=a, allow_slow_non_contiguous=True)
        else:
            fn = lambda e, o=out, a=in_: e.dma_start(out=o, in_=a)
        self.ops[q].append((waits, fn, (self.dsems[i], 16)))
        for b in writes:
            b.w = tok
            b.r = {}
        for b in reads:
            if b.r.get(i, 0) < tok[1]:
                b.r[i] = tok[1]
        return tok

    def barrier(self):
        for e in self.ENGS:
            waits = []
            seen = self.seen[e]
            for k in self.ENGS:
                if k != e and self.cnt[k] > seen.get(k, 0):
                    seen[k] = self.cnt[k]
                    waits.append((k, self.cnt[k]))
            for i in range(self.NDS):
                if self.dval[i] > seen.get(i, 0):
                    seen[i] = self.dval[i]
                    waits.append((i, self.dval[i]))
            if waits:
                self.ops[e].append((waits, None, None))

    def finish(self, es):
        nc = self.nc
        block = es.enter_context(nc.Block())

        def run(ename):
            def body(e):
                for waits, fn, inc in self.ops[ename]:
                    for k, v in waits:
                        e.wait_ge(self._semof(k), v)
                    if fn is not None:
                        fn(e).then_inc(inc[0], inc[1])
            return body

        block.tensor(run("pe"))
        block.scalar(run("act"))
        block.vector(run("dve"))
        block.gpsimd(run("pool"))
        block.sync(run("sp"))


class Builder:
    def __init__(self, nc, nseq, n_layers):
        self.nc = nc
        self.nseq = nseq
        self.ntok = nseq * SEQ
        self.n_layers = n_layers
        self.uid = 0

    def tile(self, es, shape, dt=F32, name="t"):
        self.uid += 1
        return TT(es.enter_context(self.nc.sbuf_tensor("%s_%d" % (name, self.uid), list(shape), dt)))

    def ps(self):
        p = self.psum[self.psi]
        self.psi = (self.psi + 1) % len(self.psum)
        return p

    def MM(self, out, lhsT, rhs, start, stop, rd, wr):
        self.S.op("pe", lambda e: e.matmul(out, lhsT=lhsT, rhs=rhs, start=start, stop=stop), rd, wr)

    def TR(self, out, in_, ident, rd, wr):
        self.S.op("pe", lambda e: e.transpose(out, in_, ident), rd, wr)

    def ACT(self, out, in_, func, rd, wr, bias=None, scale=None, accum=None):
        kw = {}
        if bias is not None:
            kw["bias"] = bias
        if scale is not None:
            kw["scale"] = scale
        if accum is not None:
            kw["accum_out"] = accum
        self.S.op("act", lambda e: e.activation(out=out, in_=in_, func=func, **kw), rd, wr)

    def TT_(self, eng, out, in0, in1, op, rd, wr):
        self.S.op(eng, lambda e: e.tensor_tensor(out=out, in0=in0, in1=in1, op=op), rd, wr)

    def TS(self, eng, out, in0, s1, s2, op0, op1, rd, wr):
        if op1 is None:
            self.S.op(eng, lambda e: e.tensor_scalar(out=out, in0=in0, scalar1=s1, scalar2=None, op0=op0), rd, wr)
        else:
            self.S.op(eng, lambda e: e.tensor_scalar(out=out, in0=in0, scalar1=s1, scalar2=s2, op0=op0, op1=op1), rd, wr)

    def STT(self, out, in0, scalar, in1, op0, op1, rd, wr):
        self.S.op("dve", lambda e: e.scalar_tensor_tensor(out=out, in0=in0, scalar=scalar, in1=in1, op0=op0, op1=op1), rd, wr)

    def CP(self, eng, out, in_, rd, wr):
        if eng == "act":
            self.S.op("act", lambda e: e.activation(out=out, in_=in_, func=AF.Copy), rd, wr)
        else:
            self.S.op(eng, lambda e: e.tensor_copy(out=out, in_=in_), rd, wr)

    def RED(self, out, in_, op, rd, wr):
        self.S.op("dve", lambda e: e.tensor_reduce(out=out, in_=in_, axis=AX.X, op=op), rd, wr)

    def RECIP(self, out, in_, rd, wr):
        self.S.op("dve", lambda e: e.reciprocal(out=out, in_=in_), rd, wr)

    def MEMSET(self, eng, ap, val, wr):
        self.S.op(eng, lambda e: e.memset(ap, val), [], wr)

    def rstd(self, out, in_, rd, wr, eps_ap):
        self.ACT(out, in_, AF.Sqrt, rd, wr, bias=eps_ap, scale=1.0)
        self.RECIP(out, out, wr, wr)

    def dbg(self, name, src, ap, shape, dt=F32, psum=False):
        if not self.debug or name in self._dbg:
            return
        self._dbg.add(name)
        d = self.nc.dram_tensor("dbg_" + name, list(shape), dt, kind="ExternalOutput").ap()
        if psum:
            tmp = self.dbgtmp[len(self._dbg) % 4]
            self.CP("dve", tmp[0:shape[0], 0:shape[1]], ap, [src], [tmp])
            self.S.dma("sp", d, tmp[0:shape[0], 0:shape[1]], [tmp], [])
        else:
            self.S.dma("sp", d, ap, [src], [])

    def build(self):
        nc = self.nc
        NT = self.ntok
        L = self.n_layers
        dr = {}

        def din(name, shape, dt=F32):
            dr[name] = nc.dram_tensor(name, list(shape), dt, kind="ExternalInput").ap()
            return dr[name]

        x_in = din("x", [NT, D])
        w_in = din("w_in", [DEPTH, D, PROJ])
        w_o = din("w_o", [DEPTH, D, D])
        ln1_g = din("ln1_g", [DEPTH, D]); ln1_b = din("ln1_b", [DEPTH, D])
        ln2_g = din("ln2_g", [DEPTH, D]); ln2_b = din("ln2_b", [DEPTH, D])
        cp_in = din("conv_cp", [DEPTH, 128, 2, 34])
        conv_pw = din("conv_pw", [DEPTH, 256, 256])
        diff_lambda = din("diff_lambda", [DEPTH, 256])
        diff_subln_g = din("diff_subln_g", [DEPTH, 128])
        dcp_in = din("dn_cp", [DEPTH, 128, 6, 5])
        dn_a_log = din("dn_a_log", [DEPTH, 8])
        dn_dt_bias = din("dn_dt_bias", [DEPTH, 8])
        dn_norm_g = din("dn_norm_g", [DEPTH, 64])
        nd = (L + 1) // 2
        nm = L // 2
        ffn_w1 = din("ffn_w1", [nd, D, DFF]); ffn_w3 = din("ffn_w3", [nd, D, DFF]); ffn_w2 = din("ffn_w2", [nd, DFF, D])
        if nm > 0:
            router_w = din("router_w", [nm, 128, 8, NEXP])
            moe_w1 = din("moe_w1", [nm, NEXP, D, DFFE]); moe_w3 = din("moe_w3", [nm, NEXP, D, DFFE])
            moe_w2 = din("moe_w2", [nm, NEXP, DFFE, D])
        c_ident = din("c_ident", [128, 128])
        c_masks = din("c_masks", [6, 128, 128])
        c_rope = din("c_rope", [2, 128, SEQ])
        y_out = nc.dram_tensor("y", [NT, D], F32, kind="ExternalOutput").ap()
        import os as _os
        ik = "ExternalOutput" if _os.environ.get("KDBG") else "Internal"
        xres = nc.dram_tensor("xres", [NT, D], F32, kind=ik).ap()
        xT = nc.dram_tensor("xT", [128, 8, NT], BF16, kind=ik).ap()
        self.ycT = nc.dram_tensor("ycT", [128, 8, NT], BF16, kind=ik).ap()

        with contextlib.ExitStack() as es:
            S = self.S = Sched(nc, es)
            self.psum = [TT(es.enter_context(nc.psum_tensor("psb%d" % i, [128, 512], F32))) for i in range(8)]
            self.psi = 0
            ident = self.tile(es, [128, 128], F32, "ident")
            identb = self.tile(es, [128, 128], BF16, "identb")
            onesm = self.tile(es, [128, 128], F32, "onesm")
            ones256 = self.tile(es, [128, 128], F32, "ones256")
            epsc = self.tile(es, [128, 2], F32, "epsc")
            S.dma("sp", ident[:], c_ident, [], [ident])
            S.dma("pool", identb[:], c_ident, [], [identb])
            self.MEMSET("dve", onesm[:], 1.0, [onesm])
            self.MEMSET("dve", ones256[:], 1.0 / 256.0, [ones256])
            self.MEMSET("dve", epsc[:, 0:1], EPS, [epsc])
            self.MEMSET("dve", epsc[:, 1:2], 1e-6, [epsc])
            self.ident, self.identb, self.onesm, self.ones256, self.epsc = ident, identb, onesm, ones256, epsc
            self.comb = self.tile(es, [128, NT // 128, NEXP], F32, "comb")
            self._xtbufs = [self.tile(es, [128, 8, 128], BF16, "xtb") for _ in range(2)] + \
                           [self.tile(es, [128, 8, 128], F32, "xtf") for _ in range(2)] + \
                           [self.tile(es, [128, 16], F32, "rt") for _ in range(2)]
            self.dr = dr
            self.xres, self.xT, self.y_out = xres, xT, y_out
            self.debug = bool(_os.environ.get("KDBG"))
            self.kgate = _os.environ.get("KGATE", "")
            self.ks1 = _os.environ.get("KS1", "")
            self._dbg = set()
            if self.debug:
                self.dbgtmp = [self.tile(es, [128, 512], F32, "dbgtmp") for _ in range(4)]

            with contextlib.ExitStack() as es2:
                bufs = [self.tile(es2, [128, D], F32, "pxt") for _ in range(2)]
                for tt in range(NT // 128):
                    xt = bufs[tt % 2]
                    S.dma("sp", xt[:], x_in[tt * 128:(tt + 1) * 128, :], [], [xt])
                    self.emit_xT(es2, xt, tt, None, 0)
                S.barrier()

            import os as _os
            self.kstop = _os.environ.get("KSTOP", "")
            for l in range(L):
                last = (l == L - 1)
                if self.kstop == "pro":
                    break
                for s in range(self.nseq):
                    self.mixer(l, s)
                    if self.kstop:
                        break
                if self.kstop:
                    break
                if "noffn" in self.kgate and l % 2 == 1:
                    continue
                self.ffn(l, last)
                if "stop0" in self.kgate:
                    break
            S.barrier()
            S.finish(es)

    def emit_xT(self, es, xt, tt, router, l):
        S = self.S
        xtb = self._xtbufs[tt % 2]
        xtf = self._xtbufs[2 + tt % 2]
        rt = self._xtbufs[4 + tt % 2]
        for k in range(8):
            p = self.ps()
            self.TR(p[:, 0:128], xt[:, k * 128:(k + 1) * 128], self.ident[:], [xt, self.ident], [p])
            if router is not None:
                self.CP("act", xtf[:, k, :], p[:, 0:128], [p], [xtf])
                self.CP("pool", xtb[:, k, :], xtf[:, k, :], [xtf], [xtb])
            else:
                self.CP("act" if k % 2 else "dve", xtb[:, k, :], p[:, 0:128], [p], [xtb])
        S.dma("sp", self.xT[:, :, tt * 128:(tt + 1) * 128], xtb[:], [xtb], [])
        if router is not None and "norouter" not in self.kgate:
            wr = router
            p = self.ps()
            for k in range(8):
                self.MM(p[:, 0:8], xtf[:, k, :], wr[:, k, :], k == 0, k == 7, [xtf, wr], [p])
            lg = rt[:, 0:8]
            self.CP("dve", lg, p[:, 0:8], [p], [rt])
            m1 = rt[:, 8:9]; m2 = rt[:, 9:10]; tmp = rt[:, 10:11]
            cm = self.comb[:, tt, :]
            self.RED(m1, lg, ALU.max, [rt], [rt])
            self.TS("dve", cm, lg, m1, -1e30, ALU.is_equal, ALU.mult, [rt], [self.comb])
            self.TT_("dve", cm, cm, lg, ALU.add, [rt, self.comb], [self.comb])
            self.RED(m2, cm, ALU.max, [self.comb], [rt])
            self.TS("dve", cm, lg, m2, None, ALU.is_ge, None, [rt], [self.comb])
            self.TS("dve", tmp, m1, -1.0, None, ALU.mult, None, [rt], [rt])
            ex = rt[:, 11:12]
            self.ACT(ex, m2, AF.Exp, [rt], [rt], bias=tmp, scale=1.0)
            self.TS("dve", ex, ex, 1.0, None, ALU.add, None, [rt], [rt])
            self.RECIP(ex, ex, [rt], [rt])
            self.ACT(lg, lg, AF.Exp, [rt], [rt], bias=tmp, scale=1.0)
            self.STT(cm, lg, ex, cm, ALU.mult, ALU.mult, [rt, self.comb], [self.comb])

    def layer_norm_tile(self, t, out, gt, bt, st):
        self.RED(st[:, 0:1], t[:], ALU.add, [t], [st])
        self.TS("dve", st[:, 1:2], st[:, 0:1], -1.0 / D, None, ALU.mult, None, [st], [st])
        self.TS("dve", out[:], t[:], st[:, 1:2], None, ALU.add, None, [t, st], [out])
        self.ACT(t[:], out[:], AF.Square, [out], [t])
        self.RED(st[:, 2:3], t[:], ALU.add, [t], [st])
        self.ACT(st[:, 3:4], st[:, 2:3], AF.Sqrt, [st, self.epsc], [st], bias=self.epsc[:, 0:1], scale=1.0 / D)
        self.RECIP(st[:, 3:4], st[:, 3:4], [st], [st])
        self.STT(out[:], out[:], st[:, 3:4], gt[:], ALU.mult, ALU.mult, [out, st, gt], [out])
        self.TT_("pool", out[:], out[:], bt[:], ALU.add, [out, bt], [out])

    def layer_norm_gen(self, t, out, gt, bt, st):
        self.RED(st[:, 0:1], t[:], ALU.add, [t], [st])
        yield
        self.TS("dve", st[:, 1:2], st[:, 0:1], -1.0 / D, None, ALU.mult, None, [st], [st])
        yield
        self.TS("dve", out[:], t[:], st[:, 1:2], None, ALU.add, None, [t, st], [out])
        yield
        self.ACT(t[:], out[:], AF.Square, [out], [t])
        yield
        self.RED(st[:, 2:3], t[:], ALU.add, [t], [st])
        yield
        self.ACT(st[:, 3:4], st[:, 2:3], AF.Sqrt, [st, self.epsc], [st], bias=self.epsc[:, 0:1], scale=1.0 / D)
        yield
        self.RECIP(st[:, 3:4], st[:, 3:4], [st], [st])
        yield
        self.STT(out[:], out[:], st[:, 3:4], gt[:], ALU.mult, ALU.mult, [out, st, gt], [out])
        yield
        self.TT_("pool", out[:], out[:], bt[:], ALU.add, [out, bt], [out])
        yield

    @staticmethod
    def lockstep(gens):
        gens = list(gens)
        while gens:
            alive = []
            for g_ in gens:
                try:
                    next(g_)
                    alive.append(g_)
                except StopIteration:
                    pass
            gens = alive

    def mixer(self, l, s):
        S = self.S
        dr = self.dr
        t0 = s * SEQ
        lam_init = 0.8 - 0.6 * math.exp(-0.3 * l)
        w_in = dr["w_in"][l]
        self.t0 = t0
        with contextlib.ExitStack() as es:
            xTs = self.tile(es, [128, 8, SEQ], BF16, "xTs")
            for k in range(8):
                S.dma("sp", xTs[:, k, :], self.xT[:, k, t0:t0 + SEQ], [], [xTs])
            ks = self.kstop
            ks1 = self.ks1 if l >= 1 else ""
            if ks in ("", "conv", "out") and (not ks1 or "conv" in ks1):
                self.conv_group(l, xTs, None, w_in)
            S.barrier()
            if ks in ("", "attn", "out") and (not ks1 or "attn" in ks1):
                self.attn_group(l, xTs, None, w_in, lam_init)
            S.barrier()
            if ks in ("", "dn", "out") and (not ks1 or "dn" in ks1):
                self.dn_group(l, xTs, None, w_in)
            S.barrier()
        if self.kstop in ("conv", "attn", "dn"):
            return
        if ks1 and "out" not in ks1:
            return
        with contextlib.ExitStack() as es:
            ycat = self.tile(es, [128, 8, SEQ], BF16, "ycat")
            for k in range(8):
                S.dma("sp", ycat[:, k, :], self.ycT[:, k, t0:t0 + SEQ], [], [ycat])
            self.out_proj(l, s, ycat)
            S.barrier()

    def load_w(self, t, src, rd=()):
        self.S.dma("pool", t, src.rearrange("(k p) m -> p k m", p=128), list(rd), [])

    def conv_group(self, l, xTs, ycat, w_in):
        S = self.S
        dr = self.dr
        with contextlib.ExitStack() as es:
            wsec = self.tile(es, [128, 8, 512], BF16, "wconv")
            S.dma("pool", wsec[:], w_in[:, C_CONV:C_CONV + 512].rearrange("(k p) m -> p k m", p=128), [], [wsec])
            cp = self.tile(es, [128, 2, 34], F32, "cp")
            S.dma("sp", cp[:], dr["conv_cp"][l], [], [cp])
            wpw = self.tile(es, [128, 2, 256], BF16, "wpw")
            S.dma("pool", wpw[:], dr["conv_pw"][l].rearrange("(k p) m -> p k m", p=128), [], [wpw])
            cpad = [self.tile(es, [128, SEQ + 30], F32, "cpad") for _ in range(2)]
            acc = [self.tile(es, [128, SEQ], F32, "cacc") for _ in range(2)]
            accb = [self.tile(es, [128, SEQ], F32, "caccb") for _ in range(2)]
            sig = [self.tile(es, [128, 512], F32, "sig") for _ in range(2)]
            for j in range(2):
                self.MEMSET("pool", cpad[j][:, 0:15], 0.0, [cpad[j]])
                self.MEMSET("pool", cpad[j][:, SEQ + 15:SEQ + 30], 0.0, [cpad[j]])
            for j in range(2):
                for tb in range(4):
                    pl = self.ps(); pg = self.ps()
                    for k in range(8):
                        self.MM(pl[:, :], wsec[:, k, j * 128:(j + 1) * 128], xTs[:, k, tb * 512:(tb + 1) * 512], k == 0, k == 7, [wsec, xTs], [pl])
                    for k in range(8):
                        self.MM(pg[:, :], wsec[:, k, 256 + j * 128:256 + (j + 1) * 128], xTs[:, k, tb * 512:(tb + 1) * 512], k == 0, k == 7, [wsec, xTs], [pg])
                    sg = sig[tb % 2]
                    self.ACT(sg[:], pg[:, :], AF.Sigmoid, [pg], [sg])
                    self.TT_("dve", cpad[j][:, 15 + tb * 512:15 + (tb + 1) * 512], pl[:, :], sg[:], ALU.mult, [pl, sg], [cpad[j]])
            for j in range(2):
                a = acc[j]
                self.TS("dve", a[:], cpad[j][:, 0:SEQ], cp[:, j, 0:1], None, ALU.mult, None, [cpad[j], cp], [a])
                for w in range(1, 31):
                    self.STT(a[:], cpad[j][:, w:w + SEQ], cp[:, j, w:w + 1], a[:], ALU.mult, ALU.add, [cpad[j], cp, a], [a])
                self.TS("pool", accb[j][:], a[:], cp[:, j, 31:32], None, ALU.add, None, [a, cp], [accb[j]])
                self.ACT(a[:], accb[j][:], AF.Square, [accb[j]], [a])
            ysil = [[self.tile(es, [128, 512], BF16, "ysil") for _ in range(2)] for _ in range(2)]
            m2 = self.tile(es, [128, 512], F32, "m2")
            rs = self.tile(es, [128, 512], F32, "rs")
            tmp = [self.tile(es, [128, 512], F32, "ctmp") for _ in range(2)]
            yst = [self.tile(es, [128, 512], BF16, "cyst") for _ in range(4)]
            for tb in range(4):
                sl = slice(tb * 512, (tb + 1) * 512)
                pm = self.ps(); pq = self.ps()
                for j in range(2):
                    self.MM(pm[:, :], self.ones256[:], accb[j][:, sl], j == 0, j == 1, [self.ones256, accb[j]], [pm])
                for j in range(2):
                    self.MM(pq[:, :], self.ones256[:], acc[j][:, sl], j == 0, j == 1, [self.ones256, acc[j]], [pq])
                self.ACT(m2[:], pm[:, :], AF.Square, [pm], [m2])
                self.TT_("dve", rs[:], pq[:, :], m2[:], ALU.subtract, [pq, m2], [rs])
                self.rstd(rs[:], rs[:], [rs, self.epsc], [rs], self.epsc[:, 0:1])
                for j in range(2):
                    t = tmp[j]
                    self.TT_("dve", t[:], accb[j][:, sl], pm[:, :], ALU.subtract, [accb[j], pm], [t])
                    self.TT_("pool", t[:], t[:], rs[:], ALU.mult, [t, rs], [t])
                    self.ACT(ysil[tb % 2][j][:], t[:], AF.Silu, [t, cp], [ysil[tb % 2][j]], bias=cp[:, j, 33:34], scale=cp[:, j, 32:33])
                for co in range(2):
                    po = self.ps()
                    for j in range(2):
                        self.MM(po[:, :], wpw[:, j, co * 128:(co + 1) * 128], ysil[tb % 2][j][:], j == 0, j == 1, [wpw, ysil[tb % 2][j]], [po])
                    ys = yst[(tb * 2 + co) % 4]
                    self.CP("dve", ys[:], po[:, :], [po], [ys])
                    S.dma("sp", self.ycT[:, co, self.t0 + tb * 512:self.t0 + (tb + 1) * 512], ys[:], [ys], [])

    def attn_group(self, l, xTs, ycat, w_in, lam_init):
        S = self.S
        dr = self.dr
        with contextlib.ExitStack() as es:
            rope = self.tile(es, [128, 2, SEQ], F32, "rope")
            S.dma("sp", rope[:, 0, :], dr["c_rope"][0], [], [rope])
            S.dma("sp", rope[:, 1, :], dr["c_rope"][1], [], [rope])
            wv = self.tile(es, [128, 8, 512], BF16, "wv")
            S.dma("pool", wv[:], w_in[:, C_V:C_V + 512].rearrange("(k p) m -> p k m", p=128), [], [wv])
            lamt = self.tile(es, [128, 256], F32, "lamt")
            S.dma("sp", lamt[:], dr["diff_lambda"][l:l + 1, :].partition_broadcast(128), [], [lamt])
            subg = self.tile(es, [128, 128], F32, "subg")
            S.dma("sp", subg[:], dr["diff_subln_g"][l:l + 1, :].partition_broadcast(128), [], [subg])
            lsm = self.tile(es, [128, 8], F32, "lsm")
            prod = self.tile(es, [128, 2, 64], F32, "lprod")
            lv = lamt[:].rearrange("p (a d) -> p a d", a=4)
            self.TT_("dve", prod[:, 0, :], lv[:, 0, :], lv[:, 1, :], ALU.mult, [lamt], [prod])
            self.TT_("dve", prod[:, 1, :], lv[:, 2, :], lv[:, 3, :], ALU.mult, [lamt], [prod])
            self.RED(lsm[:, 0:2], prod[:], ALU.add, [prod], [lsm])
            self.ACT(lsm[:, 2:4], lsm[:, 0:2], AF.Exp, [lsm], [lsm])
            self.TT_("dve", lsm[:, 4:5], lsm[:, 3:4], lsm[:, 2:3], ALU.subtract, [lsm], [lsm])
            self.TS("dve", lsm[:, 4:5], lsm[:, 4:5], -lam_init, None, ALU.add, None, [lsm], [lsm])
            neglam = lsm[:, 4:5]
            self.TS("dve", subg[:], subg[:], 1.0 - lam_init, None, ALU.mult, None, [subg], [subg])
            vall = self.tile(es, [128, 16, 4, 130], BF16, "vall")
            self.MEMSET("pool", vall[:, :, :, 128:129], 1.0, [vall])
            for kt in range(16):
                p = self.ps()
                for k in range(8):
                    self.MM(p[:, :], xTs[:, k, kt * 128:(kt + 1) * 128], wv[:, k, :], k == 0, k == 7, [xTs, wv], [p])
                self.CP("dve" if kt % 2 else "act", vall[:, kt, :, 0:128], p[:, :].rearrange("p (h e) -> p h e", h=4), [p], [vall])
            wqk = [self.tile(es, [128, 4, 8, 128], BF16, "wqk") for _ in range(2)]
            qrot = [self.tile(es, [128, SEQ], BF16, "qrot") for _ in range(2)]
            krot = [self.tile(es, [128, SEQ], BF16, "krot") for _ in range(2)]
            rtmp = [self.tile(es, [128, 512], F32, "rtmp") for _ in range(4)]
            PT = [[self.tile(es, [128, 512], BF16, "PT") for _ in range(16)] for _ in range(2)]
            osb = [self.tile(es, [128, 128], F32, "osb") for _ in range(4)]
            ost = [self.tile(es, [128, 8], F32, "ost") for _ in range(4)]
            junk = [self.tile(es, [128, 128], F32, "ojunk") for _ in range(4)]
            ayst = [self.tile(es, [128, 512], BF16, "ayst") for _ in range(2)]
            for h in range(4):
                w = wqk[h % 2]
                S.dma("pool", w[:, 0, :, :], w_in[:, C_Q + h * 128:C_Q + (h + 1) * 128].rearrange("(k p) m -> p k m", p=128), [], [w])
                S.dma("pool", w[:, 1, :, :], w_in[:, C_K + h * 128:C_K + (h + 1) * 128].rearrange("(k p) m -> p k m", p=128), [], [w])
                for a in range(2):
                    src = w[:, a, :, :].rearrange("p k (c t j) -> p k c t j", c=2, t=2)
                    dst = w[:, 2 + a, :, :].rearrange("p k (c t j) -> p k c t j", c=2, t=2)
                    for c in range(2):
                        self.CP("pool", dst[:, :, c, 0, :], src[:, :, c, 1, :], [w], [w])
                        self.CP("pool", dst[:, :, c, 1, :], src[:, :, c, 0, :], [w], [w])
                qr = qrot[h % 2]; kr = krot[h % 2]
                ri = 0
                for a, dstt in ((0, qr), (1, kr)):
                    for tb in range(4):
                        sl = slice(tb * 512, (tb + 1) * 512)
                        p0 = self.ps(); p1 = self.ps()
                        for k in range(8):
                            self.MM(p0[:, :], w[:, a, k, :], xTs[:, k, sl], k == 0, k == 7, [w, xTs], [p0])
                        for k in range(8):
                            self.MM(p1[:, :], w[:, 2 + a, k, :], xTs[:, k, sl], k == 0, k == 7, [w, xTs], [p1])
                        ta = rtmp[ri % 4]; tb_ = rtmp[(ri + 1) % 4]; ri += 2
                        self.TT_("dve", ta[:], p0[:, :], rope[:, 0, sl], ALU.mult, [p0, rope], [ta])
                        self.TT_("dve", tb_[:], p1[:, :], rope[:, 1, sl], ALU.mult, [p1, rope], [tb_])
                        self.TT_("pool", dstt[:, sl], ta[:], tb_[:], ALU.add, [ta, tb_], [dstt])
                self.dbg("qr", qr, qr[:], [128, SEQ], BF16)
                self.dbg("kr", kr, kr[:], [128, SEQ], BF16)
                self.dbg("vall", vall, vall[:, 0, :, :], [128, 4, 130], BF16)
                for qb in range(4):
                    qs = slice(qb * 512, (qb + 1) * 512)
                    for kt in range(16):
                        for c in range(2):
                            p = self.ps()
                            self.MM(p[:, :], kr[c * 64:(c + 1) * 64, kt * 128:(kt + 1) * 128], qr[c * 64:(c + 1) * 64, qs], True, True, [kr, qr], [p])
                            self.ACT(PT[c][kt][:], p[:, :], AF.Exp, [p], [PT[c][kt]], scale=0.125)
                    def norm_gen(qt, r, qb=qb, h=h):
                        po = self.ps()
                        for c in range(2):
                            for kt in range(16):
                                self.MM(po[:, c * 256:c * 256 + 129], PT[c][kt][:, qt * 128:(qt + 1) * 128], vall[:, kt, h, 0:129], kt == 0, kt == 15, [PT[c][kt], vall], [po])
                        st = ost[r]; ob = osb[r]; jk = junk[r]
                        yield
                        self.CP("dve", st[:, 0:1], po[:, 128:129], [po], [st])
                        self.CP("dve", st[:, 1:2], po[:, 256 + 128:256 + 129], [po], [st])
                        yield
                        self.RECIP(st[:, 0:2], st[:, 0:2], [st], [st])
                        yield
                        self.TT_("dve", st[:, 1:2], st[:, 1:2], neglam, ALU.mult, [st, lsm], [st])
                        yield
                        self.TS("dve", ob[:], po[:, 0:128], st[:, 0:1], None, ALU.mult, None, [po, st], [ob])
                        yield
                        self.STT(ob[:], po[:, 256:256 + 128], st[:, 1:2], ob[:], ALU.mult, ALU.add, [po, st, ob], [ob])
                        yield
                        self.ACT(jk[:], ob[:], AF.Square, [ob], [jk])
                        yield
                        self.RED(st[:, 2:3], jk[:], ALU.add, [jk], [st])
                        yield
                        self.ACT(st[:, 3:4], st[:, 2:3], AF.Sqrt, [st, self.epsc], [st], bias=self.epsc[:, 0:1], scale=1.0 / 128.0)
                        yield
                        self.RECIP(st[:, 3:4], st[:, 3:4], [st], [st])
                        yield
                        self.STT(ob[:], ob[:], st[:, 3:4], subg[:], ALU.mult, ALU.mult, [ob, st, subg], [ob])
                        yield
                        pt = self.ps()
                        self.TR(pt[:, 0:128], ob[:], self.ident[:], [ob, self.ident], [pt])
                        ys = ayst[qb % 2]
                        self.CP("act", ys[:, qt * 128:(qt + 1) * 128], pt[:, 0:128], [pt], [ys])
                        if qt == 3:
                            S.dma("sp", self.ycT[:, 2 + h, self.t0 + qb * 512:self.t0 + (qb + 1) * 512], ys[:], [ys], [])

                    self.lockstep([norm_gen(qt, qt) for qt in range(4)])

    def dn_group(self, l, xTs, ycat, w_in):
        S = self.S
        dr = self.dr
        NCH = 16
        with contextlib.ExitStack() as es:
            ident, identb = self.ident, self.identb
            masks = self.tile(es, [128, 6, 128], F32, "masks")
            for i in range(6):
                S.dma("sp", masks[:, i, :], dr["c_masks"][i], [], [masks])
            qkv = self.tile(es, [128, NCH, 768], BF16, "qkvtm")
            zba = self.tile(es, [128, NCH, 272], F32, "zba")
            qkfm = self.tile(es, [128, 4, NCH, 128], BF16, "qkfm")
            gb = self.tile(es, [128, NCH, 16], F32, "gb")
            with contextlib.ExitStack() as es1:
                wdn = self.tile(es1, [128, 8, 768], BF16, "wdn")
                S.dma("pool", wdn[:], w_in[:, C_DN:C_DN + 768].rearrange("(k p) m -> p k m", p=128), [], [wdn])
                wz = self.tile(es1, [128, 8, 272], BF16, "wz")
                S.dma("pool", wz[:], w_in[:, C_Z:C_Z + 272].rearrange("(k p) m -> p k m", p=128), [], [wz])
                dcp = self.tile(es1, [128, 6, 5], F32, "dcp")
                S.dma("sp", dcp[:], dr["dn_cp"][l], [], [dcp])
                rows = self.tile(es1, [128, 16], F32, "dnrows")
                S.dma("sp", rows[:, 0:8], dr["dn_a_log"][l:l + 1, :].partition_broadcast(128), [], [rows])
                S.dma("sp", rows[:, 8:16], dr["dn_dt_bias"][l:l + 1, :].partition_broadcast(128), [], [rows])
                dpad = [self.tile(es1, [128, SEQ + 4], F32, "dpad")]
                dacc = [self.tile(es1, [128, SEQ], F32, "dacc")]
                qf32 = self.tile(es1, [128, NCH, 512], F32, "qf32")
                for j in range(1):
                    self.MEMSET("pool", dpad[j][:, 0:2], 0.0, [dpad[j]])
                    self.MEMSET("pool", dpad[j][:, SEQ + 2:SEQ + 4], 0.0, [dpad[j]])
                for ch in range(6):
                    dp = dpad[0]; da = dacc[0]
                    for tb in range(4):
                        p = self.ps()
                        for k in range(8):
                            self.MM(p[:, :], wdn[:, k, ch * 128:(ch + 1) * 128], xTs[:, k, tb * 512:(tb + 1) * 512], k == 0, k == 7, [wdn, xTs], [p])
                        self.CP("act", dp[:, 2 + tb * 512:2 + (tb + 1) * 512], p[:, :], [p], [dp])
                    self.TS("dve", da[:], dp[:, 0:SEQ], dcp[:, ch, 0:1], None, ALU.mult, None, [dp, dcp], [da])
                    for w in range(1, 5):
                        self.STT(da[:], dp[:, w:w + SEQ], dcp[:, ch, w:w + 1], da[:], ALU.mult, ALU.add, [dp, dcp, da], [da])
                    self.ACT(da[:], da[:], AF.Silu, [da], [da])
                    for tt in range(NCH):
                        p = self.ps()
                        self.TR(p[:, 0:128], da[:, tt * 128:(tt + 1) * 128], ident[:], [da, ident], [p])
                        if ch < 4:
                            self.CP("dve" if tt % 2 else "act", qf32[:, tt, ch * 128:(ch + 1) * 128], p[:, 0:128], [p], [qf32])
                        else:
                            self.CP("dve" if tt % 2 else "act", qkv[:, tt, ch * 128:(ch + 1) * 128], p[:, 0:128], [p], [qkv])
                for tt in range(NCH):
                    p = self.ps()
                    for k in range(8):
                        self.MM(p[:, 0:272], xTs[:, k, tt * 128:(tt + 1) * 128], wz[:, k, :], k == 0, k == 7, [xTs, wz], [p])
                    self.CP("dve" if tt % 2 else "act", zba[:, tt, :], p[:, 0:272], [p], [zba])
                sqt = [self.tile(es1, [128, 512], F32, "dsqt") for _ in range(2)]
                ss = self.tile(es1, [128, NCH, 8], F32, "dss")
                for tt in range(NCH):
                    self.ACT(sqt[tt % 2][:], qf32[:, tt, :], AF.Square, [qf32], [sqt[tt % 2]])
                    self.RED(ss[:, tt, :], sqt[tt % 2][:].rearrange("p (h d) -> p h d", h=8), ALU.add, [sqt[tt % 2]], [ss])
                self.rstd(ss[:], ss[:], [ss, self.epsc], [ss], self.epsc[:, 1:2])
                self.TS("dve", ss[:, :, 0:4], ss[:, :, 0:4], 0.125, None, ALU.mult, None, [ss], [ss])
                for tt in range(NCH):
                    self.TT_("dve", qf32[:, tt, :].rearrange("p (h d) -> p h d", h=8), qf32[:, tt, :].rearrange("p (h d) -> p h d", h=8),
                             ss[:, tt, :].unsqueeze(2).to_broadcast([128, 8, 64]), ALU.mult, [qf32, ss], [qf32])
                self.CP("pool", qkv[:, :, 0:512], qf32[:], [qf32], [qkv])
                for tt in range(NCH):
                    for i in range(4):
                        p = self.ps()
                        self.TR(p[:, 0:128], qf32[:, tt, i * 128:(i + 1) * 128], ident[:], [qf32, ident], [p])
                        self.CP("dve" if i % 2 else "act", qkfm[:, i, tt, :], p[:, 0:128], [p], [qkfm])
                self.ACT(gb[:, :, 0:8], zba[:, :, 256:264], AF.Sigmoid, [zba], [gb])
                self.TT_("dve", gb[:, :, 8:16], zba[:, :, 264:272], rows[:, 8:16].unsqueeze(1).to_broadcast([128, NCH, 8]), ALU.add, [zba, rows], [gb])
                self.ACT(gb[:, :, 8:16], gb[:, :, 8:16], AF.Exp, [gb], [gb])
                self.ACT(gb[:, :, 8:16], gb[:, :, 8:16], AF.Ln, [gb], [gb], bias=1.0)
                self.ACT(rows[:, 0:8], rows[:, 0:8], AF.Exp, [rows], [rows])
                self.STT(gb[:, :, 8:16], gb[:, :, 8:16], -1.0, rows[:, 0:8].unsqueeze(1).to_broadcast([128, NCH, 8]), ALU.mult, ALU.mult, [gb, rows], [gb])
                S.barrier()
            import os as _os
            kdn = _os.environ.get("KDN", "")
            if kdn == "A":
                return
            ng = self.tile(es, [128, 64], F32, "dng")
            S.dma("sp", ng[:], dr["dn_norm_g"][l:l + 1, :].partition_broadcast(128), [], [ng])
            sz = zba
            self.ACT(zba[:, :, 0:256], zba[:, :, 0:256], AF.Silu, [zba], [zba])
            ydst = self.tile(es, [128, 2, SEQ], BF16, "ydst")
            MT = [self.tile(es, [64, NCH, 64], BF16, "MT") for _ in range(2)]
            Bm = [self.tile(es, [64, NCH, 64], F32, "Bm") for _ in range(2)]
            PTs = [self.tile(es, [64, NCH, 128], BF16, "PTs") for _ in range(2)]
            QKm = [self.tile(es, [128, NCH, 128], BF16, "QKm") for _ in range(2)]
            Ua = [self.tile(es, [128, NCH, 64], BF16, "Ua") for _ in range(2)]
            Sa = [self.tile(es, [64, NCH, 64], BF16, "Sa") for _ in range(2)]
            G = 4
            NR = G

            def wt(shape, dt, name):
                return [self.tile(es, shape, dt, name) for _ in range(NR)]
            gL = wt([128, 128], F32, "gL"); sc = wt([128, 8], F32, "dsc"); E = wt([128, 128], F32, "dE")
            Gi = wt([128, 128], F32, "Gi"); Gs = wt([128, 128], F32, "Gs")
            PP = [wt([128, 256], F32, "PP%d" % i) for i in range(2)]
            Z = [wt([128, 128], F32, "Z%d" % i) for i in range(2)]
            UW = wt([128, 128], BF16, "UW"); nW = wt([128, 64], BF16, "nW")
            Kd = wt([128, 64], BF16, "Kd"); Qd = wt([128, 64], BF16, "Qd")
            osum = self.tile(es, [128, 2, NCH], F32, "dosum")
            ojunk = self.tile(es, [128, 2, NCH * 64], F32, "dojunk")

            def lockstep(gens):
                gens = list(gens)
                while gens:
                    alive = []
                    for g_ in gens:
                        try:
                            next(g_)
                            alive.append(g_)
                        except StopIteration:
                            pass
                    gens = alive

            def chain(h, d, n, r):
                hb = (h % 2) * 64
                qi_ = h // 2
                Ld = masks[:, d, :]
                Mi = masks[:, 2 + 2 * d, :]
                Ms = masks[:, 3 + 2 * d, :]
                lastc = 127 if d == 0 else 0
                gcol = gb[:, n, 8 + d * 4 + h:8 + d * 4 + h + 1]
                bcol = gb[:, n, d * 4 + h:d * 4 + h + 1]
                kfm = qkfm[hb:hb + 64, 2 + qi_, n, :]
                qfm = qkfm[hb:hb + 64, qi_, n, :]
                q_tm = qkv[:, n, h * 64:(h + 1) * 64]
                k_tm = qkv[:, n, 256 + h * 64:256 + (h + 1) * 64]
                v_tm = qkv[:, n, 512 + h * 64:512 + (h + 1) * 64]
                s_ = sc[r]
                P0 = PP[0][r]
                self.ACT(gL[r][:], Ld, AF.Copy, [masks, gb], [gL[r]], scale=gcol)
                yield
                pg = self.ps()
                self.MM(pg[:, 0:128], self.onesm[:], gL[r][:], True, True, [self.onesm, gL[r]], [pg])
                self.MM(pg[:, 128:129], Ld, gcol, True, True, [masks, gb], [pg])
                pk = self.ps()
                self.MM(pk[:, 0:128], kfm, kfm, True, True, [qkfm], [pk])
                self.MM(pk[:, 128:256], kfm, qfm, True, True, [qkfm], [pk])
                yield
                self.TS("dve", s_[:, 0:1], pg[:, 128:129], -1.0, None, ALU.mult, None, [pg], [s_])
                self.CP("dve", s_[:, 4:5], pg[:, lastc:lastc + 1], [pg], [s_])
                self.TS("dve", E[r][:], pg[:, 0:128], s_[:, 0:1], 0.0, ALU.add, ALU.min, [pg, s_], [E[r]])
                yield
                self.ACT(E[r][:], E[r][:], AF.Exp, [E[r]], [E[r]])
                self.ACT(s_[:, 1:2], s_[:, 0:1], AF.Exp, [s_], [s_], scale=-1.0)
                self.ACT(s_[:, 2:3], s_[:, 4:5], AF.Exp, [s_], [s_], bias=s_[:, 0:1], scale=1.0)
                self.ACT(s_[:, 3:4], s_[:, 4:5], AF.Exp, [s_], [s_])
                yield
                self.TT_("pool", Gs[r][:], E[r][:], Ms, ALU.mult, [E[r], masks], [Gs[r]])
                self.TT_("pool", Gi[r][:], E[r][:], Mi, ALU.mult, [E[r], masks], [Gi[r]])
                self.CP("act", Z[0][r][:, 0:64], v_tm, [qkv], [Z[0][r]])
                self.ACT(Z[0][r][:, 64:128], k_tm, AF.Copy, [qkv, s_], [Z[0][r]], scale=s_[:, 1:2])
                self.ACT(Kd[r][:], k_tm, AF.Copy, [qkv, s_], [Kd[r]], scale=s_[:, 2:3])
                self.ACT(Qd[r][:], q_tm, AF.Copy, [qkv, s_], [Qd[r]], scale=s_[:, 1:2])
                yield
                self.STT(P0[:, 0:128], pk[:, 0:128], bcol, Gs[r][:], ALU.mult, ALU.mult, [pk, gb, Gs[r]], [P0])
                self.TT_("dve", QKm[d][:, n, :], pk[:, 128:256], Gi[r][:], ALU.mult, [pk, Gi[r]], [QKm[d]])
                yield
                px = self.ps()
                self.TR(px[:, 0:128], P0[:, 0:128], ident[:], [P0, ident], [px])
                yield
                self.CP("act", P0[:, 128:256], px[:, 0:128], [px], [P0])
                yield
                zc = 0
                for k in range(7):
                    cur = PP[k % 2][r]; nxt = PP[(k + 1) % 2][r]
                    pz = self.ps()
                    self.MM(pz[:, 0:128], cur[:, 0:128], Z[zc][r][:], True, True, [cur, Z[zc][r]], [pz])
                    if k < 6:
                        pp = self.ps()
                        self.MM(pp[:, 0:128], cur[:, 128:256], cur[:, 0:128], True, True, [cur], [pp])
                        if k < 5:
                            self.MM(pp[:, 128:256], cur[:, 0:128], cur[:, 128:256], True, True, [cur], [pp])
                    yield
                    self.TT_("dve", Z[1 - zc][r][:], Z[zc][r][:], pz[:, 0:128], ALU.subtract if k == 0 else ALU.add, [Z[zc][r], pz], [Z[1 - zc][r]])
                    zc = 1 - zc
                    if k < 5:
                        self.CP("act", nxt[:, 0:256], pp[:, 0:256], [pp], [nxt])
                    elif k == 5:
                        self.CP("act", nxt[:, 0:128], pp[:, 0:128], [pp], [nxt])
                    yield
                Zf = Z[zc][r]
                self.TS("dve", UW[r][:], Zf[:], bcol, None, ALU.mult, None, [Zf, gb], [UW[r]])
                yield
                self.ACT(nW[r][:], UW[r][:, 64:128], AF.Copy, [UW[r]], [nW[r]], scale=-1.0)
                self.CP("act", Ua[d][:, n, :], UW[r][:, 0:64], [UW[r]], [Ua[d]])
                pm = self.ps()
                self.MM(pm[0:64, 0:64], UW[r][:, 64:128], Kd[r][:], True, True, [UW[r], Kd[r]], [pm])
                self.MM(pm[0:64, 64:128], Kd[r][:], UW[r][:, 0:64], True, True, [UW[r], Kd[r]], [pm])
                yield
                self.STT(MT[d][:, n, :], ident[0:64, 0:64], s_[0:64, 3:4], pm[0:64, 0:64], ALU.mult, ALU.subtract, [ident, s_, pm], [MT[d]])
                self.CP("dve", Bm[d][:, n, :], pm[0:64, 64:128], [pm], [Bm[d]])
                pq = self.ps()
                self.MM(pq[0:64, 0:128], Qd[r][:], identb[:], True, False, [Qd[r], identb], [pq])
                self.MM(pq[0:64, 0:128], nW[r][:], QKm[d][:, n, :], False, True, [nW[r], QKm[d]], [pq])
                yield
                self.CP("dve", PTs[d][:, n, :], pq[0:64, 0:128], [pq], [PTs[d]])

            def scan(d):
                order = list(range(NCH)) if d == 0 else list(range(NCH - 1, -1, -1))
                self.MEMSET("pool", Sa[d][:, order[0], :], 0.0, [Sa[d]])
                yield
                for i in range(NCH - 1):
                    n = order[i]; nn = order[i + 1]
                    pss = self.ps()
                    self.MM(pss[0:64, 0:64], MT[d][:, n, :], Sa[d][:, n, :], True, True, [MT[d], Sa[d]], [pss])
                    yield
                    self.TT_("dve", Sa[d][:, nn, :], pss[0:64, 0:64], Bm[d][:, n, :], ALU.add, [pss, Bm[d]], [Sa[d]])
                    yield

            for h in range(4):
                for d in range(2):
                    for n0 in range(0, NCH, G):
                        lockstep([chain(h, d, n0 + g_, g_) for g_ in range(G)])
                lockstep([scan(0), scan(1)])
                oc = ojunk[:, 0, :]; sq = ojunk[:, 1, :]
                for half in range(2):
                    po = self.ps()
                    for j in range(8):
                        n = half * 8 + j
                        for d in range(2):
                            self.MM(po[:, j * 64:(j + 1) * 64], PTs[d][:, n, :], Sa[d][:, n, :], d == 0, False, [PTs[d], Sa[d]], [po])
                            self.MM(po[:, j * 64:(j + 1) * 64], QKm[d][:, n, :], Ua[d][:, n, :], False, d == 1, [QKm[d], Ua[d]], [po])
                    self.CP("dve", ojunk[:, 0, half * 512:(half + 1) * 512], po[:, :], [po], [ojunk])
                oc3 = oc.rearrange("p (n e) -> p n e", e=64)
                self.ACT(sq, oc, AF.Square, [ojunk], [ojunk])
                self.RED(osum[:, 0, :], sq.rearrange("p (n e) -> p n e", e=64), ALU.add, [ojunk], [osum])
                self.ACT(osum[:, 1, :], osum[:, 0, :], AF.Sqrt, [osum, self.epsc], [osum], bias=self.epsc[:, 0:1], scale=1.0 / 64.0)
                self.RECIP(osum[:, 1, :], osum[:, 1, :], [osum], [osum])
                self.TT_("dve", oc3, oc3, osum[:, 1, :].unsqueeze(2).to_broadcast([128, NCH, 64]), ALU.mult, [ojunk, osum], [ojunk])
                self.TT_("dve", oc3, oc3, ng[:].unsqueeze(1).to_broadcast([128, NCH, 64]), ALU.mult, [ojunk, ng], [ojunk])
                yv = sz[:, :, h * 64:(h + 1) * 64]
                self.TT_("dve", yv, yv, oc3, ALU.mult, [sz, ojunk], [sz])
            for n in range(NCH):
                for c in range(2):
                    p = self.ps()
                    self.TR(p[:, 0:128], sz[:, n, c * 128:(c + 1) * 128], ident[:], [sz, ident], [p])
                    self.CP("act" if c else "dve", ydst[:, c, n * 128:(n + 1) * 128], p[:, 0:128], [p], [ydst])
            for c in range(2):
                S.dma("sp", self.ycT[:, 6 + c, self.t0:self.t0 + SEQ], ydst[:, c, :], [ydst], [])

    def out_proj(self, l, s, ycat):
        S = self.S
        dr = self.dr
        moe = (l % 2 == 1)
        with contextlib.ExitStack() as es:
            wo = self.tile(es, [128, 8, D], BF16, "wo")
            S.dma("pool", wo[:], dr["w_o"][l].rearrange("(k p) m -> p k m", p=128), [], [wo])
            gt = self.tile(es, [128, D], F32, "l1g"); bt = self.tile(es, [128, D], F32, "l1b")
            S.dma("sp", gt[:], dr["ln1_g"][l:l + 1, :].partition_broadcast(128), [], [gt])
            S.dma("sp", bt[:], dr["ln1_b"][l:l + 1, :].partition_broadcast(128), [], [bt])
            router = None
            if moe and "nortile" not in self.kgate:
                router = self.tile(es, [128, 8, NEXP], F32, "wr")
                if "nordma" in self.kgate:
                    self.MEMSET("pool", router[:], 0.0, [router])
                else:
                    S.dma("sp", router[:], dr["router_w"][l // 2], [], [router])
            GT = 4
            xin = [self.tile(es, [128, D], F32, "xin") for _ in range(GT)]
            tt_ = [self.tile(es, [128, D], F32, "tsum") for _ in range(GT)]
            xo = [self.tile(es, [128, D], F32, "xo") for _ in range(GT)]
            st = [self.tile(es, [128, 8], F32, "lnst") for _ in range(GT)]
            src = dr["x"] if l == 0 else self.xres

            def tile_gen(i, r):
                tg = s * (SEQ // 128) + i
                xi = xin[r]; t = tt_[r]; o = xo[r]
                S.dma("sp", xi[:], src[tg * 128:(tg + 1) * 128, :], [], [xi])
                yield
                for half in range(2):
                    p = self.ps()
                    for k in range(8):
                        self.MM(p[:, :], ycat[:, k, i * 128:(i + 1) * 128], wo[:, k, half * 512:(half + 1) * 512], k == 0, k == 7, [ycat, wo], [p])
                    self.STT(t[:, half * 512:(half + 1) * 512], xi[:, half * 512:(half + 1) * 512], ALPHA, p[:, :], ALU.mult, ALU.add, [xi, p], [t])
                yield
                yield from self.layer_norm_gen(t, o, gt, bt, st[r])
                S.dma("sp", self.xres[tg * 128:(tg + 1) * 128, :], o[:], [o], [])
                self.emit_xT(es, o, tg, router, l)

            for i0 in range(0, SEQ // 128, GT):
                self.lockstep([tile_gen(i0 + g_, g_) for g_ in range(GT)])

    def ffn(self, l, last):
        S = self.S
        dr = self.dr
        moe = (l % 2 == 1)
        j = l // 2
        NT = self.ntok
        TB = 1024
        G = 4
        nff = (DFFE if moe else DFF) // 128
        groups = [(c0, min(G, nff - c0)) for c0 in range(0, nff, G)]
        nexp = NEXP if moe else 1
        with contextlib.ExitStack() as es:
            gt = self.tile(es, [128, D], F32, "l2g"); bt = self.tile(es, [128, D], F32, "l2b")
            S.dma("sp", gt[:], dr["ln2_g"][l:l + 1, :].partition_broadcast(128), [], [gt])
            S.dma("sp", bt[:], dr["ln2_b"][l:l + 1, :].partition_broadcast(128), [], [bt])
            xTb = self.tile(es, [128, 8, TB], BF16, "xTb")
            hT = [self.tile(es, [128, G, TB], BF16, "hT") for _ in range(2)]
            w1g = [self.tile(es, [128, 8, G * 128], BF16, "w1g") for _ in range(2)]
            w3g = [self.tile(es, [128, 8, G * 128], BF16, "w3g") for _ in range(2)]
            w2g = [self.tile(es, [128, G, D], BF16, "w2g") for _ in range(2)]
            acc = self.tile(es, [128, TB // 128, D], F32, "facc")
            sil = [self.tile(es, [128, 512], F32, "sil") for _ in range(3)]
            GT = 4
            xin = [self.tile(es, [128, D], F32, "fxin") for _ in range(GT)]
            xo = [self.tile(es, [128, D], F32, "fxo") for _ in range(GT)]
            st = [self.tile(es, [128, 8], F32, "flnst") for _ in range(GT)]
            gi = 0
            si = 0
            for tb in range(NT // TB):
                t0 = tb * TB
                for k in range(8):
                    S.dma("sp", xTb[:, k, :], self.xT[:, k, t0:t0 + TB], [], [xTb])
                first = True
                for e in range(nexp):
                    if moe:
                        W1 = dr["moe_w1"][j, e]; W3 = dr["moe_w3"][j, e]; W2 = dr["moe_w2"][j, e]
                    else:
                        W1 = dr["ffn_w1"][j]; W3 = dr["ffn_w3"][j]; W2 = dr["ffn_w2"][j]
                    for (c0, gn) in groups:
                        r = gi % 2
                        gi += 1
                        S.dma("pool", w1g[r][:, :, 0:gn * 128], W1[:, c0 * 128:(c0 + gn) * 128].rearrange("(k p) m -> p k m", p=128), [], [w1g[r]])
                        S.dma("pool", w3g[r][:, :, 0:gn * 128], W3[:, c0 * 128:(c0 + gn) * 128].rearrange("(k p) m -> p k m", p=128), [], [w3g[r]])
                        S.dma("pool", w2g[r][:, 0:gn, :], W2[c0 * 128:(c0 + gn) * 128, :].rearrange("(c p) m -> p c m", p=128), [], [w2g[r]])
                        for c in range(gn):
                            for hb in range(TB // 512):
                                ts_ = slice(hb * 512, (hb + 1) * 512)
                                pa = self.ps(); pb = self.ps()
                                for k in range(8):
                                    self.MM(pa[:, :], w1g[r][:, k, c * 128:(c + 1) * 128], xTb[:, k, ts_], k == 0, k == 7, [w1g[r], xTb], [pa])
                                for k in range(8):
                                    self.MM(pb[:, :], w3g[r][:, k, c * 128:(c + 1) * 128], xTb[:, k, ts_], k == 0, k == 7, [w3g[r], xTb], [pb])
                                sl_ = sil[si % 3]
                                si += 1
                                self.ACT(sl_[:], pa[:, :], AF.Silu, [pa], [sl_])
                                self.TT_("dve", hT[r][:, c, ts_], pb[:, :], sl_[:], ALU.mult, [pb, sl_], [hT[r]])
                        for tt in range(TB // 128):
                            tg = tb * (TB // 128) + tt
                            for half in range(2):
                                p = self.ps()
                                for c in range(gn):
                                    self.MM(p[:, :], hT[r][:, c, tt * 128:(tt + 1) * 128], w2g[r][:, c, half * 512:(half + 1) * 512], c == 0, c == gn - 1, [hT[r], w2g[r]], [p])
                                a_ = acc[:, tt, half * 512:(half + 1) * 512]
                                if moe:
                                    cw = self.comb[:, tg, e:e + 1]
                                    if first:
                                        self.TS("dve", a_, p[:, :], cw, None, ALU.mult, None, [p, self.comb], [acc])
                                    else:
                                        self.STT(a_, p[:, :], cw, a_, ALU.mult, ALU.add, [p, self.comb, acc], [acc])
                                else:
                                    if first:
                                        self.CP("dve", a_, p[:, :], [p], [acc])
                                    else:
                                        self.TT_("dve", a_, p[:, :], a_, ALU.add, [p, acc], [acc])
                        first = False
                def ep_gen(tt, r, tb=tb):
                    tg = tb * (TB // 128) + tt
                    xi = xin[r]; o = xo[r]
                    S.dma("sp", xi[:], self.xres[tg * 128:(tg + 1) * 128, :], [], [xi])
                    yield
                    self.STT(xi[:], xi[:], ALPHA, acc[:, tt, :], ALU.mult, ALU.add, [xi, acc], [xi])
                    yield
                    yield from self.layer_norm_gen(xi, o, gt, bt, st[r])
                    if last:
                        S.dma("sp", self.y_out[tg * 128:(tg + 1) * 128, :], o[:], [o], [])
                    else:
                        S.dma("sp", self.xres[tg * 128:(tg + 1) * 128, :], o[:], [o], [])
                        self.emit_xT(es, o, tg, None, l)

                for t0_ in range(0, TB // 128, GT):
                    self.lockstep([ep_gen(t0_ + g_, g_) for g_ in range(GT)])
            S.barrier()


def _consts():
    ident = np.eye(128, dtype=np.float32)
    p = np.arange(128)[:, None]
    f = np.arange(128)[None, :]
    Lf = (p <= f).astype(np.float32)
    Lb = (p >= f).astype(np.float32)
    masks = np.stack([Lf, Lb, (p <= f), (p < f), (p >= f), (p > f)]).astype(np.float32)
    inv = (10000.0 ** (-np.arange(0, 64, 2, dtype=np.float32) / 64.0)).astype(np.float32)
    ang = np.arange(SEQ, dtype=np.float32)[:, None] * inv[None, :]
    cos = np.cos(ang).astype(np.float32).T
    sin = np.sin(ang).astype(np.float32).T
    cosT = np.concatenate([cos, cos, cos, cos], 0)
    sinT = np.concatenate([-sin, sin, -sin, sin], 0)
    rope = np.stack([cosT, sinT]).astype(np.float32)
    return ident, masks, rope


_CACHE = {}
_LAST = None


def run(inputs, n_cores=8, n_layers=DEPTH, nseq=2):
    key = (n_cores, n_layers, nseq)
    nc = bass.Bass("TRN2", target_bir_lowering=False)
    Builder(nc, nseq, n_layers).build()
    ident, masks, rope = _consts()
    f = lambda a: np.ascontiguousarray(np.asarray(a, dtype=np.float32))
    x = f(inputs["x"])
    cdw = f(inputs["conv_dw"])
    cp = np.concatenate([cdw.transpose(0, 2, 1), f(inputs["conv_dw_b"])[:, :, None], f(inputs["conv_ln_g"])[:, :, None],
                         f(inputs["conv_ln_b"])[:, :, None]], axis=2)
    cp = np.ascontiguousarray(cp.reshape(DEPTH, 2, 128, 34).transpose(0, 2, 1, 3))
    dcp = f(inputs["dn_conv"]).transpose(0, 2, 1)
    dcp = np.ascontiguousarray(dcp.reshape(DEPTH, 6, 128, 5).transpose(0, 2, 1, 3))
    shared = {
        "w_in": f(inputs["w_in"]), "w_o": f(inputs["w_o"]),
        "ln1_g": f(inputs["ln1_g"]), "ln1_b": f(inputs["ln1_b"]), "ln2_g": f(inputs["ln2_g"]), "ln2_b": f(inputs["ln2_b"]),
        "conv_cp": cp, "conv_pw": f(inputs["conv_pw"]),
        "diff_lambda": f(inputs["diff_lambda"]).reshape(DEPTH, 256), "diff_subln_g": f(inputs["diff_subln_g"]),
        "dn_cp": dcp, "dn_a_log": f(inputs["dn_a_log"]).reshape(DEPTH, 8), "dn_dt_bias": f(inputs["dn_dt_bias"]).reshape(DEPTH, 8),
        "dn_norm_g": f(inputs["dn_norm_g"]),
        "c_ident": ident, "c_masks": masks, "c_rope": rope,
    }
    nd = (n_layers + 1) // 2
    nm = n_layers // 2
    for k_ in ("ffn_w1", "ffn_w3", "ffn_w2"):
        shared[k_] = f(inputs[k_])[:nd]
    if nm > 0:
        for k_ in ("moe_w1", "moe_w3", "moe_w2"):
            shared[k_] = f(inputs[k_])[:nm]
        shared["router_w"] = np.ascontiguousarray(f(inputs["router_w"])[:nm].reshape(nm, 8, 128, NEXP).transpose(0, 2, 1, 3))
    in_maps = []
    for c in range(n_cores):
        m = dict(shared)
        m["x"] = np.ascontiguousarray(x[c * nseq:(c + 1) * nseq].reshape(nseq * SEQ, D))
        in_maps.append(m)
    import os as _os
    if _os.environ.get("KTRACE"):
        res = run_bass_kernel_spmd(nc, in_maps, core_ids=list(range(n_cores)), trace=True)
        print("EXEC_TIME_NS", res.exec_time_ns, flush=True)
    else:
        res = run_bass_kernel_spmd(nc, in_maps, core_ids=list(range(n_cores)))
    global _LAST
    _LAST = res.results
    out = np.stack([res.results[c]["y"].reshape(nseq, SEQ, D) for c in range(n_cores)], 0)
    return out.reshape(n_cores * nseq, SEQ, D).astype(np.float32)


def kernel(**inputs):
    return run(inputs, n_cores=8, n_layers=DEPTH, nseq=2)
```
